# Optimizing a Trainium2 kernel written in Bass

```python
import math
import jax
import jax.numpy as jnp
from jax import lax
import numpy as np

D_MODEL = 2048
BATCH = 2
SEQ = 4096
DEPTH = 2

CTX_LEN = 256
GRID_W = 64
EPS = 1e-6

N_HEADS = D_MODEL // 128
Q_LORA = 512
KV_LORA = 512
NOPE_DIM = 128
ROPE_DIM = 64
V_DIM = 128
ROPE_BASE = 10000.0
Q_BLOCK = 128
SM_SCALE = (NOPE_DIM + ROPE_DIM) ** -0.5

D_INNER = 2 * D_MODEL
SSM_HEAD_DIM = 64
SSM_HEADS = D_INNER // SSM_HEAD_DIM
SSM_GROUPS = 8
D_STATE = 128
CONV_K = 5
CONV_DIM = D_INNER + 2 * SSM_GROUPS * D_STATE
CHUNK = 128

PROJ_SIZES = (Q_LORA, KV_LORA, ROPE_DIM, D_INNER, CONV_DIM, 2 * SSM_HEADS, D_MODEL, D_MODEL)
IN_COLS = sum(PROJ_SIZES)

D_FF = 256 * ((8 * D_MODEL // 3 + 255) // 256)
N_EXPERTS = 8
TOP_K = 2
D_FF_EXPERT = 7 * D_MODEL // 2
MOE_BLOCK = 128
N_DENSE = (DEPTH + 1) // 2
N_MOE = DEPTH // 2

kernel_name = 'hybrid_mla_ssd_moe_dit_block'


def rms_norm(x, g):
    xf = x.astype(jnp.float32)
    y = xf * lax.rsqrt(jnp.mean(xf * xf, axis=-1, keepdims=True) + EPS)
    return y.astype(x.dtype) * g


def modulate(h, shift, scale):
    return h * (1 + scale) + shift


def swiglu(t, w1, w3, w2):
    return (jax.nn.silu(t @ w1) * (t @ w3)) @ w2


def split_proj(p):
    idx = np.cumsum(PROJ_SIZES)[:-1]
    return jnp.split(p, [int(i) for i in idx], axis=-1)


def axial_rope_tables(n):
    rows = n // GRID_W
    row = jnp.repeat(jnp.arange(rows, dtype=jnp.float32), GRID_W)
    col = jnp.tile(jnp.arange(GRID_W, dtype=jnp.float32), rows)
    axis_dim = ROPE_DIM // 2
    inv = ROPE_BASE ** (-jnp.arange(0, axis_dim, 2, dtype=jnp.float32) / axis_dim)
    ang = jnp.stack([row[:, None] * inv, col[:, None] * inv], axis=1)
    return jnp.cos(ang), jnp.sin(ang)


def apply_axial_rope(t, cos, sin):
    B, n, H, _ = t.shape
    tr = t.reshape(B, n, H, 2, 2, ROPE_DIM // 4)
    t1, t2 = tr[..., 0, :], tr[..., 1, :]
    cs = cos[None, :, None].astype(t.dtype)
    sn = sin[None, :, None].astype(t.dtype)
    out = jnp.stack([t1 * cs - t2 * sn, t2 * cs + t1 * sn], axis=-2)
    return out.reshape(B, n, H, ROPE_DIM)


def mla_qkv(c_q, c_kv, q_norm, w_uq, kv_norm, w_ukv):
    B, N = c_q.shape[:2]
    q = (rms_norm(c_q, q_norm) @ w_uq).reshape(B, N, N_HEADS, NOPE_DIM + ROPE_DIM)
    kv = (rms_norm(c_kv, kv_norm) @ w_ukv).reshape(B, N, N_HEADS, NOPE_DIM + V_DIM)
    return q[..., :NOPE_DIM], q[..., NOPE_DIM:], kv[..., :NOPE_DIM], kv[..., NOPE_DIM:]


def mla_attend(qn, qr, kn, kr, v):
    s = jnp.einsum('bqhd,bkhd->bhqk', qn, kn) + jnp.einsum('bqhr,bkr->bhqk', qr, kr)
    p = jax.nn.softmax(s.astype(jnp.float32) * SM_SCALE, axis=-1).astype(v.dtype)
    return jnp.einsum('bhqk,bkhd->bqhd', p, v)


def blocked_attend(qn, qr, kn, kr, v):
    B, Q, H, _ = qn.shape
    nb = Q // Q_BLOCK
    blk = lambda t: jnp.moveaxis(t.reshape(B, nb, Q_BLOCK, *t.shape[2:]), 1, 0)
    o = lax.map(lambda qs: mla_attend(qs[0], qs[1], kn, kr, v), (blk(qn), blk(qr)))
    return jnp.moveaxis(o, 0, 1).reshape(B, Q, H * V_DIM)


def dwconv_silu(u, w, b):
    y = lax.conv_general_dilated(u, w[:, None, :], window_strides=(1,),
                                 padding=[(CONV_K // 2, CONV_K // 2)],
                                 dimension_numbers=('NWC', 'WIO', 'NWC'),
                                 feature_group_count=u.shape[-1])
    return jax.nn.silu(y + b)


def ssd_inputs(xbc_raw, dt_raw, conv_w, conv_b, dt_bias):
    B, N, _ = xbc_raw.shape
    xbc = dwconv_silu(xbc_raw, conv_w, conv_b)
    gs = SSM_GROUPS * D_STATE
    xs = xbc[..., :D_INNER].reshape(B, N, SSM_HEADS, SSM_HEAD_DIM)
    bm = xbc[..., D_INNER:D_INNER + gs].reshape(B, N, SSM_GROUPS, D_STATE)
    cm = xbc[..., D_INNER + gs:].reshape(B, N, SSM_GROUPS, D_STATE)
    dt = jax.nn.softplus(dt_raw.astype(jnp.float32).reshape(B, N, 2, SSM_HEADS) + dt_bias.astype(jnp.float32))
    return xs, bm, cm, dt


def ssd_scan(x, dt, A, bm, cm, init_state, with_output):
    Bsz, N, H, P = x.shape
    G, DS = bm.shape[2], bm.shape[3]
    E = H // G
    nc = N // CHUNK
    dtype = x.dtype
    xc = (x * dt[..., None].astype(dtype)).reshape(Bsz, nc, CHUNK, G, E, P)
    a = jnp.moveaxis((dt * A).reshape(Bsz, nc, CHUNK, G, E), 2, -1)
    a_cum = jnp.cumsum(a, axis=-1)
    bc = bm.reshape(Bsz, nc, CHUNK, G, DS)
    cc = cm.reshape(Bsz, nc, CHUNK, G, DS)
    to_end = jnp.exp(a_cum[..., -1:] - a_cum).astype(dtype)
    states = jnp.einsum('bcsgn,bcges,bcsgep->bcgepn', bc, to_end, xc)
    chunk_decay = jnp.exp(a_cum[..., -1]).astype(dtype)

    def step(carry, inp):
        st, dec = inp
        return carry * dec[..., None, None] + st, carry

    final, prev = lax.scan(step, init_state.reshape(Bsz, G, E, P, DS),
                           (jnp.moveaxis(states, 1, 0), jnp.moveaxis(chunk_decay, 1, 0)))
    final = final.reshape(Bsz, H, P, DS)
    if not with_output:
        return None, final
    prev = jnp.moveaxis(prev, 0, 1)
    lower = jnp.tril(jnp.ones((CHUNK, CHUNK), dtype=bool))
    seg = jnp.where(lower, a_cum[..., :, None] - a_cum[..., None, :], -jnp.inf)
    decay = jnp.exp(seg).astype(dtype)
    cb = jnp.einsum('bclgn,bcsgn->bcgls', cc, bc)
    y_diag = jnp.einsum('bcgls,bcgels,bcsgep->bclgep', cb, decay, xc)
    y_off = jnp.einsum('bclgn,bcgepn,bcgel->bclgep', cc, prev, jnp.exp(a_cum).astype(dtype))
    return (y_diag + y_off).reshape(Bsz, N, H, P), final


def gated_group_rmsnorm(y, z, w):
    B, N, _ = y.shape
    u = (y * jax.nn.silu(z)).reshape(B, N, SSM_GROUPS, D_INNER // SSM_GROUPS).astype(jnp.float32)
    u = u * lax.rsqrt(jnp.mean(u * u, axis=-1, keepdims=True) + EPS)
    return u.reshape(B, N, D_INNER).astype(y.dtype) * w


def merge_branches(att, ssm, z, g_a, g_b, ssm_norm, w_oa, w_ob, w_out):
    B, N = att.shape[:2]
    o_a = att.reshape(B, N, -1) @ w_oa
    o_b = gated_group_rmsnorm(ssm.reshape(B, N, D_INNER), z, ssm_norm) @ w_ob
    return (jax.nn.sigmoid(g_a) * o_a + jax.nn.sigmoid(g_b) * o_b) @ w_out


def token_mixer(h_ctx, h_lat, cos, sin, w_in, q_norm, w_uq, kv_norm, w_ukv, conv_w, conv_b,
                a_log, dt_bias, d_skip, ssm_norm, w_oa, w_ob, w_out, need_ctx):
    cq_c, ckv_c, kr_c, z_c, xbc_c, dtr_c, ga_c, gb_c = split_proj(h_ctx @ w_in)
    cq_l, ckv_l, kr_l, z_l, xbc_l, dtr_l, ga_l, gb_l = split_proj(h_lat @ w_in)
    qn_c, qr_c, kn_c, v_c = mla_qkv(cq_c, ckv_c, q_norm, w_uq, kv_norm, w_ukv)
    qn_l, qr_l, kn_l, v_l = mla_qkv(cq_l, ckv_l, q_norm, w_uq, kv_norm, w_ukv)
    qr_l = apply_axial_rope(qr_l, cos, sin)
    kr_l = apply_axial_rope(kr_l[:, :, None], cos, sin)[:, :, 0]
    kn = jnp.concatenate([kn_c, kn_l], axis=1)
    kr = jnp.concatenate([kr_c, kr_l], axis=1)
    v = jnp.concatenate([v_c, v_l], axis=1)
    att_l = blocked_attend(qn_l, qr_l, kn, kr, v)
    xs_c, b_c, c_c, dt_c = ssd_inputs(xbc_c, dtr_c, conv_w, conv_b, dt_bias)
    xs_l, b_l, c_l, dt_l = ssd_inputs(xbc_l, dtr_l, conv_w, conv_b, dt_bias)
    A = -jnp.exp(a_log.astype(jnp.float32))
    d_sum = (d_skip[0] + d_skip[1])[:, None]
    flip = lambda t: jnp.flip(t, axis=1)
    zeros = jnp.zeros((xs_c.shape[0], SSM_HEADS, SSM_HEAD_DIM, D_STATE), xs_c.dtype)
    y_cf, s_f = ssd_scan(xs_c, dt_c[:, :, 0], A[0], b_c, c_c, zeros, need_ctx)
    y_cb, s_b = ssd_scan(flip(xs_c), flip(dt_c[:, :, 1]), A[1], flip(b_c), flip(c_c), zeros, need_ctx)
    y_lf, _ = ssd_scan(xs_l, dt_l[:, :, 0], A[0], b_l, c_l, s_f, True)
    y_lb, _ = ssd_scan(flip(xs_l), flip(dt_l[:, :, 1]), A[1], flip(b_l), flip(c_l), s_b, True)
    ssm_l = y_lf + flip(y_lb) + d_sum * xs_l
    out_l = merge_branches(att_l, ssm_l, z_l, ga_l, gb_l, ssm_norm, w_oa, w_ob, w_out)
    if not need_ctx:
        return None, out_l
    att_c = mla_attend(qn_c, qr_c, kn_c, kr_c, v_c)
    ssm_c = y_cf + flip(y_cb) + d_sum * xs_c
    out_c = merge_branches(att_c, ssm_c, z_c, ga_c, gb_c, ssm_norm, w_oa, w_ob, w_out)
    return out_c, out_l


def moe_swiglu(h, w_router, w1, w3, w2):
    B, N, D = h.shape
    T = B * N
    t = h.reshape(T, D)
    logits = (t @ w_router).astype(jnp.float32)
    top_v, top_i = lax.top_k(logits, TOP_K)
    gate = jax.nn.softmax(top_v, axis=-1).astype(h.dtype).reshape(-1)
    expert = top_i.reshape(-1)
    token = jnp.repeat(jnp.arange(T), TOP_K)
    order = jnp.argsort(expert)
    s_exp, s_tok, s_gate = expert[order], token[order], gate[order]
    counts = jnp.bincount(expert, length=N_EXPERTS)
    padded = (counts + MOE_BLOCK - 1) // MOE_BLOCK * MOE_BLOCK
    pad_end = jnp.cumsum(padded)
    rank = jnp.arange(T * TOP_K) - (jnp.cumsum(counts) - counts)[s_exp]
    dest = (pad_end - padded)[s_exp] + rank
    n_blocks = -(-(T * TOP_K) // MOE_BLOCK) + N_EXPERTS
    block_expert = jnp.minimum(
        jnp.searchsorted(pad_end, jnp.arange(n_blocks) * MOE_BLOCK, side='right'), N_EXPERTS - 1)
    buf = jnp.zeros((n_blocks * MOE_BLOCK, D), h.dtype).at[dest].set(t[s_tok])

    def expert_block(args):
        xb, e = args
        return swiglu(xb, w1[e], w3[e], w2[e])

    y = lax.map(expert_block, (buf.reshape(n_blocks, MOE_BLOCK, D), block_expert))
    y = y.reshape(n_blocks * MOE_BLOCK, D)[dest] * s_gate[:, None]
    return jnp.zeros_like(t).at[s_tok].add(y).reshape(B, N, D)


def setup_inputs(seed: int = 0) -> dict:
    key = jax.random.key(seed)
    ks = iter(jax.random.split(key, 40))
    f32 = jnp.float32
    L = DEPTH

    def nrm(shape, fan_in, scale=1.0):
        return jax.random.normal(next(ks), shape, f32) * (scale * fan_in ** -0.5)

    def gain(shape):
        return 1.0 + 0.05 * jax.random.normal(next(ks), shape, f32)

    def small(shape):
        return 0.02 * jax.random.normal(next(ks), shape, f32)

    x = jax.random.normal(next(ks), (BATCH, SEQ, D_MODEL), f32)
    c = jax.random.normal(next(ks), (BATCH, D_MODEL), f32)
    ctx = jax.random.normal(next(ks), (BATCH, CTX_LEN, D_MODEL), f32)
    c_ctx = jax.random.normal(next(ks), (D_MODEL,), f32)
    norm_mix = gain((L, D_MODEL))
    norm_ffn = gain((L, D_MODEL))
    w_ada = nrm((L, D_MODEL, 6 * D_MODEL), D_MODEL, 0.2)
    b_ada = small((L, 6 * D_MODEL))
    w_in = nrm((L, D_MODEL, IN_COLS), D_MODEL)
    q_norm = gain((L, Q_LORA))
    w_uq = nrm((L, Q_LORA, N_HEADS * (NOPE_DIM + ROPE_DIM)), Q_LORA)
    kv_norm = gain((L, KV_LORA))
    w_ukv = nrm((L, KV_LORA, N_HEADS * (NOPE_DIM + V_DIM)), KV_LORA)
    conv_w = nrm((L, CONV_K, CONV_DIM), CONV_K)
    conv_b = small((L, CONV_DIM))
    a_log = jnp.log(jax.random.uniform(next(ks), (L, 2, SSM_HEADS), f32, 1.0, 16.0))
    dt0 = jnp.exp(jax.random.uniform(next(ks), (L, 2, SSM_HEADS), f32, math.log(1e-3), math.log(1e-1)))
    dt_bias = dt0 + jnp.log(-jnp.expm1(-dt0))
    d_skip = 1.0 + 0.1 * jax.random.normal(next(ks), (L, 2, SSM_HEADS), f32)
    ssm_norm = gain((L, D_INNER))
    w_oa = nrm((L, N_HEADS * V_DIM, D_MODEL), N_HEADS * V_DIM)
    w_ob = nrm((L, D_INNER, D_MODEL), D_INNER)
    w_out = nrm((L, D_MODEL, D_MODEL), D_MODEL)
    w1_dense = nrm((N_DENSE, D_MODEL, D_FF), D_MODEL)
    w3_dense = nrm((N_DENSE, D_MODEL, D_FF), D_MODEL)
    w2_dense = nrm((N_DENSE, D_FF, D_MODEL), D_FF)
    w_router = nrm((N_MOE, D_MODEL, N_EXPERTS), D_MODEL)
    w1_moe = nrm((N_MOE, N_EXPERTS, D_MODEL, D_FF_EXPERT), D_MODEL)
    w3_moe = nrm((N_MOE, N_EXPERTS, D_MODEL, D_FF_EXPERT), D_MODEL)
    w2_moe = nrm((N_MOE, N_EXPERTS, D_FF_EXPERT, D_MODEL), D_FF_EXPERT)
    final_norm = gain((D_MODEL,))
    return {'x': x, 'c': c, 'ctx': ctx, 'c_ctx': c_ctx, 'norm_mix': norm_mix, 'norm_ffn': norm_ffn,
            'w_ada': w_ada, 'b_ada': b_ada, 'w_in': w_in, 'q_norm': q_norm, 'w_uq': w_uq,
            'kv_norm': kv_norm, 'w_ukv': w_ukv, 'conv_w': conv_w, 'conv_b': conv_b, 'a_log': a_log,
            'dt_bias': dt_bias, 'd_skip': d_skip, 'ssm_norm': ssm_norm, 'w_oa': w_oa, 'w_ob': w_ob,
            'w_out': w_out, 'w1_dense': w1_dense, 'w3_dense': w3_dense, 'w2_dense': w2_dense,
            'w_router': w_router, 'w1_moe': w1_moe, 'w3_moe': w3_moe, 'w2_moe': w2_moe,
            'final_norm': final_norm}


def reference(x, c, ctx, c_ctx, norm_mix, norm_ffn, w_ada, b_ada, w_in, q_norm, w_uq, kv_norm, w_ukv,
              conv_w, conv_b, a_log, dt_bias, d_skip, ssm_norm, w_oa, w_ob, w_out, w1_dense, w3_dense,
              w2_dense, w_router, w1_moe, w3_moe, w2_moe, final_norm):
    cos, sin = axial_rope_tables(x.shape[1])
    for l in range(DEPTH):
        last = l == DEPTH - 1
        m_lat = [t[:, None] for t in jnp.split(jax.nn.silu(c) @ w_ada[l] + b_ada[l], 6, axis=-1)]
        m_ctx = jnp.split(jax.nn.silu(c_ctx) @ w_ada[l] + b_ada[l], 6, axis=-1)
        h_l = modulate(rms_norm(x, norm_mix[l]), m_lat[0], m_lat[1])
        h_c = modulate(rms_norm(ctx, norm_mix[l]), m_ctx[0], m_ctx[1])
        y_c, y_l = token_mixer(h_c, h_l, cos, sin, w_in[l], q_norm[l], w_uq[l], kv_norm[l], w_ukv[l],
                               conv_w[l], conv_b[l], a_log[l], dt_bias[l], d_skip[l], ssm_norm[l],
                               w_oa[l], w_ob[l], w_out[l], not last)
        x = x + m_lat[2] * y_l
        h = modulate(rms_norm(x, norm_ffn[l]), m_lat[3], m_lat[4])
        if not last:
            ctx = ctx + m_ctx[2] * y_c
            h_c = modulate(rms_norm(ctx, norm_ffn[l]), m_ctx[3], m_ctx[4])
            h = jnp.concatenate([h_c, h], axis=1)
        if l % 2 == 0:
            f = swiglu(h, w1_dense[l // 2], w3_dense[l // 2], w2_dense[l // 2])
        else:
            f = moe_swiglu(h, w_router[l // 2], w1_moe[l // 2], w3_moe[l // 2], w2_moe[l // 2])
        if not last:
            ctx = ctx + m_ctx[5] * f[:, :CTX_LEN]
            f = f[:, CTX_LEN:]
        x = x + m_lat[5] * f
    return rms_norm(x, final_norm)
```

```python
import numpy as np
from contextlib import ExitStack
import concourse.bass as bass
import concourse.mybir as mybir
from concourse.bass_utils import run_bass_kernel_spmd

F32 = mybir.dt.float32
BF16 = mybir.dt.bfloat16
AF = mybir.ActivationFunctionType
ALU = mybir.AluOpType
AX = mybir.AxisListType

SAME_ENGINE_SYNC = True
NDSEM = 24


class Trk:
    __slots__ = ("lw", "rd", "name")

    def __init__(self, name=""):
        self.lw = None
        self.rd = []
        self.name = name


class T:
    __slots__ = ("ap", "trk")

    def __init__(self, ap, trk=None, name=""):
        self.ap = ap
        self.trk = trk if trk is not None else Trk(name)

    def __getitem__(self, k):
        return self.ap[k]


class Op:
    __slots__ = ("eng", "fn", "deps", "dma", "ordinal", "sig", "sigidx", "dsem", "dval", "waits", "prewait")

    def __init__(self, eng, fn, deps, dma):
        self.eng = eng
        self.fn = fn
        self.deps = deps
        self.dma = dma
        self.sig = False
        self.sigidx = 0
        self.dsem = None
        self.dval = 0
        self.waits = []
        self.prewait = None


class Prog:
    ENGS = ("pe", "act", "dve", "pool", "sp")

    def __init__(self, nc):
        self.nc = nc
        self.ops = []
        self.es = ExitStack()
        self._n = 0
        self.bar_deps = set()
        self.bar_start = 0

    def sbuf(self, shape, dtype, name=None):
        self._n += 1
        name = f"sb{self._n}_" + (name or "")
        h = self.es.enter_context(self.nc.sbuf_tensor(name, list(shape), dtype))
        return h

    def psum(self, shape, dtype=F32, name=None):
        self._n += 1
        name = f"ps{self._n}_" + (name or "")
        h = self.es.enter_context(self.nc.psum_tensor(name, list(shape), dtype))
        return h

    def tile(self, shape, dtype, name=None):
        h = self.sbuf(shape, dtype, name)
        return T(h[:], name=name or "")

    def ptile(self, shape, dtype=F32, name=None):
        h = self.psum(shape, dtype, name)
        return T(h[:], name=name or "")

    def op(self, eng, fn, reads=(), writes=(), dma=False):
        i = len(self.ops)
        deps = set()
        for t in reads:
            k = t.trk if isinstance(t, T) else t
            if k.lw is not None:
                deps.add(k.lw)
        for t in writes:
            k = t.trk if isinstance(t, T) else t
            if k.lw is not None:
                deps.add(k.lw)
            deps.update(k.rd)
        for t in reads:
            k = t.trk if isinstance(t, T) else t
            k.rd.append(i)
        for t in writes:
            k = t.trk if isinstance(t, T) else t
            k.lw = i
            k.rd = []
        deps.discard(i)
        deps.update(self.bar_deps)
        self.ops.append(Op(eng, fn, deps, dma))
        return i

    def dma(self, q, out, in_, reads=(), writes=()):
        return self.op(q, lambda e: e.dma_start(out=out, in_=in_), reads, writes, dma=True)

    def mm(self, out, lhsT, rhs, start, stop, reads=(), writes=()):
        return self.op("pe", lambda e: e.matmul(out, lhsT, rhs, start=start, stop=stop), reads, writes)

    def transpose(self, out, in_, ident, reads=(), writes=()):
        return self.op("pe", lambda e: e.transpose(out, in_, ident), reads, writes)

    def act(self, out, in_, func, reads=(), writes=(), **kw):
        return self.op("act", lambda e: e.activation(out, in_, func, **kw), reads, writes)

    def dve(self, fn, reads=(), writes=()):
        return self.op("dve", fn, reads, writes)

    def scope(self):
        prog = self
        class _S:
            def __enter__(s_):
                s_.saved = prog.es
                prog.es = ExitStack()
                return s_
            def __exit__(s_, *a):
                prog.barrier()
                prog.es.close()
                prog.es = s_.saved
                return False
        return _S()

    def barrier(self):
        last = {}
        used = set()
        for i in range(self.bar_start, len(self.ops)):
            o = self.ops[i]
            used.update(o.deps)
        for i in range(self.bar_start, len(self.ops)):
            o = self.ops[i]
            if o.dma:
                if i not in used:
                    last[("dma", i)] = i
            else:
                last[o.eng] = i
        deps = set(last.values()) | set(self.bar_deps)
        self.bar_deps = deps
        self.bar_start = len(self.ops)

    def wait_all(self, eng, ids):
        o = Op(eng, None, set(ids), False)
        self.ops.append(o)
        return len(self.ops) - 1

    def emit(self):
        nc = self.nc
        ops = self.ops
        per = {e: [] for e in self.ENGS}
        for i, o in enumerate(ops):
            o.ordinal = len(per[o.eng])
            per[o.eng].append(i)
        dsems = {}
        dcount = {e: 0 for e in self.ENGS}
        for e in self.ENGS:
            if any(ops[i].dma for i in per[e]):
                dsems[e] = [self.es.enter_context(nc.semaphore(f"d_{e}_{k}")) for k in range(NDSEM)]
        esem = {e: self.es.enter_context(nc.semaphore(f"s_{e}")) for e in self.ENGS}
        known = {e: {s: 0 for s in self.ENGS} for e in self.ENGS}
        known_dma = {e: set() for e in self.ENGS}
        for i, o in enumerate(ops):
            eb = o.eng
            kn = known[eb]
            if o.dma:
                n = dcount[eb]
                dcount[eb] += 1
                o.dsem = dsems[eb][n % NDSEM]
                o.dval = 16 * (n // NDSEM + 1)
                if n >= NDSEM:
                    o.prewait = (o.dsem, 16 * (n // NDSEM))
            need = {}
            for d in o.deps:
                a = ops[d]
                if a.dma:
                    if d not in known_dma[eb]:
                        known_dma[eb].add(d)
                        o.waits.append(("dma", d))
                else:
                    ea = a.eng
                    if ea == eb and (ea == "pe" or not SAME_ENGINE_SYNC):
                        continue
                    if a.ordinal + 1 > kn[ea]:
                        need[ea] = max(need.get(ea, 0), a.ordinal + 1)
            for ea, v in need.items():
                kn[ea] = v
                src = per[ea][v - 1]
                ops[src].sig = True
                o.waits.append(("eng", src))
        for e in self.ENGS:
            c = 0
            for i in per[e]:
                if ops[i].sig:
                    c += 1
                    ops[i].sigidx = c
        self.nsig = {e: sum(1 for i in per[e] if ops[i].sig) for e in self.ENGS}
        self.nops = {e: len(per[e]) for e in self.ENGS}

        def run(eng_name):
            def body(e):
                for i in per[eng_name]:
                    o = ops[i]
                    if o.prewait is not None:
                        e.wait_ge(o.prewait[0], o.prewait[1])
                    for kind, d in o.waits:
                        a = ops[d]
                        if kind == "dma":
                            e.wait_ge(a.dsem, a.dval)
                        else:
                            e.wait_ge(esem[a.eng], a.sigidx)
                    if o.fn is None:
                        continue
                    ins = o.fn(e)
                    if o.dma:
                        ins.then_inc(o.dsem, 16)
                    elif o.sig:
                        ins.then_inc(esem[eng_name], 1)
            return body

        with nc.Block() as block:
            block.tensor(run("pe"))
            block.scalar(run("act"))
            block.vector(run("dve"))
            block.gpsimd(run("pool"))
            block.sync(run("sp"))

    def close(self):
        self.es.close()
D = 2048
KC = 16
EPS = 1e-6


class TA:
    def __init__(self, P, nk, blocks, dtype, name):
        self.blocks = blocks
        self.nt = blocks[-1][0] + blocks[-1][1]
        self.nk = nk
        h = P.sbuf([128, nk, self.nt], dtype, name)
        self.ap = h[:]
        self.trk = [[Trk(f"{name}{k}_{b}") for b in range(len(blocks))] for k in range(nk)]

    def all(self):
        return [t for row in self.trk for t in row]

    def col(self, b):
        return [self.trk[k][b] for k in range(self.nk)]


def rr(lst, st):
    i = st[0] % len(lst)
    st[0] += 1
    return lst[i]


def emit_adaln(P, cT_d, wada_d, bada_d, mod):
    with P.scope():
        cT = P.tile([128, 16, 2], F32, "cT")
        sc = P.tile([128, 16, 2], F32, "scT")
        bt = P.tile([128, 96], F32, "badaT")
        P.dma("sp", cT.ap, cT_d, writes=[cT])
        P.dma("sp", bt.ap, bada_d, writes=[bt])
        P.act(sc.ap, cT.ap, AF.Silu, reads=[cT], writes=[sc])
        wb = [P.tile([128, 16, 512], F32, f"wada{i}") for i in range(2)]
        pm = [P.ptile([128, 512], F32, f"pm{i}") for i in range(2)]
        wv = wada_d.rearrange("(k p) n -> p k n", p=128)
        for cb in range(24):
            w = wb[cb % 2]
            P.dma("sp", w.ap, wv[:, :, cb * 512:(cb + 1) * 512], writes=[w])
            ps = pm[cb % 2]
            for j in range(4):
                for k in range(16):
                    P.mm(ps.ap[:, j * 2:(j + 1) * 2], w.ap[:, k, j * 128:(j + 1) * 128], sc.ap[:, k, :], k == 0, k == 15,
                         reads=[w, sc], writes=[ps])
            P.dve(lambda e, ps=ps, cb=cb: e.tensor_tensor(mod.ap[:, cb * 4:(cb + 1) * 4, :], ps.ap[:, 0:8].rearrange("p (j s) -> p j s", s=2),
                                                           bt.ap[:, cb * 4:(cb + 1) * 4].unsqueeze(2).to_broadcast([128, 4, 2]), ALU.add),
                  reads=[ps, bt], writes=[mod])


def emit_gsc(P, gsc, mod, gn_d, scale_j):
    gn = P.tile([128, 16], F32, "gn")
    P.dma("sp", gn.ap, gn_d, writes=[gn])
    P.dve(lambda e: e.tensor_scalar(gsc.ap, mod.ap[:, scale_j * 16:(scale_j + 1) * 16, :], 1.0, None, ALU.add), reads=[mod], writes=[gsc])
    P.dve(lambda e: e.tensor_tensor(gsc.ap, gsc.ap, gn.ap.unsqueeze(2).to_broadcast([128, 16, 2]), ALU.mult), reads=[gsc, gn], writes=[gsc])


def emit_norm_mod(P, xT, hT, sel, gsc, mod, shift_j, ones, pss, router=None):
    sq = [P.tile([128, 512], BF16, f"sq{i}") for i in range(2)]
    rs = [P.tile([128, 512], F32, f"rs{i}") for i in range(2)]
    tmp = [P.tile([128, 512], F32, f"nt{i}") for i in range(3)]
    epst = P.tile([128, 1], F32, "epst")
    P.dve(lambda e: e.memset(epst.ap, EPS), writes=[epst])
    n = [0]
    m = [0]
    for bi, (t0, nt) in enumerate(xT.blocks):
        s = sel[bi]
        ps = pss[bi % len(pss)]
        for k in range(16):
            q = rr(sq, n)
            P.act(q.ap[:, :nt], xT.ap[:, k, t0:t0 + nt], AF.Square, reads=[xT.trk[k][bi]], writes=[q])
            P.mm(ps.ap[:, :nt], ones.ap, q.ap[:, :nt], k == 0, k == 15, reads=[ones, q], writes=[ps])
        r = rs[bi % 2]
        P.act(r.ap[:, :nt], ps.ap[:, :nt], AF.Sqrt, reads=[ps, epst], writes=[r], bias=epst.ap, scale=1.0 / D)
        P.dve(lambda e, r=r, nt=nt: e.reciprocal(r.ap[:, :nt], r.ap[:, :nt]), reads=[r], writes=[r])
        for k in range(16):
            tt = rr(tmp, m)
            P.dve(lambda e, tt=tt, k=k, r=r, t0=t0, nt=nt: e.tensor_tensor(tt.ap[:, :nt], xT.ap[:, k, t0:t0 + nt], r.ap[:, :nt], ALU.mult),
                  reads=[xT.trk[k][bi], r], writes=[tt])
            if gsc is None:
                P.dve(lambda e, tt=tt, k=k, t0=t0, nt=nt: e.tensor_scalar(hT.ap[:, k, t0:t0 + nt], tt.ap[:, :nt], mod.ap[:, k:k + 1], None, ALU.mult),
                      reads=[tt, mod], writes=[hT.trk[k][bi]])
            elif router is None:
                P.dve(lambda e, tt=tt, k=k, t0=t0, nt=nt, s=s: e.tensor_scalar(hT.ap[:, k, t0:t0 + nt], tt.ap[:, :nt], gsc.ap[:, k, s:s + 1],
                                                                          mod.ap[:, shift_j * 16 + k, s:s + 1], ALU.mult, ALU.add),
                      reads=[tt, gsc, mod], writes=[hT.trk[k][bi]])
            else:
                wr, psr, logT = router
                P.dve(lambda e, tt=tt, k=k, t0=t0, nt=nt, s=s: e.tensor_scalar(tt.ap[:, :nt], tt.ap[:, :nt], gsc.ap[:, k, s:s + 1],
                                                                          mod.ap[:, shift_j * 16 + k, s:s + 1], ALU.mult, ALU.add),
                      reads=[tt, gsc, mod], writes=[tt])
                P.act(hT.ap[:, k, t0:t0 + nt], tt.ap[:, :nt], AF.Copy, reads=[tt], writes=[hT.trk[k][bi]])
                P.mm(psr.ap[:8, :nt], wr.ap[:, k, :], tt.ap[:, :nt], k == 0, k == 15, reads=[wr, tt], writes=[psr])
        if router is not None:
            wr, psr, logT = router
            P.act(logT.ap[:, t0:t0 + nt], psr.ap[:8, :nt], AF.Copy, reads=[psr], writes=[logT])


class WStream:
    def __init__(self, P, nk, mcols, nbuf, name, q="pool"):
        self.P = P
        self.bufs = [P.tile([128, nk, mcols], BF16, f"{name}{i}") for i in range(nbuf)]
        self.n = [0]
        self.nk = nk
        self.q = q

    def load(self, wd, c0, mc):
        t = rr(self.bufs, self.n)
        self.P.dma(self.q, t.ap[:, :, :mc], wd.rearrange("(k p) n -> p k n", p=128)[:, :, c0:c0 + mc], writes=[t])
        return t


def gemm_fm(P, ws, wd, M, rhs, pss, pst, evac, mcols=None):
    mcols = mcols or ws.bufs[0].ap.shape[2]
    nk = rhs.nk
    for c0 in range(0, M, mcols):
        mc = min(mcols, M - c0)
        wt = ws.load(wd, c0, mc)
        for mi in range(mc // 128):
            for bi, (t0, nt) in enumerate(rhs.blocks):
                ps = rr(pss, pst)
                for k in range(nk):
                    P.mm(ps.ap[:, :nt], wt.ap[:, k, mi * 128:(mi + 1) * 128], rhs.ap[:, k, t0:t0 + nt], k == 0, k == nk - 1,
                         reads=[wt, rhs.trk[k][bi]], writes=[ps])
                evac((c0 // 128) + mi, bi, t0, nt, ps)
DFF = 5632
DFFE = 7168
NEXP = 8


def emit_ffn(P, xT, h2T, sel, mod, w1d, w3d, w2d, FF, ws1, ws3, ws2, gTs, psAB, psO, sa_t, tg_t, st, gbc=None):
    for g in range(FF // 256):
        w1t = ws1.load(w1d, g * 256, 256)
        w3t = ws3.load(w3d, g * 256, 256)
        w2t = rr(ws2.bufs, ws2.n)
        P.dma(ws2.q, w2t.ap, w2d[g * 256:(g + 1) * 256, :].rearrange("(j p) n -> p j n", p=128), writes=[w2t])
        gT = rr(gTs, st["g"])
        for bi, (t0, nt) in enumerate(h2T.blocks):
            s = sel[bi]
            for j in range(2):
                pa = rr(psAB, st["ab"])
                for k in range(16):
                    P.mm(pa.ap[:, :nt], w1t.ap[:, k, j * 128:(j + 1) * 128], h2T.ap[:, k, t0:t0 + nt], k == 0, k == 15,
                         reads=[w1t, h2T.trk[k][bi]], writes=[pa])
                pb = rr(psAB, st["ab"])
                for k in range(16):
                    P.mm(pb.ap[:, :nt], w3t.ap[:, k, j * 128:(j + 1) * 128], h2T.ap[:, k, t0:t0 + nt], k == 0, k == 15,
                         reads=[w3t, h2T.trk[k][bi]], writes=[pb])
                sa = rr(sa_t, st["sa"])
                P.act(sa.ap[:, :nt], pa.ap[:, :nt], AF.Silu, reads=[pa], writes=[sa])
                if gbc is None:
                    P.dve(lambda e, sa=sa, pb=pb, gT=gT, j=j, t0=t0, nt=nt: e.tensor_tensor(gT.ap[:, j, t0:t0 + nt], sa.ap[:, :nt], pb.ap[:, :nt], ALU.mult),
                          reads=[sa, pb], writes=[gT.trk[j][bi]])
                else:
                    tg = rr(tg_t, st["tg"])
                    P.dve(lambda e, tg=tg, pb=pb, t0=t0, nt=nt: e.tensor_tensor(tg.ap[:, :nt], pb.ap[:, :nt], gbc.ap[:, t0:t0 + nt], ALU.mult),
                          reads=[pb, gbc], writes=[tg])
                    P.dve(lambda e, sa=sa, tg=tg, gT=gT, j=j, t0=t0, nt=nt: e.tensor_tensor(gT.ap[:, j, t0:t0 + nt], sa.ap[:, :nt], tg.ap[:, :nt], ALU.mult),
                          reads=[sa, tg], writes=[gT.trk[j][bi]])
            for m in range(16):
                po = rr(psO, st["o"])
                for j in range(2):
                    P.mm(po.ap[:, :nt], w2t.ap[:, j, m * 128:(m + 1) * 128], gT.ap[:, j, t0:t0 + nt], j == 0, j == 1,
                         reads=[w2t, gT.trk[j][bi]], writes=[po])
                P.dve(lambda e, po=po, m=m, t0=t0, nt=nt, s=s: e.scalar_tensor_tensor(xT.ap[:, m, t0:t0 + nt], po.ap[:, :nt], mod.ap[:, 80 + m, s:s + 1],
                                                                                 xT.ap[:, m, t0:t0 + nt], ALU.mult, ALU.add),
                      reads=[po, mod, xT.trk[m][bi]], writes=[xT.trk[m][bi]])


def emit_routing(P, logT, gateT, ident, nt_total, PSr):
    with P.scope():
        pt = PSr
        lg = [P.tile([128, 8], F32, f"rt_lg{i}") for i in range(2)]
        mk1 = [P.tile([128, 8], F32, f"rt_m1{i}") for i in range(2)]
        l2 = [P.tile([128, 8], F32, f"rt_l2{i}") for i in range(2)]
        mk2 = [P.tile([128, 8], F32, f"rt_m2{i}") for i in range(2)]
        sc = [P.tile([128, 8], F32, f"rt_sc{i}") for i in range(2)]
        ga = [P.tile([128, 8], F32, f"rt_ga{i}") for i in range(2)]
        for ti in range(nt_total // 128):
            i = ti % 2
            p = pt[i]
            tsl = slice(ti * 128, (ti + 1) * 128)
            P.transpose(p.ap[:, 0:8], logT.ap[:, tsl], ident.ap[:8, :8], reads=[logT, ident], writes=[p])
            P.dve(lambda e, i=i, p=p: e.tensor_copy(lg[i].ap, p.ap[:, 0:8]), reads=[p], writes=[lg[i]])
            P.dve(lambda e, i=i: e.reduce_max(sc[i].ap[:, 0:1], lg[i].ap, AX.X), reads=[lg[i]], writes=[sc[i]])
            P.dve(lambda e, i=i: e.tensor_scalar(mk1[i].ap, lg[i].ap, sc[i].ap[:, 0:1], None, ALU.is_equal), reads=[lg[i], sc[i]], writes=[mk1[i]])
            P.dve(lambda e, i=i: e.scalar_tensor_tensor(l2[i].ap, mk1[i].ap, -1e30, lg[i].ap, ALU.mult, ALU.add), reads=[mk1[i], lg[i]], writes=[l2[i]])
            P.dve(lambda e, i=i: e.reduce_max(sc[i].ap[:, 1:2], l2[i].ap, AX.X), reads=[l2[i]], writes=[sc[i]])
            P.dve(lambda e, i=i: e.tensor_scalar(mk2[i].ap, l2[i].ap, sc[i].ap[:, 1:2], None, ALU.is_equal), reads=[l2[i], sc[i]], writes=[mk2[i]])
            P.dve(lambda e, i=i: e.tensor_tensor(sc[i].ap[:, 2:3], sc[i].ap[:, 0:1], sc[i].ap[:, 1:2], ALU.subtract), reads=[sc[i]], writes=[sc[i]])
            P.act(sc[i].ap[:, 3:4], sc[i].ap[:, 2:3], AF.Sigmoid, reads=[sc[i]], writes=[sc[i]])
            P.act(sc[i].ap[:, 4:5], sc[i].ap[:, 2:3], AF.Sigmoid, reads=[sc[i]], writes=[sc[i]], scale=-1.0)
            P.dve(lambda e, i=i: e.tensor_scalar(ga[i].ap, mk1[i].ap, sc[i].ap[:, 3:4], None, ALU.mult), reads=[mk1[i], sc[i]], writes=[ga[i]])
            P.dve(lambda e, i=i: e.scalar_tensor_tensor(ga[i].ap, mk2[i].ap, sc[i].ap[:, 4:5], ga[i].ap, ALU.mult, ALU.add),
                  reads=[mk2[i], sc[i], ga[i]], writes=[ga[i]])
            P.mm(p.ap[:8, 128:256], ga[i].ap, ident.ap, True, True, reads=[ga[i], ident], writes=[p])
            P.act(gateT.ap[:, tsl], p.ap[:8, 128:256], AF.Copy, reads=[p], writes=[gateT])


def build_kc(layer, NT):
    moe = layer == 1
    nc = bass.Bass("TRN2", target_bir_lowering=False)
    dt = lambda name, shape, dtype=F32, kind="ExternalInput": nc.dram_tensor(name, list(shape), dtype, kind=kind).ap()
    xT_d = dt("xT", [D, NT]); hT_d = dt("hT", [D, NT], BF16); attT_d = dt("attT", [D, NT], BF16); uT_d = dt("uT", [2 * D, NT], BF16)
    mod_d = dt("mod", [128, 96, 2]); gn_d = dt("gn", [128, 16]); ident_d = dt("ident", [128, 128])
    wg_d = dt("wg", [D, 2 * D]); woa_d = dt("woa", [D, D]); wob_d = dt("wob", [2 * D, D]); wout_d = dt("wout", [D, D])
    if moe:
        wr_d = dt("wr", [128, 16, 8])
        h2_d = dt("h2T", [D, NT], BF16, "ExternalOutput"); gate_d = dt("gateT", [8, NT], F32, "ExternalOutput")
    else:
        w1_d = dt("w1", [D, DFF]); w3_d = dt("w3", [D, DFF]); w2_d = dt("w2", [DFF, D])
    out_d = dt("xoutT", [D, NT], F32, "ExternalOutput")
    P = Prog(nc)
    blocks = [(0, 512), (512, 512)] + ([(1024, 64)] if NT > 1024 else [])
    sel = [0, 0, 1]
    view = lambda d: d.rearrange("(k p) t -> p k t", p=128)

    def load_ta(ta, d):
        v = view(d)
        for bi, (t0, nt) in enumerate(ta.blocks):
            P.dma("sp", ta.ap[:, :, t0:t0 + nt], v[:, :, t0:t0 + nt], writes=ta.col(bi))

    mod = P.tile([128, 96, 2], F32, "mod")
    P.dma("sp", mod.ap, mod_d, writes=[mod])
    ones = P.tile([128, 128], BF16, "ones")
    P.dve(lambda e: e.memset(ones.ap, 1.0), writes=[ones])
    ident = P.tile([128, 128], F32, "ident")
    P.dma("sp", ident.ap, ident_d, writes=[ident])
    mrg = TA(P, 16, blocks, BF16, "mrg")
    PS = [P.ptile([128, 512], F32, f"PS{i}") for i in range(8)]
    if True:
        pss = PS[0:4]
        pst = [0]
        with P.scope():
            hT = TA(P, 16, blocks, BF16, "hT")
            load_ta(hT, hT_d)
            ws16 = WStream(P, 16, 256, 2, "ws16")
            with P.scope():
                uT = TA(P, 32, blocks, BF16, "uT")
                load_ta(uT, uT_d)
                ws32 = WStream(P, 32, 256, 2, "ws32")
                def ev_sgb(m, bi, t0, nt, ps):
                    P.act(mrg.ap[:, m, t0:t0 + nt], ps.ap[:, :nt], AF.Sigmoid, reads=[ps], writes=[mrg.trk[m][bi]])
                gemm_fm(P, ws16, wg_d[:, D:2 * D], D, hT, pss, pst, ev_sgb)
                def ev_ob(m, bi, t0, nt, ps):
                    P.dve(lambda e: e.tensor_tensor(mrg.ap[:, m, t0:t0 + nt], ps.ap[:, :nt], mrg.ap[:, m, t0:t0 + nt], ALU.mult),
                          reads=[ps, mrg.trk[m][bi]], writes=[mrg.trk[m][bi]])
                gemm_fm(P, ws32, wob_d, D, uT, pss, pst, ev_ob)
            with P.scope():
                attT = TA(P, 16, blocks, BF16, "attT")
                load_ta(attT, attT_d)
                sga = TA(P, 16, blocks, BF16, "sga")
                tmpa = [P.tile([128, 512], F32, f"tmpa{i}") for i in range(2)]
                tn = [0]
                def ev_sga(m, bi, t0, nt, ps):
                    P.act(sga.ap[:, m, t0:t0 + nt], ps.ap[:, :nt], AF.Sigmoid, reads=[ps], writes=[sga.trk[m][bi]])
                gemm_fm(P, ws16, wg_d[:, 0:D], D, hT, pss, pst, ev_sga)
                def ev_oa(m, bi, t0, nt, ps):
                    tt = rr(tmpa, tn)
                    P.dve(lambda e: e.tensor_tensor(tt.ap[:, :nt], ps.ap[:, :nt], sga.ap[:, m, t0:t0 + nt], ALU.mult),
                          reads=[ps, sga.trk[m][bi]], writes=[tt])
                    P.dve(lambda e: e.tensor_tensor(mrg.ap[:, m, t0:t0 + nt], tt.ap[:, :nt], mrg.ap[:, m, t0:t0 + nt], ALU.add),
                          reads=[tt, mrg.trk[m][bi]], writes=[mrg.trk[m][bi]])
                gemm_fm(P, ws16, woa_d, D, attT, pss, pst, ev_oa)
        xT = TA(P, 16, blocks, F32, "xT")
        load_ta(xT, xT_d)
        ws16b = WStream(P, 16, 256, 2, "ws16b")
        def ev_out(m, bi, t0, nt, ps):
            s = sel[bi]
            P.dve(lambda e: e.scalar_tensor_tensor(xT.ap[:, m, t0:t0 + nt], ps.ap[:, :nt], mod.ap[:, 32 + m, s:s + 1], xT.ap[:, m, t0:t0 + nt],
                                                   ALU.mult, ALU.add), reads=[ps, mod, xT.trk[m][bi]], writes=[xT.trk[m][bi]])
        gemm_fm(P, ws16b, wout_d, D, mrg, pss, pst, ev_out)
    h2T = mrg
    gsc = P.tile([128, 16, 2], F32, "gsc")
    router = None
    if moe:
        logT = P.tile([8, NT], F32, "logT")
        gateT = P.tile([8, NT], F32, "gateT")
    with P.scope():
        emit_gsc(P, gsc, mod, gn_d, 4)
        npss = PS[4:6]
        if moe:
            wr = P.tile([128, 16, 8], F32, "wr")
            P.dma("sp", wr.ap, wr_d, writes=[wr])
            psr = PS[6]
            router = (wr, psr, logT)
        emit_norm_mod(P, xT, h2T, sel, gsc, mod, 3, ones, npss, router)
    if moe:
        emit_routing(P, logT, gateT, ident, NT, PS[0:2])
    outs = []
    ov = view(out_d)
    if moe:
        hv2 = view(h2_d)
        for bi, (t0, nt) in enumerate(blocks):
            outs.append(P.dma("sp", hv2[:, :, t0:t0 + nt], h2T.ap[:, :, t0:t0 + nt], reads=h2T.col(bi)))
        outs.append(P.dma("sp", gate_d, gateT.ap, reads=[gateT]))
    else:
        with P.scope():
            ws1 = WStream(P, 16, 256, 2, "w1s")
            ws3 = WStream(P, 16, 256, 2, "w3s")
            ws2 = WStream(P, 2, 2048, 2, "w2s")
            gTs = [TA(P, 2, blocks, BF16, f"gT{i}") for i in range(2)]
            sa_t = [P.tile([128, 512], F32, f"sa{i}") for i in range(2)]
            tg_t = [P.tile([128, 512], F32, f"tg{i}") for i in range(2)]
            st = {k: [0] for k in ("g", "ab", "sa", "tg", "o")}
            emit_ffn(P, xT, h2T, sel, mod, w1_d, w3_d, w2_d, DFF, ws1, ws3, ws2, gTs, PS[0:4], PS[4:7], sa_t, tg_t, st)
    for bi, (t0, nt) in enumerate(blocks):
        outs.append(P.dma("sp", ov[:, :, t0:t0 + nt], xT.ap[:, :, t0:t0 + nt], reads=xT.col(bi)))
    P.wait_all("sp", outs)
    P.emit()
    return nc, P


def build_ke():
    nc = bass.Bass("TRN2", target_bir_lowering=False)
    dt = lambda name, shape, dtype=F32, kind="ExternalInput": nc.dram_tensor(name, list(shape), dtype, kind=kind).ap()
    h2_d = dt("h2T", [D, 8192], BF16); gbc_d = dt("gbc", [128, 8192]); mod_d = dt("mod", [128, 96, 2])
    w1_d = dt("w1", [D, DFFE]); w3_d = dt("w3", [D, DFFE]); w2_d = dt("w2", [DFFE, D])
    part_d = dt("part", [D, 8192], F32, "ExternalOutput")
    P = Prog(nc)
    PS = [P.ptile([128, 512], F32, f"PS{i}") for i in range(8)]
    blocks = [(0, 512), (512, 512)]
    mod = P.tile([128, 96, 2], F32, "mod")
    P.dma("sp", mod.ap, mod_d, writes=[mod])
    ws1 = WStream(P, 16, 256, 2, "w1s"); ws3 = WStream(P, 16, 256, 2, "w3s"); ws2 = WStream(P, 2, 2048, 2, "w2s")
    gTs = [TA(P, 2, blocks, BF16, f"gT{i}") for i in range(2)]
    sa_t = [P.tile([128, 512], F32, f"sa{i}") for i in range(2)]
    tg_t = [P.tile([128, 512], F32, f"tg{i}") for i in range(2)]
    st = {k: [0] for k in ("g", "ab", "sa", "tg", "o")}
    xTs = [TA(P, 16, blocks, F32, f"acc{i}") for i in range(1)]
    h2s = [TA(P, 16, blocks, BF16, f"h2_{i}") for i in range(1)]
    gbs = [P.tile([128, 1024], F32, f"gb{i}") for i in range(2)]
    hv = h2_d.rearrange("(k p) t -> p k t", p=128); pv = part_d.rearrange("(k p) t -> p k t", p=128)
    outs = []
    for c in range(8):
        xT = xTs[0]; h2T = h2s[0]; gbc = gbs[c % 2]
        c0 = c * 1024
        for bi, (t0, nt) in enumerate(blocks):
            P.dma("sp", h2T.ap[:, :, t0:t0 + nt], hv[:, :, c0 + t0:c0 + t0 + nt], writes=h2T.col(bi))
            for k in range(16):
                P.dve(lambda e, xT=xT, k=k, t0=t0, nt=nt: e.memset(xT.ap[:, k, t0:t0 + nt], 0.0), writes=[xT.trk[k][bi]])
        P.dma("sp", gbc.ap, gbc_d[:, c0:c0 + 1024], writes=[gbc])
        s = 0 if c < 4 else 1
        emit_ffn(P, xT, h2T, [s, s], mod, w1_d, w3_d, w2_d, DFFE, ws1, ws3, ws2, gTs, PS[0:4], PS[4:7], sa_t, tg_t, st, gbc=gbc)
        for bi, (t0, nt) in enumerate(blocks):
            outs.append(P.dma("sp", pv[:, :, c0 + t0:c0 + t0 + nt], xT.ap[:, :, t0:t0 + nt], reads=xT.col(bi)))
    P.wait_all("sp", outs)
    P.emit()
    return nc, P


def build_kf():
    nc = bass.Bass("TRN2", target_bir_lowering=False)
    dt = lambda name, shape, dtype=F32, kind="ExternalInput": nc.dram_tensor(name, list(shape), dtype, kind=kind).ap()
    xT_d = dt("xT", [D, 1024]); parts_d = dt("parts", [8, D, 1024]); fn_d = dt("fn", [128, 16])
    out_d = dt("outT", [D, 1024], F32, "ExternalOutput")
    P = Prog(nc)
    PS = [P.ptile([128, 512], F32, f"PS{i}") for i in range(2)]
    blocks = [(0, 512), (512, 512)]
    ones = P.tile([128, 128], BF16, "ones")
    P.dve(lambda e: e.memset(ones.ap, 1.0), writes=[ones])
    fn = P.tile([128, 16], F32, "fn")
    P.dma("sp", fn.ap, fn_d, writes=[fn])
    xT = TA(P, 16, blocks, F32, "xT"); oT = TA(P, 16, blocks, F32, "oT")
    xv = xT_d.rearrange("(k p) t -> p k t", p=128); ov = out_d.rearrange("(k p) t -> p k t", p=128)
    for bi, (t0, nt) in enumerate(blocks):
        P.dma("sp", xT.ap[:, :, t0:t0 + nt], xv[:, :, t0:t0 + nt], writes=xT.col(bi))
    pb = [P.tile([128, 8, 512], F32, f"pb{i}") for i in range(2)]
    n = 0
    for ex in range(8):
        pvw = parts_d[ex].rearrange("(k p) t -> p k t", p=128)
        for bi, (t0, nt) in enumerate(blocks):
            for hf in range(2):
                t = pb[n % 2]; n += 1
                P.dma("sp", t.ap, pvw[:, hf * 8:(hf + 1) * 8, t0:t0 + nt], writes=[t])
                for k8 in range(8):
                    k = hf * 8 + k8
                    P.dve(lambda e, t=t, k=k, k8=k8, t0=t0, nt=nt: e.tensor_tensor(xT.ap[:, k, t0:t0 + nt], xT.ap[:, k, t0:t0 + nt], t.ap[:, k8, :], ALU.add),
                          reads=[t, xT.trk[k][bi]], writes=[xT.trk[k][bi]])
    emit_norm_mod(P, xT, oT, [0, 0], None, fn, 0, ones, PS)
    outs = []
    for bi, (t0, nt) in enumerate(blocks):
        outs.append(P.dma("sp", ov[:, :, t0:t0 + nt], oT.ap[:, :, t0:t0 + nt], reads=oT.col(bi)))
    P.wait_all("sp", outs)
    P.emit()
    return nc, P
NTOK = 8704
WS_COLS = 1288


def build_ssd():
    nc = bass.Bass("TRN2", target_bir_lowering=False)
    dt_ = lambda name, shape, dtype=F32, kind="ExternalInput": nc.dram_tensor(name, list(shape), dtype, kind=kind).ap()
    hT_d = dt_("hT", [D, NTOK], BF16)
    w_d = dt_("wssd", [D, WS_COLS])
    cw_d = dt_("convw", [128, 6, 5]); cb_d = dt_("convb", [128, 6])
    dtb_d = dt_("dtb", [128, 8]); alog_d = dt_("alog", [128, 8]); dsk_d = dt_("dsk", [128, 2, 8])
    gain_d = dt_("gain", [128, 512])
    yprev_d = dt_("yprev", [NTOK, 512])
    ident_d = dt_("ident", [128, 128]); mU_d = dt_("mU", [128, 128]); mL_d = dt_("mL", [128, 128]); mF_d = dt_("mF", [128, 128])
    y_d = dt_("y", [NTOK, 512], F32, "ExternalOutput")
    u_d = dt_("u", [NTOK, 512], BF16, "ExternalOutput")
    P = Prog(nc)
    PS = [P.ptile([128, 512], F32, f"PS{i}") for i in range(8)]
    cst = lambda shape, d, name, dtype=F32: (lambda t: (P.dma("sp", t.ap, d, writes=[t]), t)[1])(P.tile(shape, dtype, name))
    ident = cst([128, 128], ident_d, "ident"); mU = cst([128, 128], mU_d, "mU"); mL = cst([128, 128], mL_d, "mL"); mF = cst([128, 128], mF_d, "mF")
    cw = cst([128, 6, 5], cw_d, "cw"); cb = cst([128, 6], cb_d, "cb"); dtb = cst([128, 8], dtb_d, "dtb")
    alog = cst([128, 8], alog_d, "alog"); dsk = cst([128, 2, 8], dsk_d, "dsk"); gain = cst([128, 512], gain_d, "gain")
    onesf = P.tile([128, 128], F32, "onesf")
    P.dve(lambda e: e.memset(onesf.ap, 1.0), writes=[onesf])
    A = P.tile([128, 8], F32, "A")
    P.act(A.ap, alog.ap, AF.Exp, reads=[alog], writes=[A])
    P.dve(lambda e: e.tensor_scalar(A.ap, A.ap, -1.0, None, ALU.mult), reads=[A], writes=[A])
    dsum = P.tile([128, 8], F32, "dsum")
    P.dve(lambda e: e.tensor_tensor(dsum.ap, dsk.ap[:, 0, :], dsk.ap[:, 1, :], ALU.add), reads=[dsk], writes=[dsum])
    epst = P.tile([128, 1], F32, "epst")
    P.dve(lambda e: e.memset(epst.ap, EPS), writes=[epst])
    w = P.tile([128, 16, WS_COLS], BF16, "wssd")
    P.dma("pool", w.ap, w_d.rearrange("(k p) n -> p k n", p=128), writes=[w])
    hb = [P.tile([128, 16, 260], BF16, f"hb{i}") for i in range(2)]
    xsT = [P.tile([128, 4, 256], F32, f"xsT{i}") for i in range(2)]
    BTf = [P.tile([128, 256], F32, f"BTf{i}") for i in range(2)]
    BTb = [P.tile([128, 256], BF16, f"BTb{i}") for i in range(2)]
    CTb = [P.tile([128, 256], BF16, f"CTb{i}") for i in range(2)]
    acc = [P.tile([128, 256], F32, f"acc{i}") for i in range(2)]
    S = P.tile([128, 512], F32, "S"); Sb = P.tile([128, 512], BF16, "Sb")
    mk = lambda shape, dtype, name, n=2: [P.tile(shape, dtype, f"{name}{i}") for i in range(n)]
    zs = mk([128, 512], F32, "zs"); xs_sb = mk([128, 512], F32, "xs_sb"); xc = mk([128, 512], BF16, "xc"); xw = mk([128, 512], BF16, "xw")
    Btok = mk([128, 128], BF16, "Btok"); dta = mk([128, 16], F32, "dta"); ex = mk([128, 24], F32, "ex")
    cbm = mk([128, 128], F32, "cbm"); R = mk([128, 4, 128], F32, "R"); dec = mk([128, 4, 128], F32, "dec"); Mt = mk([128, 8, 128], BF16, "Mt")
    yo = mk([128, 512], F32, "yo"); ysb = mk([128, 512], F32, "ysb"); ypv = mk([128, 512], F32, "ypv"); ug = mk([128, 512], F32, "ug")
    usq = mk([128, 512], F32, "usq"); ss = mk([128, 2], F32, "ss"); uo = mk([128, 512], BF16, "uo"); tdt = mk([128, 8], F32, "tdt")
    hview = hT_d.rearrange("(k p) t -> p k t", p=128)
    outs = []
    ci = 0
    for blk in range(NTOK // 256):
        t0 = blk * 256
        bseq = blk % 17
        left0 = bseq in (0, 1)
        right0 = bseq in (0, 16)
        h = hb[blk % 2]
        lo = t0 - (0 if left0 else 2); hi = t0 + 256 + (0 if right0 else 2)
        if left0:
            P.dve(lambda e, h=h: e.memset(h.ap[:, :, 0:2], 0.0), writes=[h])
        if right0:
            P.dve(lambda e, h=h: e.memset(h.ap[:, :, 258:260], 0.0), writes=[h])
        P.dma("sp", h.ap[:, :, (2 if left0 else 0):(258 if right0 else 260)], hview[:, :, lo:hi], writes=[h])
        if bseq == 0:
            P.dve(lambda e: e.memset(S.ap, 0.0), writes=[S])
            P.dve(lambda e: e.memset(Sb.ap, 0.0), writes=[Sb])
        b2 = blk % 2
        for ch in range(6):
            ps = PS[ch % 2]
            c0 = 512 + ch * 128
            for k in range(16):
                P.mm(ps.ap[:, 0:260], w.ap[:, k, c0:c0 + 128], h.ap[:, k, :], k == 0, k == 15, reads=[w, h], writes=[ps])
            a = acc[ch % 2]
            P.dve(lambda e, a=a, ps=ps, ch=ch: e.tensor_scalar(a.ap, ps.ap[:, 0:256], cw.ap[:, ch, 0:1], cb.ap[:, ch:ch + 1], ALU.mult, ALU.add),
                  reads=[ps, cw, cb], writes=[a])
            for j in range(1, 5):
                P.dve(lambda e, a=a, ps=ps, ch=ch, j=j: e.scalar_tensor_tensor(a.ap, ps.ap[:, j:j + 256], cw.ap[:, ch, j:j + 1], a.ap, ALU.mult, ALU.add),
                      reads=[ps, cw, a], writes=[a])
            if ch < 4:
                P.act(xsT[b2].ap[:, ch, :], a.ap, AF.Silu, reads=[a], writes=[xsT[b2]])
            elif ch == 4:
                P.act(BTf[b2].ap, a.ap, AF.Silu, reads=[a], writes=[BTf[b2]])
                P.dve(lambda e, b2=b2: e.tensor_copy(BTb[b2].ap, BTf[b2].ap), reads=[BTf[b2]], writes=[BTb[b2]])
            else:
                P.act(CTb[b2].ap, a.ap, AF.Silu, reads=[a], writes=[CTb[b2]])
        for c in range(2):
            i = ci % 2
            ci += 1
            cs = slice(c * 128, (c + 1) * 128)
            hs = slice(2 + c * 128, 2 + (c + 1) * 128)
            tok0 = t0 + c * 128
            P.dma("sp", ypv[i].ap, yprev_d[tok0:tok0 + 128, :], writes=[ypv[i]])
            pz = PS[2]
            for k in range(16):
                P.mm(pz.ap, h.ap[:, k, hs], w.ap[:, k, 0:512], k == 0, k == 15, reads=[w, h], writes=[pz])
            P.act(zs[i].ap, pz.ap, AF.Silu, reads=[pz], writes=[zs[i]])
            pd = PS[3]
            for k in range(16):
                P.mm(pd.ap[:, 0:8], h.ap[:, k, hs], w.ap[:, k, 1280:1288], k == 0, k == 15, reads=[w, h], writes=[pd])
            P.dve(lambda e, i=i, pd=pd: e.tensor_tensor(tdt[i].ap, pd.ap[:, 0:8], dtb.ap, ALU.add), reads=[pd, dtb], writes=[tdt[i]])
            P.act(tdt[i].ap, tdt[i].ap, AF.Exp, reads=[tdt[i]], writes=[tdt[i]])
            P.act(dta[i].ap[:, 0:8], tdt[i].ap, AF.Ln, reads=[tdt[i]], writes=[dta[i]], bias=1.0)
            P.dve(lambda e, i=i: e.tensor_tensor(dta[i].ap[:, 8:16], dta[i].ap[:, 0:8], A.ap, ALU.mult), reads=[dta[i], A], writes=[dta[i]])
            pc = PS[3]
            P.mm(pc.ap[:, 16:24], mU.ap, dta[i].ap[:, 8:16], True, True, reads=[mU, dta[i]], writes=[pc])
            P.mm(pc.ap[:, 24:32], mL.ap, dta[i].ap[:, 8:16], True, True, reads=[mL, dta[i]], writes=[pc])
            P.mm(pc.ap[:, 32:40], onesf.ap, dta[i].ap[:, 8:16], True, True, reads=[onesf, dta[i]], writes=[pc])
            P.act(ex[i].ap, pc.ap[:, 16:40], AF.Exp, reads=[pc], writes=[ex[i]])
            px = PS[4]
            for ch in range(4):
                P.transpose(px.ap[:, ch * 128:(ch + 1) * 128], xsT[b2].ap[:, ch, cs], ident.ap, reads=[xsT[b2], ident], writes=[px])
            P.act(xs_sb[i].ap, px.ap, AF.Copy, reads=[px], writes=[xs_sb[i]])
            bc = lambda t8: t8.unsqueeze(2).to_broadcast([128, 8, 64])
            v3 = lambda ap: ap.rearrange("p (e q) -> p e q", q=64)
            P.dve(lambda e, i=i: e.tensor_tensor(v3(xc[i].ap), v3(xs_sb[i].ap), bc(dta[i].ap[:, 0:8]), ALU.mult), reads=[xs_sb[i], dta[i]], writes=[xc[i]])
            P.dve(lambda e, i=i: e.tensor_tensor(v3(xw[i].ap), v3(xc[i].ap), bc(ex[i].ap[:, 0:8]), ALU.mult), reads=[xc[i], ex[i]], writes=[xw[i]])
            pb = PS[5]
            P.transpose(pb.ap[:, 0:128], BTf[b2].ap[:, cs], ident.ap, reads=[BTf[b2], ident], writes=[pb])
            P.act(Btok[i].ap, pb.ap[:, 0:128], AF.Copy, reads=[pb], writes=[Btok[i]])
            P.mm(pb.ap[:, 128:256], BTb[b2].ap[:, cs], CTb[b2].ap[:, cs], True, True, reads=[BTb[b2], CTb[b2]], writes=[pb])
            P.dve(lambda e, i=i, pb=pb: e.tensor_tensor(cbm[i].ap, pb.ap[:, 128:256], mF.ap, ALU.mult), reads=[pb, mF], writes=[cbm[i]])
            for hh in range(2):
                r = R[hh]; d = dec[hh]
                P.dve(lambda e, r=r, i=i, hh=hh: e.tensor_tensor(r.ap, mL.ap.unsqueeze(1).to_broadcast([128, 4, 128]),
                                                                 dta[i].ap[:, 8 + hh * 4:12 + hh * 4].unsqueeze(2).to_broadcast([128, 4, 128]), ALU.mult),
                      reads=[mL, dta[i]], writes=[r])
                pg = PS[6 + hh]
                P.mm(pg.ap, mU.ap, r.ap.rearrange("p e l -> p (e l)"), True, True, reads=[mU, r], writes=[pg])
                P.act(d.ap.rearrange("p e l -> p (e l)"), pg.ap, AF.Exp, reads=[pg], writes=[d])
                P.dve(lambda e, d=d, i=i, hh=hh: e.tensor_tensor(Mt[i].ap[:, hh * 4:(hh + 1) * 4, :], d.ap, cbm[i].ap.unsqueeze(1).to_broadcast([128, 4, 128]), ALU.mult),
                      reads=[d, cbm[i]], writes=[Mt[i]])
            py = PS[0]
            for e8 in range(8):
                P.mm(py.ap[:, e8 * 64:(e8 + 1) * 64], Mt[i].ap[:, e8, :], xc[i].ap[:, e8 * 64:(e8 + 1) * 64], True, True, reads=[Mt[i], xc[i]], writes=[py])
            po = PS[1]
            P.mm(po.ap, CTb[b2].ap[:, cs], Sb.ap, True, True, reads=[CTb[b2], Sb], writes=[po])
            P.dve(lambda e, i=i, po=po: e.tensor_tensor(v3(yo[i].ap), v3(po.ap), bc(ex[i].ap[:, 8:16]), ALU.mult), reads=[po, ex[i]], writes=[yo[i]])
            P.dve(lambda e, i=i, py=py: e.tensor_tensor(ysb[i].ap, py.ap, yo[i].ap, ALU.add), reads=[py, yo[i]], writes=[ysb[i]])
            outs.append(P.dma("sp", y_d[tok0:tok0 + 128, :], ysb[i].ap, reads=[ysb[i]]))
            pst_ = PS[2]
            P.mm(pst_.ap, Btok[i].ap, xw[i].ap, True, True, reads=[Btok[i], xw[i]], writes=[pst_])
            P.dve(lambda e, i=i: e.tensor_tensor(v3(S.ap), v3(S.ap), bc(ex[i].ap[:, 16:24]), ALU.mult), reads=[S, ex[i]], writes=[S])
            P.dve(lambda e, pst_=pst_: e.tensor_tensor(S.ap, S.ap, pst_.ap, ALU.add), reads=[S, pst_], writes=[S])
            P.act(Sb.ap, S.ap, AF.Copy, reads=[S], writes=[Sb])
            P.dve(lambda e, i=i: e.tensor_tensor(v3(ug[i].ap), v3(xs_sb[i].ap), bc(dsum.ap), ALU.mult), reads=[xs_sb[i], dsum], writes=[ug[i]])
            P.dve(lambda e, i=i: e.tensor_tensor(ug[i].ap, ug[i].ap, ysb[i].ap, ALU.add), reads=[ug[i], ysb[i]], writes=[ug[i]])
            P.dve(lambda e, i=i: e.tensor_tensor(ug[i].ap, ug[i].ap, ypv[i].ap, ALU.add), reads=[ug[i], ypv[i]], writes=[ug[i]])
            P.dve(lambda e, i=i: e.tensor_tensor(ug[i].ap, ug[i].ap, zs[i].ap, ALU.mult), reads=[ug[i], zs[i]], writes=[ug[i]])
            P.dve(lambda e, i=i: e.memset(ss[i].ap, 0.0), writes=[ss[i]])
            P.act(usq[i].ap, ug[i].ap, AF.Square, reads=[ug[i]], writes=[usq[i], ss[i]], accum_out=ss[i].ap[:, 0:1])
            P.act(ss[i].ap[:, 1:2], ss[i].ap[:, 0:1], AF.Sqrt, reads=[ss[i], epst], writes=[ss[i]], bias=epst.ap, scale=1.0 / 512)
            P.dve(lambda e, i=i: e.reciprocal(ss[i].ap[:, 1:2], ss[i].ap[:, 1:2]), reads=[ss[i]], writes=[ss[i]])
            P.dve(lambda e, i=i: e.scalar_tensor_tensor(uo[i].ap, ug[i].ap, ss[i].ap[:, 1:2], gain.ap, ALU.mult, ALU.mult), reads=[ug[i], ss[i], gain], writes=[uo[i]])
            outs.append(P.dma("sp", u_d[tok0:tok0 + 128, :], uo[i].ap, reads=[uo[i]]))
    P.wait_all("sp", outs)
    P.emit()
    return nc, P
NKEY = 4352
NQ = 1088
SM_SCALE = 192.0 ** -0.5


def build_att(with_ctx=True):
    nc = bass.Bass("TRN2", target_bir_lowering=False)
    dt_ = lambda name, shape, dtype=F32, kind="ExternalInput": nc.dram_tensor(name, list(shape), dtype, kind=kind).ap()
    hTb_d = dt_("hTb", [D, NKEY], BF16); hTq_d = dt_("hTq", [D, NQ], BF16)
    wkv_d = dt_("wkv", [D, 640]); wq_d = dt_("wq", [D, 512])
    wuq_d = dt_("wuq", [512, 4096]); wukv_d = dt_("wukv", [512, 4096])
    qg_d = dt_("qg", [128, 4]); kvg_d = dt_("kvg", [128, 4])
    cosk_d = dt_("cosk", [64, NKEY]); sink_d = dt_("sink", [64, NKEY]); cosq_d = dt_("cosq", [64, NQ]); sinq_d = dt_("sinq", [64, NQ])
    att_d = dt_("attT", [D, NQ], BF16, "ExternalOutput")
    P = Prog(nc)
    PS = [P.ptile([128, 512], F32, f"PS{i}") for i in range(8)]
    ones = P.tile([128, 128], BF16, "ones")
    P.dve(lambda e: e.memset(ones.ap, 1.0), writes=[ones])
    epst = P.tile([128, 1], F32, "epst")
    P.dve(lambda e: e.memset(epst.ap, EPS), writes=[epst])
    kblocks = [(0, 256)] + [(256 + i * 512, 512) for i in range(8)]
    qblocks = [(0, 512), (512, 512)] + ([(1024, 64)] if with_ctx else [])
    ckvnT = TA(P, 4, kblocks, BF16, "ckvnT")
    krot = TA(P, 1, kblocks, BF16, "krot")
    cqnT = TA(P, 4, qblocks, BF16, "cqnT")
    cosq = P.tile([64, NQ], F32, "cosq"); sinq = P.tile([64, NQ], F32, "sinq")
    P.dma("sp", cosq.ap, cosq_d, writes=[cosq]); P.dma("sp", sinq.ap, sinq_d, writes=[sinq])

    def norm4(cf, nt, gain, out_ta, bi, t0, sq, rs, psn):
        for k in range(4):
            P.act(sq.ap[:, :nt], cf.ap[:, k, :nt], AF.Square, reads=[cf], writes=[sq])
            P.mm(psn.ap[:, :nt], ones.ap, sq.ap[:, :nt], k == 0, k == 3, reads=[ones, sq], writes=[psn])
        P.act(rs.ap[:, :nt], psn.ap[:, :nt], AF.Sqrt, reads=[psn, epst], writes=[rs], bias=epst.ap, scale=1.0 / 512)
        P.dve(lambda e: e.reciprocal(rs.ap[:, :nt], rs.ap[:, :nt]), reads=[rs], writes=[rs])
        for k in range(4):
            P.dve(lambda e, k=k: e.scalar_tensor_tensor(out_ta.ap[:, k, t0:t0 + nt], cf.ap[:, k, :nt], gain.ap[:, k:k + 1], rs.ap[:, :nt], ALU.mult, ALU.mult),
                  reads=[cf, gain, rs], writes=[out_ta.trk[k][bi]])

    with P.scope():
        wkv = P.tile([128, 16, 640], BF16, "wkv"); wq = P.tile([128, 16, 512], BF16, "wq")
        P.dma("pool", wkv.ap, wkv_d.rearrange("(k p) n -> p k n", p=128), writes=[wkv])
        P.dma("pool", wq.ap, wq_d.rearrange("(k p) n -> p k n", p=128), writes=[wq])
        qg = P.tile([128, 4], F32, "qg"); kvg = P.tile([128, 4], F32, "kvg")
        P.dma("sp", qg.ap, qg_d, writes=[qg]); P.dma("sp", kvg.ap, kvg_d, writes=[kvg])
        hb = [P.tile([128, 16, 512], BF16, f"hb{i}") for i in range(2)]
        cf = [P.tile([128, 4, 512], F32, f"cf{i}") for i in range(2)]
        sq = P.tile([128, 512], BF16, "sq"); rs = P.tile([128, 512], F32, "rs")
        ck = [P.tile([64, 512], F32, f"ck{i}") for i in range(2)]; sk = [P.tile([64, 512], F32, f"sk{i}") for i in range(2)]
        ra = P.tile([64, 512], F32, "ra"); rb = P.tile([64, 512], F32, "rb")
        hvb = hTb_d.rearrange("(k p) t -> p k t", p=128)
        hvq = hTq_d.rearrange("(k p) t -> p k t", p=128)
        n = 0
        for bi, (t0, nt) in enumerate(kblocks):
            h = hb[n % 2]; c = cf[n % 2]; n += 1
            P.dma("sp", h.ap[:, :, :nt], hvb[:, :, t0:t0 + nt], writes=[h])
            P.dma("sp", ck[bi % 2].ap[:, :nt], cosk_d[:, t0:t0 + nt], writes=[ck[bi % 2]])
            P.dma("sp", sk[bi % 2].ap[:, :nt], sink_d[:, t0:t0 + nt], writes=[sk[bi % 2]])
            for m in range(4):
                ps = PS[m % 2]
                for k in range(16):
                    P.mm(ps.ap[:, :nt], wkv.ap[:, k, m * 128:(m + 1) * 128], h.ap[:, k, :nt], k == 0, k == 15, reads=[wkv, h], writes=[ps])
                P.act(c.ap[:, m, :nt], ps.ap[:, :nt], AF.Copy, reads=[ps], writes=[c])
            pa1, pb1 = PS[2], PS[3]
            for k in range(16):
                P.mm(pa1.ap[:64, :nt], wkv.ap[:, k, 512:576], h.ap[:, k, :nt], k == 0, k == 15, reads=[wkv, h], writes=[pa1])
            for k in range(16):
                P.mm(pb1.ap[:64, :nt], wkv.ap[:, k, 576:640], h.ap[:, k, :nt], k == 0, k == 15, reads=[wkv, h], writes=[pb1])
            P.dve(lambda e, nt=nt, bi=bi: e.tensor_tensor(ra.ap[:, :nt], pa1.ap[:64, :nt], ck[bi % 2].ap[:, :nt], ALU.mult), reads=[pa1, ck[bi % 2]], writes=[ra])
            P.dve(lambda e, nt=nt, bi=bi: e.tensor_tensor(rb.ap[:, :nt], pb1.ap[:64, :nt], sk[bi % 2].ap[:, :nt], ALU.mult), reads=[pb1, sk[bi % 2]], writes=[rb])
            P.dve(lambda e, nt=nt, t0=t0: e.tensor_tensor(krot.ap[:64, 0, t0:t0 + nt], ra.ap[:, :nt], rb.ap[:, :nt], ALU.add), reads=[ra, rb], writes=[krot.trk[0][bi]])
            norm4(c, nt, kvg, ckvnT, bi, t0, sq, rs, PS[4])
        for bi, (t0, nt) in enumerate(qblocks):
            h = hb[n % 2]; c = cf[n % 2]; n += 1
            P.dma("sp", h.ap[:, :, :nt], hvq[:, :, t0:t0 + nt], writes=[h])
            for m in range(4):
                ps = PS[m % 2]
                for k in range(16):
                    P.mm(ps.ap[:, :nt], wq.ap[:, k, m * 128:(m + 1) * 128], h.ap[:, k, :nt], k == 0, k == 15, reads=[wq, h], writes=[ps])
                P.act(c.ap[:, m, :nt], ps.ap[:, :nt], AF.Copy, reads=[ps], writes=[c])
            norm4(c, nt, qg, cqnT, bi, t0, sq, rs, PS[4])
    wuq = P.tile([128, 4, 4096], BF16, "wuq"); wukv = P.tile([128, 4, 4096], BF16, "wukv")
    P.dma("pool", wuq.ap, wuq_d.rearrange("(k p) n -> p k n", p=128), writes=[wuq])
    P.dma("pool", wukv.ap, wukv_d.rearrange("(k p) n -> p k n", p=128), writes=[wukv])
    KnT = [TA(P, 1, kblocks, BF16, f"KnT{i}") for i in range(2)]
    Vt = [P.tile([128, 34, 128], BF16, f"Vt{i}") for i in range(2)]
    QnT = [TA(P, 1, qblocks, BF16, f"QnT{i}") for i in range(2)]
    qrot = [TA(P, 1, qblocks, BF16, f"qrot{i}") for i in range(2)]
    ra2 = P.tile([64, 512], F32, "ra2"); rb2 = P.tile([64, 512], F32, "rb2")
    pT = [P.tile([128, 512], BF16, f"pT{i}") for i in range(3)]
    rec = [P.tile([128, 512], F32, f"rec{i}") for i in range(2)]
    ao = [P.tile([128, NQ], BF16, f"ao{i}") for i in range(2)]
    outs = []
    av = att_d.rearrange("(h p) t -> p h t", p=128)
    pn = [0]
    for hd in range(16):
        i = hd % 2
        c0 = hd * 256
        for bi, (t0, nt) in enumerate(kblocks):
            ps = PS[bi % 2]
            for k in range(4):
                P.mm(ps.ap[:, :nt], wukv.ap[:, k, c0:c0 + 128], ckvnT.ap[:, k, t0:t0 + nt], k == 0, k == 3, reads=[wukv, ckvnT.trk[k][bi]], writes=[ps])
            P.act(KnT[i].ap[:, 0, t0:t0 + nt], ps.ap[:, :nt], AF.Copy, reads=[ps], writes=[KnT[i].trk[0][bi]])
        for g4 in range(9):
            ps = PS[2 + g4 % 2]
            nkt = min(4, 34 - g4 * 4)
            for j in range(nkt):
                kt = g4 * 4 + j
                for k in range(4):
                    P.mm(ps.ap[:, j * 128:(j + 1) * 128], ckvnT.ap[:, k, kt * 128:(kt + 1) * 128], wukv.ap[:, k, c0 + 128:c0 + 256], k == 0, k == 3,
                         reads=[wukv] + ckvnT.col((kt * 128 + 256) // 512 if kt >= 2 else 0)[k:k + 1], writes=[ps])
            P.dve(lambda e, ps=ps, g4=g4, nkt=nkt, i=i: e.tensor_copy(Vt[i].ap[:, g4 * 4:g4 * 4 + nkt, :], ps.ap[:, :nkt * 128].rearrange("p (j d) -> p j d", d=128)),
                  reads=[ps], writes=[Vt[i]])
        for bi, (t0, nt) in enumerate(qblocks):
            ps = PS[4]
            for k in range(4):
                P.mm(ps.ap[:, :nt], wuq.ap[:, k, c0:c0 + 128], cqnT.ap[:, k, t0:t0 + nt], k == 0, k == 3, reads=[wuq, cqnT.trk[k][bi]], writes=[ps])
            P.act(QnT[i].ap[:, 0, t0:t0 + nt], ps.ap[:, :nt], AF.Copy, reads=[ps], writes=[QnT[i].trk[0][bi]])
            pa, pb = PS[5], PS[6]
            for k in range(4):
                P.mm(pa.ap[:64, :nt], wuq.ap[:, k, c0 + 128:c0 + 192], cqnT.ap[:, k, t0:t0 + nt], k == 0, k == 3, reads=[wuq, cqnT.trk[k][bi]], writes=[pa])
            for k in range(4):
                P.mm(pb.ap[:64, :nt], wuq.ap[:, k, c0 + 192:c0 + 256], cqnT.ap[:, k, t0:t0 + nt], k == 0, k == 3, reads=[wuq, cqnT.trk[k][bi]], writes=[pb])
            P.dve(lambda e, nt=nt, t0=t0: e.tensor_tensor(ra2.ap[:, :nt], pa.ap[:64, :nt], cosq.ap[:, t0:t0 + nt], ALU.mult), reads=[pa, cosq], writes=[ra2])
            P.dve(lambda e, nt=nt, t0=t0: e.tensor_tensor(rb2.ap[:, :nt], pb.ap[:64, :nt], sinq.ap[:, t0:t0 + nt], ALU.mult), reads=[pb, sinq], writes=[rb2])
            P.dve(lambda e, nt=nt, t0=t0, i=i: e.tensor_tensor(qrot[i].ap[:64, 0, t0:t0 + nt], ra2.ap[:, :nt], rb2.ap[:, :nt], ALU.add), reads=[ra2, rb2], writes=[qrot[i].trk[0][bi]])
        for bi, (t0, nt) in enumerate(qblocks):
            kts = range(34) if bi < 2 else range(2)
            pO, pD = PS[bi % 2], PS[2 + bi % 2]
            for kt in kts:
                kb = (kt * 128 + 256) // 512 if kt >= 2 else 0
                pS = PS[4 + pn[0] % 3]
                pt = pT[pn[0] % 3]
                pn[0] += 1
                ksl = slice(kt * 128, (kt + 1) * 128)
                P.mm(pS.ap[:, :nt], KnT[i].ap[:, 0, ksl], QnT[i].ap[:, 0, t0:t0 + nt], True, False, reads=[KnT[i].trk[0][kb], QnT[i].trk[0][bi]], writes=[pS])
                P.mm(pS.ap[:, :nt], krot.ap[:64, 0, ksl], qrot[i].ap[:64, 0, t0:t0 + nt], False, True, reads=[krot.trk[0][kb], qrot[i].trk[0][bi]], writes=[pS])
                P.act(pt.ap[:, :nt], pS.ap[:, :nt], AF.Exp, reads=[pS], writes=[pt], scale=SM_SCALE)
                P.mm(pO.ap[:, :nt], Vt[i].ap[:, kt, :], pt.ap[:, :nt], kt == kts[0], kt == kts[-1], reads=[Vt[i], pt], writes=[pO])
                P.mm(pD.ap[:, :nt], ones.ap, pt.ap[:, :nt], kt == kts[0], kt == kts[-1], reads=[ones, pt], writes=[pD])
            r = rec[bi % 2]
            P.dve(lambda e, r=r, pD=pD, nt=nt: e.reciprocal(r.ap[:, :nt], pD.ap[:, :nt]), reads=[pD], writes=[r])
            P.dve(lambda e, r=r, pO=pO, nt=nt, t0=t0, i=i: e.tensor_tensor(ao[i].ap[:, t0:t0 + nt], pO.ap[:, :nt], r.ap[:, :nt], ALU.mult), reads=[pO, r], writes=[ao[i]])
        nq = qblocks[-1][0] + qblocks[-1][1]
        outs.append(P.dma("sp", av[:, hd, :nq], ao[i].ap[:, :nq], reads=[ao[i]]))
    P.wait_all("sp", outs)
    P.emit()
    return nc, P
def rope_tables(q):
    n = 4096
    rows = n // 64
    row = np.repeat(np.arange(rows, dtype=np.float32), 64)
    col = np.tile(np.arange(64, dtype=np.float32), rows)
    inv = (np.float32(10000.0) ** (-np.arange(0, 32, 2, dtype=np.float32) / np.float32(32))).astype(np.float32)
    ang = np.stack([row[:, None] * inv, col[:, None] * inv], axis=1)
    cos = np.cos(ang).astype(np.float32); sin = np.sin(ang).astype(np.float32)
    C = np.zeros((64, n), np.float32); S = np.zeros((64, n), np.float32)
    for ax in range(2):
        for half in range(2):
            r = slice(ax * 32 + half * 16, ax * 32 + half * 16 + 16)
            C[r] = cos[:, ax, :].T
            S[r] = (-sin[:, ax, :].T) if half == 0 else sin[:, ax, :].T
    onesc = np.ones((64, 256), np.float32); zc = np.zeros((64, 256), np.float32)
    cosk = np.concatenate([onesc, C], 1); sink = np.concatenate([zc, S], 1)
    cosq = np.concatenate([C[:, q * 1024:(q + 1) * 1024], onesc[:, :64]], 1); sinq = np.concatenate([S[:, q * 1024:(q + 1) * 1024], zc[:, :64]], 1)
    return {"cosk": cosk, "sink": sink, "cosq": np.ascontiguousarray(cosq), "sinq": np.ascontiguousarray(sinq)}


ROPE_PERM = np.concatenate([np.arange(16, 32), np.arange(0, 16), np.arange(48, 64), np.arange(32, 48)])


def att_weights(w_in, w_uq, w_ukv, q_norm, kv_norm):
    wkv = np.concatenate([w_in[:, 512:1088], w_in[:, 1024:1088][:, ROPE_PERM]], 1)
    u = w_uq.reshape(512, 16, 192)
    wuq = np.concatenate([u, u[:, :, 128:][:, :, ROPE_PERM]], 2).reshape(512, 4096)
    return {"wkv": np.ascontiguousarray(wkv), "wq": np.ascontiguousarray(w_in[:, 0:512]), "wuq": np.ascontiguousarray(wuq), "wukv": w_ukv,
            "qg": np.ascontiguousarray(q_norm.reshape(4, 128).T), "kvg": np.ascontiguousarray(kv_norm.reshape(4, 128).T)}
def build_ka(NT):
    nc = bass.Bass("TRN2", target_bir_lowering=False)
    xT_d = nc.dram_tensor("xT", [D, NT], F32, kind="ExternalInput").ap()
    cT_d = nc.dram_tensor("cT", [128, 16, 2], F32, kind="ExternalInput").ap()
    wada_d = nc.dram_tensor("wada", [D, 6 * D], F32, kind="ExternalInput").ap()
    bada_d = nc.dram_tensor("bada", [128, 96], F32, kind="ExternalInput").ap()
    gn_d = nc.dram_tensor("gn", [128, 16], F32, kind="ExternalInput").ap()
    mod_d = nc.dram_tensor("mod", [128, 96, 2], F32, kind="ExternalOutput").ap()
    hT_d = nc.dram_tensor("hT", [D, NT], BF16, kind="ExternalOutput").ap()
    P = Prog(nc)
    mod = P.tile([128, 96, 2], F32, "mod")
    emit_adaln(P, cT_d, wada_d, bada_d, mod)
    dm = P.dma("sp", mod_d, mod.ap, reads=[mod])
    gsc = P.tile([128, 16, 2], F32, "gsc")
    emit_gsc(P, gsc, mod, gn_d, 1)
    ones = P.tile([128, 128], BF16, "ones")
    P.dve(lambda e: e.memset(ones.ap, 1.0), writes=[ones])
    blocks = [(0, 512), (512, 512)] + ([(1024, 64)] if NT > 1024 else [])
    xT = TA(P, 16, blocks, F32, "xT"); hT = TA(P, 16, blocks, BF16, "hT")
    xv = xT_d.rearrange("(k p) t -> p k t", p=128); hv = hT_d.rearrange("(k p) t -> p k t", p=128)
    for bi, (t0, nt) in enumerate(blocks):
        P.dma("sp", xT.ap[:, :, t0:t0 + nt], xv[:, :, t0:t0 + nt], writes=xT.col(bi))
    pss = [P.ptile([128, 512], F32, f"n_ps{i}") for i in range(2)]
    emit_norm_mod(P, xT, hT, [0, 0, 1], gsc, mod, 0, ones, pss)
    outs = [dm]
    for bi, (t0, nt) in enumerate(blocks):
        outs.append(P.dma("sp", hv[:, :, t0:t0 + nt], hT.ap[:, :, t0:t0 + nt], reads=hT.col(bi)))
    P.wait_all("sp", outs)
    P.emit()
    return nc


def _run(nc, in_maps):
    res = run_bass_kernel_spmd(nc, in_maps, core_ids=list(range(8)))
    return res.results


def _ssd_consts(l, g, d, w_in, conv_w, conv_b, dt_bias, a_log, d_skip, ssm_norm):
    cols = np.concatenate([np.arange(1088 + g * 512, 1088 + (g + 1) * 512), np.arange(5184 + g * 512, 5184 + (g + 1) * 512),
                           np.arange(9280 + g * 128, 9280 + (g + 1) * 128), np.arange(10304 + g * 128, 10304 + (g + 1) * 128),
                           np.arange(11328 + d * 64 + g * 8, 11328 + d * 64 + g * 8 + 8)])
    chans = np.concatenate([np.arange(g * 512, (g + 1) * 512), 4096 + np.arange(g * 128, (g + 1) * 128), 5120 + np.arange(g * 128, (g + 1) * 128)])
    cw = conv_w[:, chans]
    if d == 1:
        cw = cw[::-1]
    kk = np.arange(128)
    hs = slice(8 * g, 8 * g + 8)
    return {"wssd": np.ascontiguousarray(w_in[:, cols]), "convw": np.ascontiguousarray(cw.reshape(5, 6, 128).transpose(2, 1, 0)),
            "convb": np.ascontiguousarray(conv_b[chans].reshape(6, 128).T),
            "dtb": np.broadcast_to(dt_bias[d, hs], (128, 8)).copy(), "alog": np.broadcast_to(a_log[d, hs], (128, 8)).copy(),
            "dsk": np.broadcast_to(d_skip[:, hs], (128, 2, 8)).copy(), "gain": np.broadcast_to(ssm_norm[g * 512:(g + 1) * 512], (128, 512)).copy(),
            "ident": np.eye(128, dtype=np.float32), "mU": (kk[:, None] > kk[None, :]).astype(np.float32),
            "mL": (kk[:, None] <= kk[None, :]).astype(np.float32), "mF": (kk[None, :] >= kk[:, None]).astype(np.float32)}


def _rev(y):
    out = np.empty_like(y)
    for b in range(2):
        o = b * 4352
        out[o:o + 256] = y[o:o + 256][::-1]
        out[o + 256:o + 4352] = y[o + 256:o + 4352][::-1]
    return out


def kernel(x, c, ctx, c_ctx, norm_mix, norm_ffn, w_ada, b_ada, w_in, q_norm, w_uq, kv_norm, w_ukv, conv_w, conv_b, a_log, dt_bias,
           d_skip, ssm_norm, w_oa, w_ob, w_out, w1_dense, w3_dense, w2_dense, w_router, w1_moe, w3_moe, w2_moe, final_norm):
    A = lambda a: np.ascontiguousarray(np.asarray(a))
    x = np.asarray(x, np.float32); ctx = np.asarray(ctx, np.float32)
    ident = np.eye(128, dtype=np.float32)
    progs = {}

    def prog(name, fn, *a):
        if name not in progs:
            r = fn(*a)
            progs[name] = r[0] if isinstance(r, tuple) else r
        return progs[name]

    out = None
    for l in range(2):
        last = l == 1
        ims = []
        for core in range(8):
            b, q = core // 4, core % 4
            xo = np.concatenate([x[b, q * 1024:(q + 1) * 1024], ctx[b, q * 64:(q + 1) * 64]], 0)
            cv = np.stack([np.asarray(c)[b], np.asarray(c_ctx)], 0)
            ims.append({"xT": A(xo.T), "cT": A(cv.reshape(2, 16, 128).transpose(2, 1, 0)), "wada": A(w_ada[l]),
                        "bada": A(np.asarray(b_ada[l]).reshape(96, 128).T), "gn": A(np.asarray(norm_mix[l]).reshape(16, 128).T)})
        ra = _run(prog("ka", build_ka, 1088), ims)
        mods = [r["mod"] for r in ra]
        hTs = [np.asarray(r["hT"]) for r in ra]
        xTs = [im["xT"] for im in ims]
        h_lat = [np.concatenate([hTs[b * 4 + q][:, :1024] for q in range(4)], 1) for b in range(2)]
        h_ctx = [np.concatenate([hTs[b * 4 + q][:, 1024:] for q in range(4)], 1) for b in range(2)]
        aw = att_weights(np.asarray(w_in[l]), np.asarray(w_uq[l]), np.asarray(w_ukv[l]), np.asarray(q_norm[l]), np.asarray(kv_norm[l]))
        ims = []
        for core in range(8):
            b, q = core // 4, core % 4
            m = dict(aw); m.update(rope_tables(q))
            m["hTb"] = A(np.concatenate([h_ctx[b], h_lat[b]], 1)); m["hTq"] = hTs[core]
            ims.append(m)
        rt = _run(prog("att", build_att, True), ims)
        attTs = [np.asarray(r["attT"]) for r in rt]
        hT0 = A(np.concatenate([h_ctx[0], h_lat[0], h_ctx[1], h_lat[1]], 1))
        hT1 = A(np.concatenate([h_ctx[0][:, ::-1], h_lat[0][:, ::-1], h_ctx[1][:, ::-1], h_lat[1][:, ::-1]], 1))
        sargs = (np.asarray(w_in[l]), np.asarray(conv_w[l]), np.asarray(conv_b[l]), np.asarray(dt_bias[l]), np.asarray(a_log[l]),
                 np.asarray(d_skip[l]), np.asarray(ssm_norm[l]))
        zero_y = np.zeros((8704, 512), np.float32)
        r0 = _run(prog("ssd", build_ssd), [dict(_ssd_consts(l, g, 0, *sargs), hT=hT0, yprev=zero_y) for g in range(8)])
        r1 = _run(prog("ssd", build_ssd), [dict(_ssd_consts(l, g, 1, *sargs), hT=hT1, yprev=A(_rev(r0[g]["y"]))) for g in range(8)])
        U = np.concatenate([_rev(np.asarray(r1[g]["u"])) for g in range(8)], 1)
        NT = 1024 if last else 1088
        ims = []
        for core in range(8):
            b, q = core // 4, core % 4
            o = b * 4352
            uo = np.concatenate([U[o + 256 + q * 1024:o + 256 + (q + 1) * 1024], U[o + q * 64:o + (q + 1) * 64]], 0)[:NT]
            m = {"xT": A(xTs[core][:, :NT]), "hT": A(hTs[core][:, :NT]), "attT": A(attTs[core][:, :NT]), "uT": A(uo.T), "mod": mods[core],
                 "gn": A(np.asarray(norm_ffn[l]).reshape(16, 128).T), "ident": ident, "wg": A(np.asarray(w_in[l])[:, 11456:]),
                 "woa": A(w_oa[l]), "wob": A(w_ob[l]), "wout": A(w_out[l])}
            if not last:
                m.update({"w1": A(w1_dense[0]), "w3": A(w3_dense[0]), "w2": A(w2_dense[0])})
            else:
                m["wr"] = A(np.asarray(w_router[0]).reshape(16, 128, 8).transpose(1, 0, 2))
            ims.append(m)
        rc = _run(prog("kc%d" % l, build_kc, l, NT), ims)
        if not last:
            xn = np.empty_like(x); cn = np.empty_like(ctx)
            for core in range(8):
                b, q = core // 4, core % 4
                o = rc[core]["xoutT"]
                xn[b, q * 1024:(q + 1) * 1024] = o[:, :1024].T
                cn[b, q * 64:(q + 1) * 64] = o[:, 1024:].T
            x, ctx = xn, cn
        else:
            h2_all = A(np.concatenate([np.asarray(rc[core]["h2T"]) for core in range(8)], 1))
            gate_all = np.concatenate([rc[core]["gateT"] for core in range(8)], 1)
            modb = A(np.stack([mods[0][:, :, 0], mods[4][:, :, 0]], -1))
            ims = [{"h2T": h2_all, "gbc": np.broadcast_to(gate_all[e], (128, 8192)).copy(), "mod": modb,
                    "w1": A(w1_moe[0][e]), "w3": A(w3_moe[0][e]), "w2": A(w2_moe[0][e])} for e in range(8)]
            re_ = _run(prog("ke", build_ke), ims)
            fnl = A(np.asarray(final_norm).reshape(16, 128).T)
            ims = [{"xT": rc[core]["xoutT"], "parts": A(np.stack([re_[e]["part"][:, core * 1024:(core + 1) * 1024] for e in range(8)], 0)), "fn": fnl}
                   for core in range(8)]
            rf = _run(prog("kf", build_kf), ims)
            out = np.empty((2, 4096, 2048), np.float32)
            for core in range(8):
                b, q = core // 4, core % 4
                out[b, q * 1024:(q + 1) * 1024] = rf[core]["outT"].T
    return out
```

```python
import numpy as np
from contextlib import ExitStack
import concourse.bass as bass
import concourse.mybir as mybir
from concourse.bass_utils import run_bass_kernel_spmd

F32 = mybir.dt.float32
BF16 = mybir.dt.bfloat16
AF = mybir.ActivationFunctionType
ALU = mybir.AluOpType
AX = mybir.AxisListType

SAME_ENGINE_SYNC = True
NDSEM = 24


class Trk:
    __slots__ = ("lw", "rd", "name")

    def __init__(self, name=""):
        self.lw = None
        self.rd = []
        self.name = name


class T:
    __slots__ = ("ap", "trk")

    def __init__(self, ap, trk=None, name=""):
        self.ap = ap
        self.trk = trk if trk is not None else Trk(name)

    def __getitem__(self, k):
        return self.ap[k]


class Op:
    __slots__ = ("eng", "fn", "deps", "dma", "ordinal", "sig", "sigidx", "dsem", "dval", "waits", "prewait")

    def __init__(self, eng, fn, deps, dma):
        self.eng = eng
        self.fn = fn
        self.deps = deps
        self.dma = dma
        self.sig = False
        self.sigidx = 0
        self.dsem = None
        self.dval = 0
        self.waits = []
        self.prewait = None


class Prog:
    ENGS = ("pe", "act", "dve", "pool", "sp")

    def __init__(self, nc):
        self.nc = nc
        self.ops = []
        self.es = ExitStack()
        self._n = 0
        self.bar_deps = set()
        self.bar_start = 0

    def sbuf(self, shape, dtype, name=None):
        self._n += 1
        name = f"sb{self._n}_" + (name or "")
        h = self.es.enter_context(self.nc.sbuf_tensor(name, list(shape), dtype))
        return h

    def psum(self, shape, dtype=F32, name=None):
        self._n += 1
        name = f"ps{self._n}_" + (name or "")
        h = self.es.enter_context(self.nc.psum_tensor(name, list(shape), dtype))
        return h

    def tile(self, shape, dtype, name=None):
        h = self.sbuf(shape, dtype, name)
        return T(h[:], name=name or "")

    def ptile(self, shape, dtype=F32, name=None):
        h = self.psum(shape, dtype, name)
        return T(h[:], name=name or "")

    def op(self, eng, fn, reads=(), writes=(), dma=False):
        i = len(self.ops)
        deps = set()
        for t in reads:
            k = t.trk if isinstance(t, T) else t
            if k.lw is not None:
                deps.add(k.lw)
        for t in writes:
            k = t.trk if isinstance(t, T) else t
            if k.lw is not None:
                deps.add(k.lw)
            deps.update(k.rd)
        for t in reads:
            k = t.trk if isinstance(t, T) else t
            k.rd.append(i)
        for t in writes:
            k = t.trk if isinstance(t, T) else t
            k.lw = i
            k.rd = []
        deps.discard(i)
        deps.update(self.bar_deps)
        self.ops.append(Op(eng, fn, deps, dma))
        return i

    def dma(self, q, out, in_, reads=(), writes=()):
        return self.op(q, lambda e: e.dma_start(out=out, in_=in_), reads, writes, dma=True)

    def mm(self, out, lhsT, rhs, start, stop, reads=(), writes=()):
        return self.op("pe", lambda e: e.matmul(out, lhsT, rhs, start=start, stop=stop), reads, writes)

    def transpose(self, out, in_, ident, reads=(), writes=()):
        return self.op("pe", lambda e: e.transpose(out, in_, ident), reads, writes)

    def act(self, out, in_, func, reads=(), writes=(), **kw):
        return self.op("act", lambda e: e.activation(out, in_, func, **kw), reads, writes)

    def dve(self, fn, reads=(), writes=()):
        return self.op("dve", fn, reads, writes)

    def scope(self):
        prog = self
        class _S:
            def __enter__(s_):
                s_.saved = prog.es
                prog.es = ExitStack()
                return s_
            def __exit__(s_, *a):
                prog.barrier()
                prog.es.close()
                prog.es = s_.saved
                return False
        return _S()

    def barrier(self):
        last = {}
        used = set()
        for i in range(self.bar_start, len(self.ops)):
            o = self.ops[i]
            used.update(o.deps)
        for i in range(self.bar_start, len(self.ops)):
            o = self.ops[i]
            if o.dma:
                if i not in used:
                    last[("dma", i)] = i
            else:
                last[o.eng] = i
        deps = set(last.values()) | set(self.bar_deps)
        self.bar_deps = deps
        self.bar_start = len(self.ops)

    def wait_all(self, eng, ids):
        o = Op(eng, None, set(ids), False)
        self.ops.append(o)
        return len(self.ops) - 1

    def emit(self):
        nc = self.nc
        ops = self.ops
        per = {e: [] for e in self.ENGS}
        for i, o in enumerate(ops):
            o.ordinal = len(per[o.eng])
            per[o.eng].append(i)
        dsems = {}
        dcount = {e: 0 for e in self.ENGS}
        for e in self.ENGS:
            if any(ops[i].dma for i in per[e]):
                dsems[e] = [self.es.enter_context(nc.semaphore(f"d_{e}_{k}")) for k in range(NDSEM)]
        esem = {e: self.es.enter_context(nc.semaphore(f"s_{e}")) for e in self.ENGS}
        known = {e: {s: 0 for s in self.ENGS} for e in self.ENGS}
        known_dma = {e: set() for e in self.ENGS}
        for i, o in enumerate(ops):
            eb = o.eng
            kn = known[eb]
            if o.dma:
                n = dcount[eb]
                dcount[eb] += 1
                o.dsem = dsems[eb][n % NDSEM]
                o.dval = 16 * (n // NDSEM + 1)
                if n >= NDSEM:
                    o.prewait = (o.dsem, 16 * (n // NDSEM))
            need = {}
            for d in o.deps:
                a = ops[d]
                if a.dma:
                    if d not in known_dma[eb]:
                        known_dma[eb].add(d)
                        o.waits.append(("dma", d))
                else:
                    ea = a.eng
                    if ea == eb and (ea == "pe" or not SAME_ENGINE_SYNC):
                        continue
                    if a.ordinal + 1 > kn[ea]:
                        need[ea] = max(need.get(ea, 0), a.ordinal + 1)
            for ea, v in need.items():
                kn[ea] = v
                src = per[ea][v - 1]
                ops[src].sig = True
                o.waits.append(("eng", src))
        for e in self.ENGS:
            c = 0
            for i in per[e]:
                if ops[i].sig:
                    c += 1
                    ops[i].sigidx = c
        self.nsig = {e: sum(1 for i in per[e] if ops[i].sig) for e in self.ENGS}
        self.nops = {e: len(per[e]) for e in self.ENGS}

        def run(eng_name):
            def body(e):
                for i in per[eng_name]:
                    o = ops[i]
                    if o.prewait is not None:
                        e.wait_ge(o.prewait[0], o.prewait[1])
                    for kind, d in o.waits:
                        a = ops[d]
                        if kind == "dma":
                            e.wait_ge(a.dsem, a.dval)
                        else:
                            e.wait_ge(esem[a.eng], a.sigidx)
                    if o.fn is None:
                        continue
                    ins = o.fn(e)
                    if o.dma:
                        ins.then_inc(o.dsem, 16)
                    elif o.sig:
                        ins.then_inc(esem[eng_name], 1)
            return body

        with nc.Block() as block:
            block.tensor(run("pe"))
            block.scalar(run("act"))
            block.vector(run("dve"))
            block.gpsimd(run("pool"))
            block.sync(run("sp"))

    def close(self):
        self.es.close()
D = 2048
KC = 16
EPS = 1e-6


class TA:
    def __init__(self, P, nk, blocks, dtype, name):
        self.blocks = blocks
        self.nt = blocks[-1][0] + blocks[-1][1]
        self.nk = nk
        h = P.sbuf([128, nk, self.nt], dtype, name)
        self.ap = h[:]
        self.trk = [[Trk(f"{name}{k}_{b}") for b in range(len(blocks))] for k in range(nk)]

    def all(self):
        return [t for row in self.trk for t in row]

    def col(self, b):
        return [self.trk[k][b] for k in range(self.nk)]


def rr(lst, st):
    i = st[0] % len(lst)
    st[0] += 1
    return lst[i]


def emit_adaln(P, cT_d, wada_d, bada_d, mod):
    with P.scope():
        cT = P.tile([128, 16, 2], F32, "cT")
        sc = P.tile([128, 16, 2], F32, "scT")
        bt = P.tile([128, 96], F32, "badaT")
        P.dma("sp", cT.ap, cT_d, writes=[cT])
        P.dma("sp", bt.ap, bada_d, writes=[bt])
        P.act(sc.ap, cT.ap, AF.Silu, reads=[cT], writes=[sc])
        wb = [P.tile([128, 16, 512], F32, f"wada{i}") for i in range(2)]
        pm = [P.ptile([128, 512], F32, f"pm{i}") for i in range(2)]
        wv = wada_d.rearrange("(k p) n -> p k n", p=128)
        for cb in range(24):
            w = wb[cb % 2]
            P.dma("sp", w.ap, wv[:, :, cb * 512:(cb + 1) * 512], writes=[w])
            ps = pm[cb % 2]
            for j in range(4):
                for k in range(16):
                    P.mm(ps.ap[:, j * 2:(j + 1) * 2], w.ap[:, k, j * 128:(j + 1) * 128], sc.ap[:, k, :], k == 0, k == 15,
                         reads=[w, sc], writes=[ps])
            P.dve(lambda e, ps=ps, cb=cb: e.tensor_tensor(mod.ap[:, cb * 4:(cb + 1) * 4, :], ps.ap[:, 0:8].rearrange("p (j s) -> p j s", s=2),
                                                           bt.ap[:, cb * 4:(cb + 1) * 4].unsqueeze(2).to_broadcast([128, 4, 2]), ALU.add),
                  reads=[ps, bt], writes=[mod])


def emit_gsc(P, gsc, mod, gn_d, scale_j):
    gn = P.tile([128, 16], F32, "gn")
    P.dma("sp", gn.ap, gn_d, writes=[gn])
    P.dve(lambda e: e.tensor_scalar(gsc.ap, mod.ap[:, scale_j * 16:(scale_j + 1) * 16, :], 1.0, None, ALU.add), reads=[mod], writes=[gsc])
    P.dve(lambda e: e.tensor_tensor(gsc.ap, gsc.ap, gn.ap.unsqueeze(2).to_broadcast([128, 16, 2]), ALU.mult), reads=[gsc, gn], writes=[gsc])


def emit_norm_mod(P, xT, hT, sel, gsc, mod, shift_j, ones, pss, router=None):
    sq = [P.tile([128, 512], BF16, f"sq{i}") for i in range(2)]
    rs = [P.tile([128, 512], F32, f"rs{i}") for i in range(2)]
    tmp = [P.tile([128, 512], F32, f"nt{i}") for i in range(3)]
    epst = P.tile([128, 1], F32, "epst")
    P.dve(lambda e: e.memset(epst.ap, EPS), writes=[epst])
    n = [0]
    m = [0]
    for bi, (t0, nt) in enumerate(xT.blocks):
        s = sel[bi]
        ps = pss[bi % len(pss)]
        for k in range(16):
            q = rr(sq, n)
            P.act(q.ap[:, :nt], xT.ap[:, k, t0:t0 + nt], AF.Square, reads=[xT.trk[k][bi]], writes=[q])
            P.mm(ps.ap[:, :nt], ones.ap, q.ap[:, :nt], k == 0, k == 15, reads=[ones, q], writes=[ps])
        r = rs[bi % 2]
        P.act(r.ap[:, :nt], ps.ap[:, :nt], AF.Sqrt, reads=[ps, epst], writes=[r], bias=epst.ap, scale=1.0 / D)
        P.dve(lambda e, r=r, nt=nt: e.reciprocal(r.ap[:, :nt], r.ap[:, :nt]), reads=[r], writes=[r])
        for k in range(16):
            tt = rr(tmp, m)
            P.dve(lambda e, tt=tt, k=k, r=r, t0=t0, nt=nt: e.tensor_tensor(tt.ap[:, :nt], xT.ap[:, k, t0:t0 + nt], r.ap[:, :nt], ALU.mult),
                  reads=[xT.trk[k][bi], r], writes=[tt])
            if gsc is None:
                P.dve(lambda e, tt=tt, k=k, t0=t0, nt=nt: e.tensor_scalar(hT.ap[:, k, t0:t0 + nt], tt.ap[:, :nt], mod.ap[:, k:k + 1], None, ALU.mult),
                      reads=[tt, mod], writes=[hT.trk[k][bi]])
            elif router is None:
                P.dve(lambda e, tt=tt, k=k, t0=t0, nt=nt, s=s: e.tensor_scalar(hT.ap[:, k, t0:t0 + nt], tt.ap[:, :nt], gsc.ap[:, k, s:s + 1],
                                                                          mod.ap[:, shift_j * 16 + k, s:s + 1], ALU.mult, ALU.add),
                      reads=[tt, gsc, mod], writes=[hT.trk[k][bi]])
            else:
                wr, psr, logT = router
                P.dve(lambda e, tt=tt, k=k, t0=t0, nt=nt, s=s: e.tensor_scalar(tt.ap[:, :nt], tt.ap[:, :nt], gsc.ap[:, k, s:s + 1],
                                                                          mod.ap[:, shift_j * 16 + k, s:s + 1], ALU.mult, ALU.add),
                      reads=[tt, gsc, mod], writes=[tt])
                P.act(hT.ap[:, k, t0:t0 + nt], tt.ap[:, :nt], AF.Copy, reads=[tt], writes=[hT.trk[k][bi]])
                P.mm(psr.ap[:8, :nt], wr.ap[:, k, :], tt.ap[:, :nt], k == 0, k == 15, reads=[wr, tt], writes=[psr])
        if router is not None:
            wr, psr, logT = router
            P.act(logT.ap[:, t0:t0 + nt], psr.ap[:8, :nt], AF.Copy, reads=[psr], writes=[logT])


class WStream:
    def __init__(self, P, nk, mcols, nbuf, name, q="pool"):
        self.P = P
        self.bufs = [P.tile([128, nk, mcols], BF16, f"{name}{i}") for i in range(nbuf)]
        self.n = [0]
        self.nk = nk
        self.q = q

    def load(self, wd, c0, mc):
        t = rr(self.bufs, self.n)
        self.P.dma(self.q, t.ap[:, :, :mc], wd.rearrange("(k p) n -> p k n", p=128)[:, :, c0:c0 + mc], writes=[t])
        return t


def gemm_fm(P, ws, wd, M, rhs, pss, pst, evac, mcols=None):
    mcols = mcols or ws.bufs[0].ap.shape[2]
    nk = rhs.nk
    for c0 in range(0, M, mcols):
        mc = min(mcols, M - c0)
        wt = ws.load(wd, c0, mc)
        for mi in range(mc // 128):
            for bi, (t0, nt) in enumerate(rhs.blocks):
                ps = rr(pss, pst)
                for k in range(nk):
                    P.mm(ps.ap[:, :nt], wt.ap[:, k, mi * 128:(mi + 1) * 128], rhs.ap[:, k, t0:t0 + nt], k == 0, k == nk - 1,
                         reads=[wt, rhs.trk[k][bi]], writes=[ps])
                evac((c0 // 128) + mi, bi, t0, nt, ps)
DFF = 5632
DFFE = 7168
NEXP = 8


def emit_ffn(P, xT, h2T, sel, mod, w1d, w3d, w2d, FF, ws1, ws3, ws2, gTs, psAB, psO, sa_t, tg_t, st, gbc=None):
    for g in range(FF // 256):
        w1t = ws1.load(w1d, g * 256, 256)
        w3t = ws3.load(w3d, g * 256, 256)
        w2t = rr(ws2.bufs, ws2.n)
        P.dma(ws2.q, w2t.ap, w2d[g * 256:(g + 1) * 256, :].rearrange("(j p) n -> p j n", p=128), writes=[w2t])
        gT = rr(gTs, st["g"])
        for bi, (t0, nt) in enumerate(h2T.blocks):
            s = sel[bi]
            for j in range(2):
                pa = rr(psAB, st["ab"])
                for k in range(16):
                    P.mm(pa.ap[:, :nt], w1t.ap[:, k, j * 128:(j + 1) * 128], h2T.ap[:, k, t0:t0 + nt], k == 0, k == 15,
                         reads=[w1t, h2T.trk[k][bi]], writes=[pa])
                pb = rr(psAB, st["ab"])
                for k in range(16):
                    P.mm(pb.ap[:, :nt], w3t.ap[:, k, j * 128:(j + 1) * 128], h2T.ap[:, k, t0:t0 + nt], k == 0, k == 15,
                         reads=[w3t, h2T.trk[k][bi]], writes=[pb])
                sa = rr(sa_t, st["sa"])
                P.act(sa.ap[:, :nt], pa.ap[:, :nt], AF.Silu, reads=[pa], writes=[sa])
                if gbc is None:
                    P.dve(lambda e, sa=sa, pb=pb, gT=gT, j=j, t0=t0, nt=nt: e.tensor_tensor(gT.ap[:, j, t0:t0 + nt], sa.ap[:, :nt], pb.ap[:, :nt], ALU.mult),
                          reads=[sa, pb], writes=[gT.trk[j][bi]])
                else:
                    tg = rr(tg_t, st["tg"])
                    P.dve(lambda e, tg=tg, pb=pb, t0=t0, nt=nt: e.tensor_tensor(tg.ap[:, :nt], pb.ap[:, :nt], gbc.ap[:, t0:t0 + nt], ALU.mult),
                          reads=[pb, gbc], writes=[tg])
                    P.dve(lambda e, sa=sa, tg=tg, gT=gT, j=j, t0=t0, nt=nt: e.tensor_tensor(gT.ap[:, j, t0:t0 + nt], sa.ap[:, :nt], tg.ap[:, :nt], ALU.mult),
                          reads=[sa, tg], writes=[gT.trk[j][bi]])
            for m in range(16):
                po = rr(psO, st["o"])
                for j in range(2):
                    P.mm(po.ap[:, :nt], w2t.ap[:, j, m * 128:(m + 1) * 128], gT.ap[:, j, t0:t0 + nt], j == 0, j == 1,
                         reads=[w2t, gT.trk[j][bi]], writes=[po])
                P.dve(lambda e, po=po, m=m, t0=t0, nt=nt, s=s: e.scalar_tensor_tensor(xT.ap[:, m, t0:t0 + nt], po.ap[:, :nt], mod.ap[:, 80 + m, s:s + 1],
                                                                                 xT.ap[:, m, t0:t0 + nt], ALU.mult, ALU.add),
                      reads=[po, mod, xT.trk[m][bi]], writes=[xT.trk[m][bi]])


def emit_routing(P, logT, gateT, ident, nt_total, PSr):
    with P.scope():
        pt = PSr
        lg = [P.tile([128, 8], F32, f"rt_lg{i}") for i in range(2)]
        mk1 = [P.tile([128, 8], F32, f"rt_m1{i}") for i in range(2)]
        l2 = [P.tile([128, 8], F32, f"rt_l2{i}") for i in range(2)]
        mk2 = [P.tile([128, 8], F32, f"rt_m2{i}") for i in range(2)]
        sc = [P.tile([128, 8], F32, f"rt_sc{i}") for i in range(2)]
        ga = [P.tile([128, 8], F32, f"rt_ga{i}") for i in range(2)]
        for ti in range(nt_total // 128):
            i = ti % 2
            p = pt[i]
            tsl = slice(ti * 128, (ti + 1) * 128)
            P.transpose(p.ap[:, 0:8], logT.ap[:, tsl], ident.ap[:8, :8], reads=[logT, ident], writes=[p])
            P.dve(lambda e, i=i, p=p: e.tensor_copy(lg[i].ap, p.ap[:, 0:8]), reads=[p], writes=[lg[i]])
            P.dve(lambda e, i=i: e.reduce_max(sc[i].ap[:, 0:1], lg[i].ap, AX.X), reads=[lg[i]], writes=[sc[i]])
            P.dve(lambda e, i=i: e.tensor_scalar(mk1[i].ap, lg[i].ap, sc[i].ap[:, 0:1], None, ALU.is_equal), reads=[lg[i], sc[i]], writes=[mk1[i]])
            P.dve(lambda e, i=i: e.scalar_tensor_tensor(l2[i].ap, mk1[i].ap, -1e30, lg[i].ap, ALU.mult, ALU.add), reads=[mk1[i], lg[i]], writes=[l2[i]])
            P.dve(lambda e, i=i: e.reduce_max(sc[i].ap[:, 1:2], l2[i].ap, AX.X), reads=[l2[i]], writes=[sc[i]])
            P.dve(lambda e, i=i: e.tensor_scalar(mk2[i].ap, l2[i].ap, sc[i].ap[:, 1:2], None, ALU.is_equal), reads=[l2[i], sc[i]], writes=[mk2[i]])
            P.dve(lambda e, i=i: e.tensor_tensor(sc[i].ap[:, 2:3], sc[i].ap[:, 0:1], sc[i].ap[:, 1:2], ALU.subtract), reads=[sc[i]], writes=[sc[i]])
            P.act(sc[i].ap[:, 3:4], sc[i].ap[:, 2:3], AF.Sigmoid, reads=[sc[i]], writes=[sc[i]])
            P.act(sc[i].ap[:, 4:5], sc[i].ap[:, 2:3], AF.Sigmoid, reads=[sc[i]], writes=[sc[i]], scale=-1.0)
            P.dve(lambda e, i=i: e.tensor_scalar(ga[i].ap, mk1[i].ap, sc[i].ap[:, 3:4], None, ALU.mult), reads=[mk1[i], sc[i]], writes=[ga[i]])
            P.dve(lambda e, i=i: e.scalar_tensor_tensor(ga[i].ap, mk2[i].ap, sc[i].ap[:, 4:5], ga[i].ap, ALU.mult, ALU.add),
                  reads=[mk2[i], sc[i], ga[i]], writes=[ga[i]])
            P.mm(p.ap[:8, 128:256], ga[i].ap, ident.ap, True, True, reads=[ga[i], ident], writes=[p])
            P.act(gateT.ap[:, tsl], p.ap[:8, 128:256], AF.Copy, reads=[p], writes=[gateT])


def build_kc(layer, NT):
    moe = layer == 1
    nc = bass.Bass("TRN2", target_bir_lowering=False)
    dt = lambda name, shape, dtype=F32, kind="ExternalInput": nc.dram_tensor(name, list(shape), dtype, kind=kind).ap()
    xT_d = dt("xT", [D, NT]); hT_d = dt("hT", [D, NT], BF16); attT_d = dt("attT", [D, NT], BF16); uT_d = dt("uT", [2 * D, NT], BF16)
    mod_d = dt("mod", [128, 96, 2]); gn_d = dt("gn", [128, 16]); ident_d = dt("ident", [128, 128])
    wg_d = dt("wg", [D, 2 * D]); woa_d = dt("woa", [D, D]); wob_d = dt("wob", [2 * D, D]); wout_d = dt("wout", [D, D])
    if moe:
        wr_d = dt("wr", [128, 16, 8])
        h2_d = dt("h2T", [D, NT], BF16, "ExternalOutput"); gate_d = dt("gateT", [8, NT], F32, "ExternalOutput")
    else:
        w1_d = dt("w1", [D, DFF]); w3_d = dt("w3", [D, DFF]); w2_d = dt("w2", [DFF, D])
    out_d = dt("xoutT", [D, NT], F32, "ExternalOutput")
    P = Prog(nc)
    blocks = [(0, 512), (512, 512)] + ([(1024, 64)] if NT > 1024 else [])
    sel = [0, 0, 1]
    view = lambda d: d.rearrange("(k p) t -> p k t", p=128)

    def load_ta(ta, d):
        v = view(d)
        for bi, (t0, nt) in enumerate(ta.blocks):
            P.dma("sp", ta.ap[:, :, t0:t0 + nt], v[:, :, t0:t0 + nt], writes=ta.col(bi))

    mod = P.tile([128, 96, 2], F32, "mod")
    P.dma("sp", mod.ap, mod_d, writes=[mod])
    ones = P.tile([128, 128], BF16, "ones")
    P.dve(lambda e: e.memset(ones.ap, 1.0), writes=[ones])
    ident = P.tile([128, 128], F32, "ident")
    P.dma("sp", ident.ap, ident_d, writes=[ident])
    mrg = TA(P, 16, blocks, BF16, "mrg")
    PS = [P.ptile([128, 512], F32, f"PS{i}") for i in range(8)]
    if True:
        pss = PS[0:4]
        pst = [0]
        with P.scope():
            hT = TA(P, 16, blocks, BF16, "hT")
            load_ta(hT, hT_d)
            ws16 = WStream(P, 16, 256, 2, "ws16")
            with P.scope():
                uT = TA(P, 32, blocks, BF16, "uT")
                load_ta(uT, uT_d)
                ws32 = WStream(P, 32, 256, 2, "ws32")
                def ev_sgb(m, bi, t0, nt, ps):
                    P.act(mrg.ap[:, m, t0:t0 + nt], ps.ap[:, :nt], AF.Sigmoid, reads=[ps], writes=[mrg.trk[m][bi]])
                gemm_fm(P, ws16, wg_d[:, D:2 * D], D, hT, pss, pst, ev_sgb)
                def ev_ob(m, bi, t0, nt, ps):
                    P.dve(lambda e: e.tensor_tensor(mrg.ap[:, m, t0:t0 + nt], ps.ap[:, :nt], mrg.ap[:, m, t0:t0 + nt], ALU.mult),
                          reads=[ps, mrg.trk[m][bi]], writes=[mrg.trk[m][bi]])
                gemm_fm(P, ws32, wob_d, D, uT, pss, pst, ev_ob)
            with P.scope():
                attT = TA(P, 16, blocks, BF16, "attT")
                load_ta(attT, attT_d)
                sga = TA(P, 16, blocks, BF16, "sga")
                tmpa = [P.tile([128, 512], F32, f"tmpa{i}") for i in range(2)]
                tn = [0]
                def ev_sga(m, bi, t0, nt, ps):
                    P.act(sga.ap[:, m, t0:t0 + nt], ps.ap[:, :nt], AF.Sigmoid, reads=[ps], writes=[sga.trk[m][bi]])
                gemm_fm(P, ws16, wg_d[:, 0:D], D, hT, pss, pst, ev_sga)
                def ev_oa(m, bi, t0, nt, ps):
                    tt = rr(tmpa, tn)
                    P.dve(lambda e: e.tensor_tensor(tt.ap[:, :nt], ps.ap[:, :nt], sga.ap[:, m, t0:t0 + nt], ALU.mult),
                          reads=[ps, sga.trk[m][bi]], writes=[tt])
                    P.dve(lambda e: e.tensor_tensor(mrg.ap[:, m, t0:t0 + nt], tt.ap[:, :nt], mrg.ap[:, m, t0:t0 + nt], ALU.add),
                          reads=[tt, mrg.trk[m][bi]], writes=[mrg.trk[m][bi]])
                gemm_fm(P, ws16, woa_d, D, attT, pss, pst, ev_oa)
        xT = TA(P, 16, blocks, F32, "xT")
        load_ta(xT, xT_d)
        ws16b = WStream(P, 16, 256, 2, "ws16b")
        def ev_out(m, bi, t0, nt, ps):
            s = sel[bi]
            P.dve(lambda e: e.scalar_tensor_tensor(xT.ap[:, m, t0:t0 + nt], ps.ap[:, :nt], mod.ap[:, 32 + m, s:s + 1], xT.ap[:, m, t0:t0 + nt],
                                                   ALU.mult, ALU.add), reads=[ps, mod, xT.trk[m][bi]], writes=[xT.trk[m][bi]])
        gemm_fm(P, ws16b, wout_d, D, mrg, pss, pst, ev_out)
    h2T = mrg
    gsc = P.tile([128, 16, 2], F32, "gsc")
    router = None
    if moe:
        logT = P.tile([8, NT], F32, "logT")
        gateT = P.tile([8, NT], F32, "gateT")
    with P.scope():
        emit_gsc(P, gsc, mod, gn_d, 4)
        npss = PS[4:6]
        if moe:
            wr = P.tile([128, 16, 8], F32, "wr")
            P.dma("sp", wr.ap, wr_d, writes=[wr])
            psr = PS[6]
            router = (wr, psr, logT)
        emit_norm_mod(P, xT, h2T, sel, gsc, mod, 3, ones, npss, router)
    if moe:
        emit_routing(P, logT, gateT, ident, NT, PS[0:2])
    outs = []
    ov = view(out_d)
    if moe:
        hv2 = view(h2_d)
        for bi, (t0, nt) in enumerate(blocks):
            outs.append(P.dma("sp", hv2[:, :, t0:t0 + nt], h2T.ap[:, :, t0:t0 + nt], reads=h2T.col(bi)))
        outs.append(P.dma("sp", gate_d, gateT.ap, reads=[gateT]))
    else:
        with P.scope():
            ws1 = WStream(P, 16, 256, 2, "w1s")
            ws3 = WStream(P, 16, 256, 2, "w3s")
            ws2 = WStream(P, 2, 2048, 2, "w2s")
            gTs = [TA(P, 2, blocks, BF16, f"gT{i}") for i in range(2)]
            sa_t = [P.tile([128, 512], F32, f"sa{i}") for i in range(2)]
            tg_t = [P.tile([128, 512], F32, f"tg{i}") for i in range(2)]
            st = {k: [0] for k in ("g", "ab", "sa", "tg", "o")}
            emit_ffn(P, xT, h2T, sel, mod, w1_d, w3_d, w2_d, DFF, ws1, ws3, ws2, gTs, PS[0:4], PS[4:7], sa_t, tg_t, st)
    for bi, (t0, nt) in enumerate(blocks):
        outs.append(P.dma("sp", ov[:, :, t0:t0 + nt], xT.ap[:, :, t0:t0 + nt], reads=xT.col(bi)))
    P.wait_all("sp", outs)
    P.emit()
    return nc, P


def build_ke():
    nc = bass.Bass("TRN2", target_bir_lowering=False)
    dt = lambda name, shape, dtype=F32, kind="ExternalInput": nc.dram_tensor(name, list(shape), dtype, kind=kind).ap()
    h2_d = dt("h2T", [D, 8192], BF16); gbc_d = dt("gbc", [128, 8192]); mod_d = dt("mod", [128, 96, 2])
    w1_d = dt("w1", [D, DFFE]); w3_d = dt("w3", [D, DFFE]); w2_d = dt("w2", [DFFE, D])
    part_d = dt("part", [D, 8192], F32, "ExternalOutput")
    P = Prog(nc)
    PS = [P.ptile([128, 512], F32, f"PS{i}") for i in range(8)]
    blocks = [(0, 512), (512, 512)]
    mod = P.tile([128, 96, 2], F32, "mod")
    P.dma("sp", mod.ap, mod_d, writes=[mod])
    ws1 = WStream(P, 16, 256, 2, "w1s"); ws3 = WStream(P, 16, 256, 2, "w3s"); ws2 = WStream(P, 2, 2048, 2, "w2s")
    gTs = [TA(P, 2, blocks, BF16, f"gT{i}") for i in range(2)]
    sa_t = [P.tile([128, 512], F32, f"sa{i}") for i in range(2)]
    tg_t = [P.tile([128, 512], F32, f"tg{i}") for i in range(2)]
    st = {k: [0] for k in ("g", "ab", "sa", "tg", "o")}
    xTs = [TA(P, 16, blocks, F32, f"acc{i}") for i in range(1)]
    h2s = [TA(P, 16, blocks, BF16, f"h2_{i}") for i in range(1)]
    gbs = [P.tile([128, 1024], F32, f"gb{i}") for i in range(2)]
    hv = h2_d.rearrange("(k p) t -> p k t", p=128); pv = part_d.rearrange("(k p) t -> p k t", p=128)
    outs = []
    for c in range(8):
        xT = xTs[0]; h2T = h2s[0]; gbc = gbs[c % 2]
        c0 = c * 1024
        for bi, (t0, nt) in enumerate(blocks):
            P.dma("sp", h2T.ap[:, :, t0:t0 + nt], hv[:, :, c0 + t0:c0 + t0 + nt], writes=h2T.col(bi))
            for k in range(16):
                P.dve(lambda e, xT=xT, k=k, t0=t0, nt=nt: e.memset(xT.ap[:, k, t0:t0 + nt], 0.0), writes=[xT.trk[k][bi]])
        P.dma("sp", gbc.ap, gbc_d[:, c0:c0 + 1024], writes=[gbc])
        s = 0 if c < 4 else 1
        emit_ffn(P, xT, h2T, [s, s], mod, w1_d, w3_d, w2_d, DFFE, ws1, ws3, ws2, gTs, PS[0:4], PS[4:7], sa_t, tg_t, st, gbc=gbc)
        for bi, (t0, nt) in enumerate(blocks):
            outs.append(P.dma("sp", pv[:, :, c0 + t0:c0 + t0 + nt], xT.ap[:, :, t0:t0 + nt], reads=xT.col(bi)))
    P.wait_all("sp", outs)
    P.emit()
    return nc, P


def build_kf():
    nc = bass.Bass("TRN2", target_bir_lowering=False)
    dt = lambda name, shape, dtype=F32, kind="ExternalInput": nc.dram_tensor(name, list(shape), dtype, kind=kind).ap()
    xT_d = dt("xT", [D, 1024]); parts_d = dt("parts", [8, D, 1024]); fn_d = dt("fn", [128, 16])
    out_d = dt("outT", [D, 1024], F32, "ExternalOutput")
    P = Prog(nc)
    PS = [P.ptile([128, 512], F32, f"PS{i}") for i in range(2)]
    blocks = [(0, 512), (512, 512)]
    ones = P.tile([128, 128], BF16, "ones")
    P.dve(lambda e: e.memset(ones.ap, 1.0), writes=[ones])
    fn = P.tile([128, 16], F32, "fn")
    P.dma("sp", fn.ap, fn_d, writes=[fn])
    xT = TA(P, 16, blocks, F32, "xT"); oT = TA(P, 16, blocks, F32, "oT")
    xv = xT_d.rearrange("(k p) t -> p k t", p=128); ov = out_d.rearrange("(k p) t -> p k t", p=128)
    for bi, (t0, nt) in enumerate(blocks):
        P.dma("sp", xT.ap[:, :, t0:t0 + nt], xv[:, :, t0:t0 + nt], writes=xT.col(bi))
    pb = [P.tile([128, 8, 512], F32, f"pb{i}") for i in range(2)]
    n = 0
    for ex in range(8):
        pvw = parts_d[ex].rearrange("(k p) t -> p k t", p=128)
        for bi, (t0, nt) in enumerate(blocks):
            for hf in range(2):
                t = pb[n % 2]; n += 1
                P.dma("sp", t.ap, pvw[:, hf * 8:(hf + 1) * 8, t0:t0 + nt], writes=[t])
                for k8 in range(8):
                    k = hf * 8 + k8
                    P.dve(lambda e, t=t, k=k, k8=k8, t0=t0, nt=nt: e.tensor_tensor(xT.ap[:, k, t0:t0 + nt], xT.ap[:, k, t0:t0 + nt], t.ap[:, k8, :], ALU.add),
                          reads=[t, xT.trk[k][bi]], writes=[xT.trk[k][bi]])
    emit_norm_mod(P, xT, oT, [0, 0], None, fn, 0, ones, PS)
    outs = []
    for bi, (t0, nt) in enumerate(blocks):
        outs.append(P.dma("sp", ov[:, :, t0:t0 + nt], oT.ap[:, :, t0:t0 + nt], reads=oT.col(bi)))
    P.wait_all("sp", outs)
    P.emit()
    return nc, P
NTOK = 8704
WS_COLS = 1288


def build_ssd():
    nc = bass.Bass("TRN2", target_bir_lowering=False)
    dt_ = lambda name, shape, dtype=F32, kind="ExternalInput": nc.dram_tensor(name, list(shape), dtype, kind=kind).ap()
    hT_d = dt_("hT", [D, NTOK], BF16)
    w_d = dt_("wssd", [D, WS_COLS])
    cw_d = dt_("convw", [128, 6, 5]); cb_d = dt_("convb", [128, 6])
    dtb_d = dt_("dtb", [128, 8]); alog_d = dt_("alog", [128, 8]); dsk_d = dt_("dsk", [128, 2, 8])
    gain_d = dt_("gain", [128, 512])
    yprev_d = dt_("yprev", [NTOK, 512])
    ident_d = dt_("ident", [128, 128]); mU_d = dt_("mU", [128, 128]); mL_d = dt_("mL", [128, 128]); mF_d = dt_("mF", [128, 128])
    y_d = dt_("y", [NTOK, 512], F32, "ExternalOutput")
    u_d = dt_("u", [NTOK, 512], BF16, "ExternalOutput")
    P = Prog(nc)
    PS = [P.ptile([128, 512], F32, f"PS{i}") for i in range(8)]
    cst = lambda shape, d, name, dtype=F32: (lambda t: (P.dma("sp", t.ap, d, writes=[t]), t)[1])(P.tile(shape, dtype, name))
    ident = cst([128, 128], ident_d, "ident"); mU = cst([128, 128], mU_d, "mU"); mL = cst([128, 128], mL_d, "mL"); mF = cst([128, 128], mF_d, "mF")
    cw = cst([128, 6, 5], cw_d, "cw"); cb = cst([128, 6], cb_d, "cb"); dtb = cst([128, 8], dtb_d, "dtb")
    alog = cst([128, 8], alog_d, "alog"); dsk = cst([128, 2, 8], dsk_d, "dsk"); gain = cst([128, 512], gain_d, "gain")
    onesf = P.tile([128, 128], F32, "onesf")
    P.dve(lambda e: e.memset(onesf.ap, 1.0), writes=[onesf])
    A = P.tile([128, 8], F32, "A")
    P.act(A.ap, alog.ap, AF.Exp, reads=[alog], writes=[A])
    P.dve(lambda e: e.tensor_scalar(A.ap, A.ap, -1.0, None, ALU.mult), reads=[A], writes=[A])
    dsum = P.tile([128, 8], F32, "dsum")
    P.dve(lambda e: e.tensor_tensor(dsum.ap, dsk.ap[:, 0, :], dsk.ap[:, 1, :], ALU.add), reads=[dsk], writes=[dsum])
    epst = P.tile([128, 1], F32, "epst")
    P.dve(lambda e: e.memset(epst.ap, EPS), writes=[epst])
    w = P.tile([128, 16, WS_COLS], BF16, "wssd")
    P.dma("pool", w.ap, w_d.rearrange("(k p) n -> p k n", p=128), writes=[w])
    hb = [P.tile([128, 16, 260], BF16, f"hb{i}") for i in range(2)]
    xsT = [P.tile([128, 4, 256], F32, f"xsT{i}") for i in range(2)]
    BTf = [P.tile([128, 256], F32, f"BTf{i}") for i in range(2)]
    BTb = [P.tile([128, 256], BF16, f"BTb{i}") for i in range(2)]
    CTb = [P.tile([128, 256], BF16, f"CTb{i}") for i in range(2)]
    acc = [P.tile([128, 256], F32, f"acc{i}") for i in range(2)]
    S = P.tile([128, 512], F32, "S"); Sb = P.tile([128, 512], BF16, "Sb")
    mk = lambda shape, dtype, name, n=2: [P.tile(shape, dtype, f"{name}{i}") for i in range(n)]
    zs = mk([128, 512], F32, "zs"); xs_sb = mk([128, 512], F32, "xs_sb"); xc = mk([128, 512], BF16, "xc"); xw = mk([128, 512], BF16, "xw")
    Btok = mk([128, 128], BF16, "Btok"); dta = mk([128, 16], F32, "dta"); ex = mk([128, 24], F32, "ex")
    cbm = mk([128, 128], F32, "cbm"); R = mk([128, 4, 128], F32, "R"); dec = mk([128, 4, 128], F32, "dec"); Mt = mk([128, 8, 128], BF16, "Mt")
    yo = mk([128, 512], F32, "yo"); ysb = mk([128, 512], F32, "ysb"); ypv = mk([128, 512], F32, "ypv"); ug = mk([128, 512], F32, "ug")
    usq = mk([128, 512], F32, "usq"); ss = mk([128, 2], F32, "ss"); uo = mk([128, 512], BF16, "uo"); tdt = mk([128, 8], F32, "tdt")
    hview = hT_d.rearrange("(k p) t -> p k t", p=128)
    outs = []
    ci = 0
    for blk in range(NTOK // 256):
        t0 = blk * 256
        bseq = blk % 17
        left0 = bseq in (0, 1)
        right0 = bseq in (0, 16)
        h = hb[blk % 2]
        lo = t0 - (0 if left0 else 2); hi = t0 + 256 + (0 if right0 else 2)
        if left0:
            P.dve(lambda e, h=h: e.memset(h.ap[:, :, 0:2], 0.0), writes=[h])
        if right0:
            P.dve(lambda e, h=h: e.memset(h.ap[:, :, 258:260], 0.0), writes=[h])
        P.dma("sp", h.ap[:, :, (2 if left0 else 0):(258 if right0 else 260)], hview[:, :, lo:hi], writes=[h])
        if bseq == 0:
            P.dve(lambda e: e.memset(S.ap, 0.0), writes=[S])
            P.dve(lambda e: e.memset(Sb.ap, 0.0), writes=[Sb])
        b2 = blk % 2
        for ch in range(6):
            ps = PS[ch % 2]
            c0 = 512 + ch * 128
            for k in range(16):
                P.mm(ps.ap[:, 0:260], w.ap[:, k, c0:c0 + 128], h.ap[:, k, :], k == 0, k == 15, reads=[w, h], writes=[ps])
            a = acc[ch % 2]
            P.dve(lambda e, a=a, ps=ps, ch=ch: e.tensor_scalar(a.ap, ps.ap[:, 0:256], cw.ap[:, ch, 0:1], cb.ap[:, ch:ch + 1], ALU.mult, ALU.add),
                  reads=[ps, cw, cb], writes=[a])
            for j in range(1, 5):
                P.dve(lambda e, a=a, ps=ps, ch=ch, j=j: e.scalar_tensor_tensor(a.ap, ps.ap[:, j:j + 256], cw.ap[:, ch, j:j + 1], a.ap, ALU.mult, ALU.add),
                      reads=[ps, cw, a], writes=[a])
            if ch < 4:
                P.act(xsT[b2].ap[:, ch, :], a.ap, AF.Silu, reads=[a], writes=[xsT[b2]])
            elif ch == 4:
                P.act(BTf[b2].ap, a.ap, AF.Silu, reads=[a], writes=[BTf[b2]])
                P.dve(lambda e, b2=b2: e.tensor_copy(BTb[b2].ap, BTf[b2].ap), reads=[BTf[b2]], writes=[BTb[b2]])
            else:
                P.act(CTb[b2].ap, a.ap, AF.Silu, reads=[a], writes=[CTb[b2]])
        for c in range(2):
            i = ci % 2
            ci += 1
            cs = slice(c * 128, (c + 1) * 128)
            hs = slice(2 + c * 128, 2 + (c + 1) * 128)
            tok0 = t0 + c * 128
            P.dma("sp", ypv[i].ap, yprev_d[tok0:tok0 + 128, :], writes=[ypv[i]])
            pz = PS[2]
            for k in range(16):
                P.mm(pz.ap, h.ap[:, k, hs], w.ap[:, k, 0:512], k == 0, k == 15, reads=[w, h], writes=[pz])
            P.act(zs[i].ap, pz.ap, AF.Silu, reads=[pz], writes=[zs[i]])
            pd = PS[3]
            for k in range(16):
                P.mm(pd.ap[:, 0:8], h.ap[:, k, hs], w.ap[:, k, 1280:1288], k == 0, k == 15, reads=[w, h], writes=[pd])
            P.dve(lambda e, i=i, pd=pd: e.tensor_tensor(tdt[i].ap, pd.ap[:, 0:8], dtb.ap, ALU.add), reads=[pd, dtb], writes=[tdt[i]])
            P.act(tdt[i].ap, tdt[i].ap, AF.Exp, reads=[tdt[i]], writes=[tdt[i]])
            P.act(dta[i].ap[:, 0:8], tdt[i].ap, AF.Ln, reads=[tdt[i]], writes=[dta[i]], bias=1.0)
            P.dve(lambda e, i=i: e.tensor_tensor(dta[i].ap[:, 8:16], dta[i].ap[:, 0:8], A.ap, ALU.mult), reads=[dta[i], A], writes=[dta[i]])
            pc = PS[3]
            P.mm(pc.ap[:, 16:24], mU.ap, dta[i].ap[:, 8:16], True, True, reads=[mU, dta[i]], writes=[pc])
            P.mm(pc.ap[:, 24:32], mL.ap, dta[i].ap[:, 8:16], True, True, reads=[mL, dta[i]], writes=[pc])
            P.mm(pc.ap[:, 32:40], onesf.ap, dta[i].ap[:, 8:16], True, True, reads=[onesf, dta[i]], writes=[pc])
            P.act(ex[i].ap, pc.ap[:, 16:40], AF.Exp, reads=[pc], writes=[ex[i]])
            px = PS[4]
            for ch in range(4):
                P.transpose(px.ap[:, ch * 128:(ch + 1) * 128], xsT[b2].ap[:, ch, cs], ident.ap, reads=[xsT[b2], ident], writes=[px])
            P.act(xs_sb[i].ap, px.ap, AF.Copy, reads=[px], writes=[xs_sb[i]])
            bc = lambda t8: t8.unsqueeze(2).to_broadcast([128, 8, 64])
            v3 = lambda ap: ap.rearrange("p (e q) -> p e q", q=64)
            P.dve(lambda e, i=i: e.tensor_tensor(v3(xc[i].ap), v3(xs_sb[i].ap), bc(dta[i].ap[:, 0:8]), ALU.mult), reads=[xs_sb[i], dta[i]], writes=[xc[i]])
            P.dve(lambda e, i=i: e.tensor_tensor(v3(xw[i].ap), v3(xc[i].ap), bc(ex[i].ap[:, 0:8]), ALU.mult), reads=[xc[i], ex[i]], writes=[xw[i]])
            pb = PS[5]
            P.transpose(pb.ap[:, 0:128], BTf[b2].ap[:, cs], ident.ap, reads=[BTf[b2], ident], writes=[pb])
            P.act(Btok[i].ap, pb.ap[:, 0:128], AF.Copy, reads=[pb], writes=[Btok[i]])
            P.mm(pb.ap[:, 128:256], BTb[b2].ap[:, cs], CTb[b2].ap[:, cs], True, True, reads=[BTb[b2], CTb[b2]], writes=[pb])
            P.dve(lambda e, i=i, pb=pb: e.tensor_tensor(cbm[i].ap, pb.ap[:, 128:256], mF.ap, ALU.mult), reads=[pb, mF], writes=[cbm[i]])
            for hh in range(2):
                r = R[hh]; d = dec[hh]
                P.dve(lambda e, r=r, i=i, hh=hh: e.tensor_tensor(r.ap, mL.ap.unsqueeze(1).to_broadcast([128, 4, 128]),
                                                                 dta[i].ap[:, 8 + hh * 4:12 + hh * 4].unsqueeze(2).to_broadcast([128, 4, 128]), ALU.mult),
                      reads=[mL, dta[i]], writes=[r])
                pg = PS[6 + hh]
                P.mm(pg.ap, mU.ap, r.ap.rearrange("p e l -> p (e l)"), True, True, reads=[mU, r], writes=[pg])
                P.act(d.ap.rearrange("p e l -> p (e l)"), pg.ap, AF.Exp, reads=[pg], writes=[d])
                P.dve(lambda e, d=d, i=i, hh=hh: e.tensor_tensor(Mt[i].ap[:, hh * 4:(hh + 1) * 4, :], d.ap, cbm[i].ap.unsqueeze(1).to_broadcast([128, 4, 128]), ALU.mult),
                      reads=[d, cbm[i]], writes=[Mt[i]])
            py = PS[0]
            for e8 in range(8):
                P.mm(py.ap[:, e8 * 64:(e8 + 1) * 64], Mt[i].ap[:, e8, :], xc[i].ap[:, e8 * 64:(e8 + 1) * 64], True, True, reads=[Mt[i], xc[i]], writes=[py])
            po = PS[1]
            P.mm(po.ap, CTb[b2].ap[:, cs], Sb.ap, True, True, reads=[CTb[b2], Sb], writes=[po])
            P.dve(lambda e, i=i, po=po: e.tensor_tensor(v3(yo[i].ap), v3(po.ap), bc(ex[i].ap[:, 8:16]), ALU.mult), reads=[po, ex[i]], writes=[yo[i]])
            P.dve(lambda e, i=i, py=py: e.tensor_tensor(ysb[i].ap, py.ap, yo[i].ap, ALU.add), reads=[py, yo[i]], writes=[ysb[i]])
            outs.append(P.dma("sp", y_d[tok0:tok0 + 128, :], ysb[i].ap, reads=[ysb[i]]))
            pst_ = PS[2]
            P.mm(pst_.ap, Btok[i].ap, xw[i].ap, True, True, reads=[Btok[i], xw[i]], writes=[pst_])
            P.dve(lambda e, i=i: e.tensor_tensor(v3(S.ap), v3(S.ap), bc(ex[i].ap[:, 16:24]), ALU.mult), reads=[S, ex[i]], writes=[S])
            P.dve(lambda e, pst_=pst_: e.tensor_tensor(S.ap, S.ap, pst_.ap, ALU.add), reads=[S, pst_], writes=[S])
            P.act(Sb.ap, S.ap, AF.Copy, reads=[S], writes=[Sb])
            P.dve(lambda e, i=i: e.tensor_tensor(v3(ug[i].ap), v3(xs_sb[i].ap), bc(dsum.ap), ALU.mult), reads=[xs_sb[i], dsum], writes=[ug[i]])
            P.dve(lambda e, i=i: e.tensor_tensor(ug[i].ap, ug[i].ap, ysb[i].ap, ALU.add), reads=[ug[i], ysb[i]], writes=[ug[i]])
            P.dve(lambda e, i=i: e.tensor_tensor(ug[i].ap, ug[i].ap, ypv[i].ap, ALU.add), reads=[ug[i], ypv[i]], writes=[ug[i]])
            P.dve(lambda e, i=i: e.tensor_tensor(ug[i].ap, ug[i].ap, zs[i].ap, ALU.mult), reads=[ug[i], zs[i]], writes=[ug[i]])
            P.dve(lambda e, i=i: e.memset(ss[i].ap, 0.0), writes=[ss[i]])
            P.act(usq[i].ap, ug[i].ap, AF.Square, reads=[ug[i]], writes=[usq[i], ss[i]], accum_out=ss[i].ap[:, 0:1])
            P.act(ss[i].ap[:, 1:2], ss[i].ap[:, 0:1], AF.Sqrt, reads=[ss[i], epst], writes=[ss[i]], bias=epst.ap, scale=1.0 / 512)
            P.dve(lambda e, i=i: e.reciprocal(ss[i].ap[:, 1:2], ss[i].ap[:, 1:2]), reads=[ss[i]], writes=[ss[i]])
            P.dve(lambda e, i=i: e.scalar_tensor_tensor(uo[i].ap, ug[i].ap, ss[i].ap[:, 1:2], gain.ap, ALU.mult, ALU.mult), reads=[ug[i], ss[i], gain], writes=[uo[i]])
            outs.append(P.dma("sp", u_d[tok0:tok0 + 128, :], uo[i].ap, reads=[uo[i]]))
    P.wait_all("sp", outs)
    P.emit()
    return nc, P
NKEY = 4352
NQ = 1088
SM_SCALE = 192.0 ** -0.5


def build_att(with_ctx=True):
    nc = bass.Bass("TRN2", target_bir_lowering=False)
    dt_ = lambda name, shape, dtype=F32, kind="ExternalInput": nc.dram_tensor(name, list(shape), dtype, kind=kind).ap()
    hTb_d = dt_("hTb", [D, NKEY], BF16); hTq_d = dt_("hTq", [D, NQ], BF16)
    wkv_d = dt_("wkv", [D, 640]); wq_d = dt_("wq", [D, 512])
    wuq_d = dt_("wuq", [512, 4096]); wukv_d = dt_("wukv", [512, 4096])
    qg_d = dt_("qg", [128, 4]); kvg_d = dt_("kvg", [128, 4])
    cosk_d = dt_("cosk", [64, NKEY]); sink_d = dt_("sink", [64, NKEY]); cosq_d = dt_("cosq", [64, NQ]); sinq_d = dt_("sinq", [64, NQ])
    att_d = dt_("attT", [D, NQ], BF16, "ExternalOutput")
    P = Prog(nc)
    PS = [P.ptile([128, 512], F32, f"PS{i}") for i in range(8)]
    ones = P.tile([128, 128], BF16, "ones")
    P.dve(lambda e: e.memset(ones.ap, 1.0), writes=[ones])
    epst = P.tile([128, 1], F32, "epst")
    P.dve(lambda e: e.memset(epst.ap, EPS), writes=[epst])
    kblocks = [(0, 256)] + [(256 + i * 512, 512) for i in range(8)]
    qblocks = [(0, 512), (512, 512)] + ([(1024, 64)] if with_ctx else [])
    ckvnT = TA(P, 4, kblocks, BF16, "ckvnT")
    krot = TA(P, 1, kblocks, BF16, "krot")
    cqnT = TA(P, 4, qblocks, BF16, "cqnT")
    cosq = P.tile([64, NQ], F32, "cosq"); sinq = P.tile([64, NQ], F32, "sinq")
    P.dma("sp", cosq.ap, cosq_d, writes=[cosq]); P.dma("sp", sinq.ap, sinq_d, writes=[sinq])

    def norm4(cf, nt, gain, out_ta, bi, t0, sq, rs, psn):
        for k in range(4):
            P.act(sq.ap[:, :nt], cf.ap[:, k, :nt], AF.Square, reads=[cf], writes=[sq])
            P.mm(psn.ap[:, :nt], ones.ap, sq.ap[:, :nt], k == 0, k == 3, reads=[ones, sq], writes=[psn])
        P.act(rs.ap[:, :nt], psn.ap[:, :nt], AF.Sqrt, reads=[psn, epst], writes=[rs], bias=epst.ap, scale=1.0 / 512)
        P.dve(lambda e: e.reciprocal(rs.ap[:, :nt], rs.ap[:, :nt]), reads=[rs], writes=[rs])
        for k in range(4):
            P.dve(lambda e, k=k: e.scalar_tensor_tensor(out_ta.ap[:, k, t0:t0 + nt], cf.ap[:, k, :nt], gain.ap[:, k:k + 1], rs.ap[:, :nt], ALU.mult, ALU.mult),
                  reads=[cf, gain, rs], writes=[out_ta.trk[k][bi]])

    with P.scope():
        wkv = P.tile([128, 16, 640], BF16, "wkv"); wq = P.tile([128, 16, 512], BF16, "wq")
        P.dma("pool", wkv.ap, wkv_d.rearrange("(k p) n -> p k n", p=128), writes=[wkv])
        P.dma("pool", wq.ap, wq_d.rearrange("(k p) n -> p k n", p=128), writes=[wq])
        qg = P.tile([128, 4], F32, "qg"); kvg = P.tile([128, 4], F32, "kvg")
        P.dma("sp", qg.ap, qg_d, writes=[qg]); P.dma("sp", kvg.ap, kvg_d, writes=[kvg])
        hb = [P.tile([128, 16, 512], BF16, f"hb{i}") for i in range(2)]
        cf = [P.tile([128, 4, 512], F32, f"cf{i}") for i in range(2)]
        sq = P.tile([128, 512], BF16, "sq"); rs = P.tile([128, 512], F32, "rs")
        ck = [P.tile([64, 512], F32, f"ck{i}") for i in range(2)]; sk = [P.tile([64, 512], F32, f"sk{i}") for i in range(2)]
        ra = P.tile([64, 512], F32, "ra"); rb = P.tile([64, 512], F32, "rb")
        hvb = hTb_d.rearrange("(k p) t -> p k t", p=128)
        hvq = hTq_d.rearrange("(k p) t -> p k t", p=128)
        n = 0
        for bi, (t0, nt) in enumerate(kblocks):
            h = hb[n % 2]; c = cf[n % 2]; n += 1
            P.dma("sp", h.ap[:, :, :nt], hvb[:, :, t0:t0 + nt], writes=[h])
            P.dma("sp", ck[bi % 2].ap[:, :nt], cosk_d[:, t0:t0 + nt], writes=[ck[bi % 2]])
            P.dma("sp", sk[bi % 2].ap[:, :nt], sink_d[:, t0:t0 + nt], writes=[sk[bi % 2]])
            for m in range(4):
                ps = PS[m % 2]
                for k in range(16):
                    P.mm(ps.ap[:, :nt], wkv.ap[:, k, m * 128:(m + 1) * 128], h.ap[:, k, :nt], k == 0, k == 15, reads=[wkv, h], writes=[ps])
                P.act(c.ap[:, m, :nt], ps.ap[:, :nt], AF.Copy, reads=[ps], writes=[c])
            pa1, pb1 = PS[2], PS[3]
            for k in range(16):
                P.mm(pa1.ap[:64, :nt], wkv.ap[:, k, 512:576], h.ap[:, k, :nt], k == 0, k == 15, reads=[wkv, h], writes=[pa1])
            for k in range(16):
                P.mm(pb1.ap[:64, :nt], wkv.ap[:, k, 576:640], h.ap[:, k, :nt], k == 0, k == 15, reads=[wkv, h], writes=[pb1])
            P.dve(lambda e, nt=nt, bi=bi: e.tensor_tensor(ra.ap[:, :nt], pa1.ap[:64, :nt], ck[bi % 2].ap[:, :nt], ALU.mult), reads=[pa1, ck[bi % 2]], writes=[ra])
            P.dve(lambda e, nt=nt, bi=bi: e.tensor_tensor(rb.ap[:, :nt], pb1.ap[:64, :nt], sk[bi % 2].ap[:, :nt], ALU.mult), reads=[pb1, sk[bi % 2]], writes=[rb])
            P.dve(lambda e, nt=nt, t0=t0: e.tensor_tensor(krot.ap[:64, 0, t0:t0 + nt], ra.ap[:, :nt], rb.ap[:, :nt], ALU.add), reads=[ra, rb], writes=[krot.trk[0][bi]])
            norm4(c, nt, kvg, ckvnT, bi, t0, sq, rs, PS[4])
        for bi, (t0, nt) in enumerate(qblocks):
            h = hb[n % 2]; c = cf[n % 2]; n += 1
            P.dma("sp", h.ap[:, :, :nt], hvq[:, :, t0:t0 + nt], writes=[h])
            for m in range(4):
                ps = PS[m % 2]
                for k in range(16):
                    P.mm(ps.ap[:, :nt], wq.ap[:, k, m * 128:(m + 1) * 128], h.ap[:, k, :nt], k == 0, k == 15, reads=[wq, h], writes=[ps])
                P.act(c.ap[:, m, :nt], ps.ap[:, :nt], AF.Copy, reads=[ps], writes=[c])
            norm4(c, nt, qg, cqnT, bi, t0, sq, rs, PS[4])
    wuq = P.tile([128, 4, 4096], BF16, "wuq"); wukv = P.tile([128, 4, 4096], BF16, "wukv")
    P.dma("pool", wuq.ap, wuq_d.rearrange("(k p) n -> p k n", p=128), writes=[wuq])
    P.dma("pool", wukv.ap, wukv_d.rearrange("(k p) n -> p k n", p=128), writes=[wukv])
    KnT = [TA(P, 1, kblocks, BF16, f"KnT{i}") for i in range(2)]
    Vt = [P.tile([128, 34, 128], BF16, f"Vt{i}") for i in range(2)]
    QnT = [TA(P, 1, qblocks, BF16, f"QnT{i}") for i in range(2)]
    qrot = [TA(P, 1, qblocks, BF16, f"qrot{i}") for i in range(2)]
    ra2 = P.tile([64, 512], F32, "ra2"); rb2 = P.tile([64, 512], F32, "rb2")
    pT = [P.tile([128, 512], BF16, f"pT{i}") for i in range(3)]
    rec = [P.tile([128, 512], F32, f"rec{i}") for i in range(2)]
    ao = [P.tile([128, NQ], BF16, f"ao{i}") for i in range(2)]
    outs = []
    av = att_d.rearrange("(h p) t -> p h t", p=128)
    pn = [0]
    for hd in range(16):
        i = hd % 2
        c0 = hd * 256
        for bi, (t0, nt) in enumerate(kblocks):
            ps = PS[bi % 2]
            for k in range(4):
                P.mm(ps.ap[:, :nt], wukv.ap[:, k, c0:c0 + 128], ckvnT.ap[:, k, t0:t0 + nt], k == 0, k == 3, reads=[wukv, ckvnT.trk[k][bi]], writes=[ps])
            P.act(KnT[i].ap[:, 0, t0:t0 + nt], ps.ap[:, :nt], AF.Copy, reads=[ps], writes=[KnT[i].trk[0][bi]])
        for g4 in range(9):
            ps = PS[2 + g4 % 2]
            nkt = min(4, 34 - g4 * 4)
            for j in range(nkt):
                kt = g4 * 4 + j
                for k in range(4):
                    P.mm(ps.ap[:, j * 128:(j + 1) * 128], ckvnT.ap[:, k, kt * 128:(kt + 1) * 128], wukv.ap[:, k, c0 + 128:c0 + 256], k == 0, k == 3,
                         reads=[wukv] + ckvnT.col((kt * 128 + 256) // 512 if kt >= 2 else 0)[k:k + 1], writes=[ps])
            P.dve(lambda e, ps=ps, g4=g4, nkt=nkt, i=i: e.tensor_copy(Vt[i].ap[:, g4 * 4:g4 * 4 + nkt, :], ps.ap[:, :nkt * 128].rearrange("p (j d) -> p j d", d=128)),
                  reads=[ps], writes=[Vt[i]])
        for bi, (t0, nt) in enumerate(qblocks):
            ps = PS[4]
            for k in range(4):
                P.mm(ps.ap[:, :nt], wuq.ap[:, k, c0:c0 + 128], cqnT.ap[:, k, t0:t0 + nt], k == 0, k == 3, reads=[wuq, cqnT.trk[k][bi]], writes=[ps])
            P.act(QnT[i].ap[:, 0, t0:t0 + nt], ps.ap[:, :nt], AF.Copy, reads=[ps], writes=[QnT[i].trk[0][bi]])
            pa, pb = PS[5], PS[6]
            for k in range(4):
                P.mm(pa.ap[:64, :nt], wuq.ap[:, k, c0 + 128:c0 + 192], cqnT.ap[:, k, t0:t0 + nt], k == 0, k == 3, reads=[wuq, cqnT.trk[k][bi]], writes=[pa])
            for k in range(4):
                P.mm(pb.ap[:64, :nt], wuq.ap[:, k, c0 + 192:c0 + 256], cqnT.ap[:, k, t0:t0 + nt], k == 0, k == 3, reads=[wuq, cqnT.trk[k][bi]], writes=[pb])
            P.dve(lambda e, nt=nt, t0=t0: e.tensor_tensor(ra2.ap[:, :nt], pa.ap[:64, :nt], cosq.ap[:, t0:t0 + nt], ALU.mult), reads=[pa, cosq], writes=[ra2])
            P.dve(lambda e, nt=nt, t0=t0: e.tensor_tensor(rb2.ap[:, :nt], pb.ap[:64, :nt], sinq.ap[:, t0:t0 + nt], ALU.mult), reads=[pb, sinq], writes=[rb2])
            P.dve(lambda e, nt=nt, t0=t0, i=i: e.tensor_tensor(qrot[i].ap[:64, 0, t0:t0 + nt], ra2.ap[:, :nt], rb2.ap[:, :nt], ALU.add), reads=[ra2, rb2], writes=[qrot[i].trk[0][bi]])
        for bi, (t0, nt) in enumerate(qblocks):
            kts = range(34) if bi < 2 else range(2)
            pO, pD = PS[bi % 2], PS[2 + bi % 2]
            kts = list(kts)
            def emit_S(kt):
                kb = (kt * 128 + 256) // 512 if kt >= 2 else 0
                pS = PS[4 + pn[0] % 3]
                pt = pT[pn[0] % 3]
                pn[0] += 1
                ksl = slice(kt * 128, (kt + 1) * 128)
                P.mm(pS.ap[:, :nt], KnT[i].ap[:, 0, ksl], QnT[i].ap[:, 0, t0:t0 + nt], True, False, reads=[KnT[i].trk[0][kb], QnT[i].trk[0][bi]], writes=[pS])
                P.mm(pS.ap[:, :nt], krot.ap[:64, 0, ksl], qrot[i].ap[:64, 0, t0:t0 + nt], False, True, reads=[krot.trk[0][kb], qrot[i].trk[0][bi]], writes=[pS])
                P.act(pt.ap[:, :nt], pS.ap[:, :nt], AF.Exp, reads=[pS], writes=[pt], scale=SM_SCALE)
                return pt
            pend = emit_S(kts[0])
            for n_, kt in enumerate(kts):
                pt = pend
                if n_ + 1 < len(kts):
                    pend = emit_S(kts[n_ + 1])
                P.mm(pO.ap[:, :nt], Vt[i].ap[:, kt, :], pt.ap[:, :nt], kt == kts[0], kt == kts[-1], reads=[Vt[i], pt], writes=[pO])
                P.mm(pD.ap[:, :nt], ones.ap, pt.ap[:, :nt], kt == kts[0], kt == kts[-1], reads=[ones, pt], writes=[pD])
            r = rec[bi % 2]
            P.dve(lambda e, r=r, pD=pD, nt=nt: e.reciprocal(r.ap[:, :nt], pD.ap[:, :nt]), reads=[pD], writes=[r])
            P.dve(lambda e, r=r, pO=pO, nt=nt, t0=t0, i=i: e.tensor_tensor(ao[i].ap[:, t0:t0 + nt], pO.ap[:, :nt], r.ap[:, :nt], ALU.mult), reads=[pO, r], writes=[ao[i]])
        nq = qblocks[-1][0] + qblocks[-1][1]
        outs.append(P.dma("sp", av[:, hd, :nq], ao[i].ap[:, :nq], reads=[ao[i]]))
    P.wait_all("sp", outs)
    P.emit()
    return nc, P
def rope_tables(q):
    n = 4096
    rows = n // 64
    row = np.repeat(np.arange(rows, dtype=np.float32), 64)
    col = np.tile(np.arange(64, dtype=np.float32), rows)
    inv = (np.float32(10000.0) ** (-np.arange(0, 32, 2, dtype=np.float32) / np.float32(32))).astype(np.float32)
    ang = np.stack([row[:, None] * inv, col[:, None] * inv], axis=1)
    cos = np.cos(ang).astype(np.float32); sin = np.sin(ang).astype(np.float32)
    C = np.zeros((64, n), np.float32); S = np.zeros((64, n), np.float32)
    for ax in range(2):
        for half in range(2):
            r = slice(ax * 32 + half * 16, ax * 32 + half * 16 + 16)
            C[r] = cos[:, ax, :].T
            S[r] = (-sin[:, ax, :].T) if half == 0 else sin[:, ax, :].T
    onesc = np.ones((64, 256), np.float32); zc = np.zeros((64, 256), np.float32)
    cosk = np.concatenate([onesc, C], 1); sink = np.concatenate([zc, S], 1)
    cosq = np.concatenate([C[:, q * 1024:(q + 1) * 1024], onesc[:, :64]], 1); sinq = np.concatenate([S[:, q * 1024:(q + 1) * 1024], zc[:, :64]], 1)
    return {"cosk": cosk, "sink": sink, "cosq": np.ascontiguousarray(cosq), "sinq": np.ascontiguousarray(sinq)}


ROPE_PERM = np.concatenate([np.arange(16, 32), np.arange(0, 16), np.arange(48, 64), np.arange(32, 48)])


def att_weights(w_in, w_uq, w_ukv, q_norm, kv_norm):
    wkv = np.concatenate([w_in[:, 512:1088], w_in[:, 1024:1088][:, ROPE_PERM]], 1)
    u = w_uq.reshape(512, 16, 192)
    wuq = np.concatenate([u, u[:, :, 128:][:, :, ROPE_PERM]], 2).reshape(512, 4096)
    return {"wkv": np.ascontiguousarray(wkv), "wq": np.ascontiguousarray(w_in[:, 0:512]), "wuq": np.ascontiguousarray(wuq), "wukv": w_ukv,
            "qg": np.ascontiguousarray(q_norm.reshape(4, 128).T), "kvg": np.ascontiguousarray(kv_norm.reshape(4, 128).T)}
def build_ka(NT):
    nc = bass.Bass("TRN2", target_bir_lowering=False)
    xT_d = nc.dram_tensor("xT", [D, NT], F32, kind="ExternalInput").ap()
    cT_d = nc.dram_tensor("cT", [128, 16, 2], F32, kind="ExternalInput").ap()
    wada_d = nc.dram_tensor("wada", [D, 6 * D], F32, kind="ExternalInput").ap()
    bada_d = nc.dram_tensor("bada", [128, 96], F32, kind="ExternalInput").ap()
    gn_d = nc.dram_tensor("gn", [128, 16], F32, kind="ExternalInput").ap()
    mod_d = nc.dram_tensor("mod", [128, 96, 2], F32, kind="ExternalOutput").ap()
    hT_d = nc.dram_tensor("hT", [D, NT], BF16, kind="ExternalOutput").ap()
    P = Prog(nc)
    mod = P.tile([128, 96, 2], F32, "mod")
    emit_adaln(P, cT_d, wada_d, bada_d, mod)
    dm = P.dma("sp", mod_d, mod.ap, reads=[mod])
    gsc = P.tile([128, 16, 2], F32, "gsc")
    emit_gsc(P, gsc, mod, gn_d, 1)
    ones = P.tile([128, 128], BF16, "ones")
    P.dve(lambda e: e.memset(ones.ap, 1.0), writes=[ones])
    blocks = [(0, 512), (512, 512)] + ([(1024, 64)] if NT > 1024 else [])
    xT = TA(P, 16, blocks, F32, "xT"); hT = TA(P, 16, blocks, BF16, "hT")
    xv = xT_d.rearrange("(k p) t -> p k t", p=128); hv = hT_d.rearrange("(k p) t -> p k t", p=128)
    for bi, (t0, nt) in enumerate(blocks):
        P.dma("sp", xT.ap[:, :, t0:t0 + nt], xv[:, :, t0:t0 + nt], writes=xT.col(bi))
    pss = [P.ptile([128, 512], F32, f"n_ps{i}") for i in range(2)]
    emit_norm_mod(P, xT, hT, [0, 0, 1], gsc, mod, 0, ones, pss)
    outs = [dm]
    for bi, (t0, nt) in enumerate(blocks):
        outs.append(P.dma("sp", hv[:, :, t0:t0 + nt], hT.ap[:, :, t0:t0 + nt], reads=hT.col(bi)))
    P.wait_all("sp", outs)
    P.emit()
    return nc


def _run(nc, in_maps):
    res = run_bass_kernel_spmd(nc, in_maps, core_ids=list(range(8)))
    return res.results


def _ssd_consts(l, g, d, w_in, conv_w, conv_b, dt_bias, a_log, d_skip, ssm_norm):
    cols = np.concatenate([np.arange(1088 + g * 512, 1088 + (g + 1) * 512), np.arange(5184 + g * 512, 5184 + (g + 1) * 512),
                           np.arange(9280 + g * 128, 9280 + (g + 1) * 128), np.arange(10304 + g * 128, 10304 + (g + 1) * 128),
                           np.arange(11328 + d * 64 + g * 8, 11328 + d * 64 + g * 8 + 8)])
    chans = np.concatenate([np.arange(g * 512, (g + 1) * 512), 4096 + np.arange(g * 128, (g + 1) * 128), 5120 + np.arange(g * 128, (g + 1) * 128)])
    cw = conv_w[:, chans]
    if d == 1:
        cw = cw[::-1]
    kk = np.arange(128)
    hs = slice(8 * g, 8 * g + 8)
    return {"wssd": np.ascontiguousarray(w_in[:, cols]), "convw": np.ascontiguousarray(cw.reshape(5, 6, 128).transpose(2, 1, 0)),
            "convb": np.ascontiguousarray(conv_b[chans].reshape(6, 128).T),
            "dtb": np.broadcast_to(dt_bias[d, hs], (128, 8)).copy(), "alog": np.broadcast_to(a_log[d, hs], (128, 8)).copy(),
            "dsk": np.broadcast_to(d_skip[:, hs], (128, 2, 8)).copy(), "gain": np.broadcast_to(ssm_norm[g * 512:(g + 1) * 512], (128, 512)).copy(),
            "ident": np.eye(128, dtype=np.float32), "mU": (kk[:, None] > kk[None, :]).astype(np.float32),
            "mL": (kk[:, None] <= kk[None, :]).astype(np.float32), "mF": (kk[None, :] >= kk[:, None]).astype(np.float32)}


def _rev(y):
    out = np.empty_like(y)
    for b in range(2):
        o = b * 4352
        out[o:o + 256] = y[o:o + 256][::-1]
        out[o + 256:o + 4352] = y[o + 256:o + 4352][::-1]
    return out


def kernel(x, c, ctx, c_ctx, norm_mix, norm_ffn, w_ada, b_ada, w_in, q_norm, w_uq, kv_norm, w_ukv, conv_w, conv_b, a_log, dt_bias,
           d_skip, ssm_norm, w_oa, w_ob, w_out, w1_dense, w3_dense, w2_dense, w_router, w1_moe, w3_moe, w2_moe, final_norm):
    A = lambda a: np.ascontiguousarray(np.asarray(a))
    x = np.asarray(x, np.float32); ctx = np.asarray(ctx, np.float32)
    ident = np.eye(128, dtype=np.float32)
    progs = {}

    def prog(name, fn, *a):
        if name not in progs:
            r = fn(*a)
            progs[name] = r[0] if isinstance(r, tuple) else r
        return progs[name]

    out = None
    for l in range(2):
        last = l == 1
        ims = []
        for core in range(8):
            b, q = core // 4, core % 4
            xo = np.concatenate([x[b, q * 1024:(q + 1) * 1024], ctx[b, q * 64:(q + 1) * 64]], 0)
            cv = np.stack([np.asarray(c)[b], np.asarray(c_ctx)], 0)
            ims.append({"xT": A(xo.T), "cT": A(cv.reshape(2, 16, 128).transpose(2, 1, 0)), "wada": A(w_ada[l]),
                        "bada": A(np.asarray(b_ada[l]).reshape(96, 128).T), "gn": A(np.asarray(norm_mix[l]).reshape(16, 128).T)})
        ra = _run(prog("ka", build_ka, 1088), ims)
        mods = [r["mod"] for r in ra]
        hTs = [np.asarray(r["hT"]) for r in ra]
        xTs = [im["xT"] for im in ims]
        h_lat = [np.concatenate([hTs[b * 4 + q][:, :1024] for q in range(4)], 1) for b in range(2)]
        h_ctx = [np.concatenate([hTs[b * 4 + q][:, 1024:] for q in range(4)], 1) for b in range(2)]
        aw = att_weights(np.asarray(w_in[l]), np.asarray(w_uq[l]), np.asarray(w_ukv[l]), np.asarray(q_norm[l]), np.asarray(kv_norm[l]))
        ims = []
        for core in range(8):
            b, q = core // 4, core % 4
            m = dict(aw); m.update(rope_tables(q))
            m["hTb"] = A(np.concatenate([h_ctx[b], h_lat[b]], 1)); m["hTq"] = hTs[core]
            ims.append(m)
        rt = _run(prog("att", build_att, True), ims)
        attTs = [np.asarray(r["attT"]) for r in rt]
        hT0 = A(np.concatenate([h_ctx[0], h_lat[0], h_ctx[1], h_lat[1]], 1))
        hT1 = A(np.concatenate([h_ctx[0][:, ::-1], h_lat[0][:, ::-1], h_ctx[1][:, ::-1], h_lat[1][:, ::-1]], 1))
        sargs = (np.asarray(w_in[l]), np.asarray(conv_w[l]), np.asarray(conv_b[l]), np.asarray(dt_bias[l]), np.asarray(a_log[l]),
                 np.asarray(d_skip[l]), np.asarray(ssm_norm[l]))
        zero_y = np.zeros((8704, 512), np.float32)
        r0 = _run(prog("ssd", build_ssd), [dict(_ssd_consts(l, g, 0, *sargs), hT=hT0, yprev=zero_y) for g in range(8)])
        r1 = _run(prog("ssd", build_ssd), [dict(_ssd_consts(l, g, 1, *sargs), hT=hT1, yprev=A(_rev(r0[g]["y"]))) for g in range(8)])
        U = np.concatenate([_rev(np.asarray(r1[g]["u"])) for g in range(8)], 1)
        NT = 1024 if last else 1088
        ims = []
        for core in range(8):
            b, q = core // 4, core % 4
            o = b * 4352
            uo = np.concatenate([U[o + 256 + q * 1024:o + 256 + (q + 1) * 1024], U[o + q * 64:o + (q + 1) * 64]], 0)[:NT]
            m = {"xT": A(xTs[core][:, :NT]), "hT": A(hTs[core][:, :NT]), "attT": A(attTs[core][:, :NT]), "uT": A(uo.T), "mod": mods[core],
                 "gn": A(np.asarray(norm_ffn[l]).reshape(16, 128).T), "ident": ident, "wg": A(np.asarray(w_in[l])[:, 11456:]),
                 "woa": A(w_oa[l]), "wob": A(w_ob[l]), "wout": A(w_out[l])}
            if not last:
                m.update({"w1": A(w1_dense[0]), "w3": A(w3_dense[0]), "w2": A(w2_dense[0])})
            else:
                m["wr"] = A(np.asarray(w_router[0]).reshape(16, 128, 8).transpose(1, 0, 2))
            ims.append(m)
        rc = _run(prog("kc%d" % l, build_kc, l, NT), ims)
        if not last:
            xn = np.empty_like(x); cn = np.empty_like(ctx)
            for core in range(8):
                b, q = core // 4, core % 4
                o = rc[core]["xoutT"]
                xn[b, q * 1024:(q + 1) * 1024] = o[:, :1024].T
                cn[b, q * 64:(q + 1) * 64] = o[:, 1024:].T
            x, ctx = xn, cn
        else:
            h2_all = A(np.concatenate([np.asarray(rc[core]["h2T"]) for core in range(8)], 1))
            gate_all = np.concatenate([rc[core]["gateT"] for core in range(8)], 1)
            modb = A(np.stack([mods[0][:, :, 0], mods[4][:, :, 0]], -1))
            ims = [{"h2T": h2_all, "gbc": np.broadcast_to(gate_all[e], (128, 8192)).copy(), "mod": modb,
                    "w1": A(w1_moe[0][e]), "w3": A(w3_moe[0][e]), "w2": A(w2_moe[0][e])} for e in range(8)]
            re_ = _run(prog("ke", build_ke), ims)
            fnl = A(np.asarray(final_norm).reshape(16, 128).T)
            ims = [{"xT": rc[core]["xoutT"], "parts": A(np.stack([re_[e]["part"][:, core * 1024:(core + 1) * 1024] for e in range(8)], 0)), "fn": fnl}
                   for core in range(8)]
            rf = _run(prog("kf", build_kf), ims)
            out = np.empty((2, 4096, 2048), np.float32)
            for core in range(8):
                b, q = core // 4, core % 4
                out[b, q * 1024:(q + 1) * 1024] = rf[core]["outT"].T
    return out
```

```python
import numpy as np
from contextlib import ExitStack
import concourse.bass as bass
import concourse.mybir as mybir
from concourse.bass_utils import run_bass_kernel_spmd

F32 = mybir.dt.float32
BF16 = mybir.dt.bfloat16
AF = mybir.ActivationFunctionType
ALU = mybir.AluOpType
AX = mybir.AxisListType

SAME_ENGINE_SYNC = True
NDSEM = 24


class Trk:
    __slots__ = ("lw", "rd", "name")

    def __init__(self, name=""):
        self.lw = None
        self.rd = []
        self.name = name


class T:
    __slots__ = ("ap", "trk")

    def __init__(self, ap, trk=None, name=""):
        self.ap = ap
        self.trk = trk if trk is not None else Trk(name)

    def __getitem__(self, k):
        return self.ap[k]


class Op:
    __slots__ = ("eng", "fn", "deps", "dma", "ordinal", "sig", "sigidx", "dsem", "dval", "waits", "prewait")

    def __init__(self, eng, fn, deps, dma):
        self.eng = eng
        self.fn = fn
        self.deps = deps
        self.dma = dma
        self.sig = False
        self.sigidx = 0
        self.dsem = None
        self.dval = 0
        self.waits = []
        self.prewait = None


class Prog:
    ENGS = ("pe", "act", "dve", "pool", "sp")

    def __init__(self, nc):
        self.nc = nc
        self.ops = []
        self.es = ExitStack()
        self._n = 0
        self.bar_deps = set()
        self.bar_start = 0

    def sbuf(self, shape, dtype, name=None):
        self._n += 1
        name = f"sb{self._n}_" + (name or "")
        h = self.es.enter_context(self.nc.sbuf_tensor(name, list(shape), dtype))
        return h

    def psum(self, shape, dtype=F32, name=None):
        self._n += 1
        name = f"ps{self._n}_" + (name or "")
        h = self.es.enter_context(self.nc.psum_tensor(name, list(shape), dtype))
        return h

    def tile(self, shape, dtype, name=None):
        h = self.sbuf(shape, dtype, name)
        return T(h[:], name=name or "")

    def ptile(self, shape, dtype=F32, name=None):
        h = self.psum(shape, dtype, name)
        return T(h[:], name=name or "")

    def op(self, eng, fn, reads=(), writes=(), dma=False):
        i = len(self.ops)
        deps = set()
        for t in reads:
            k = t.trk if isinstance(t, T) else t
            if k.lw is not None:
                deps.add(k.lw)
        for t in writes:
            k = t.trk if isinstance(t, T) else t
            if k.lw is not None:
                deps.add(k.lw)
            deps.update(k.rd)
        for t in reads:
            k = t.trk if isinstance(t, T) else t
            k.rd.append(i)
        for t in writes:
            k = t.trk if isinstance(t, T) else t
            k.lw = i
            k.rd = []
        deps.discard(i)
        deps.update(self.bar_deps)
        self.ops.append(Op(eng, fn, deps, dma))
        return i

    def dma(self, q, out, in_, reads=(), writes=()):
        return self.op(q, lambda e: e.dma_start(out=out, in_=in_), reads, writes, dma=True)

    def mm(self, out, lhsT, rhs, start, stop, reads=(), writes=()):
        return self.op("pe", lambda e: e.matmul(out, lhsT, rhs, start=start, stop=stop), reads, writes)

    def transpose(self, out, in_, ident, reads=(), writes=()):
        return self.op("pe", lambda e: e.transpose(out, in_, ident), reads, writes)

    def act(self, out, in_, func, reads=(), writes=(), **kw):
        return self.op("act", lambda e: e.activation(out, in_, func, **kw), reads, writes)

    def dve(self, fn, reads=(), writes=()):
        return self.op("dve", fn, reads, writes)

    def scope(self):
        prog = self
        class _S:
            def __enter__(s_):
                s_.saved = prog.es
                prog.es = ExitStack()
                return s_
            def __exit__(s_, *a):
                prog.barrier()
                prog.es.close()
                prog.es = s_.saved
                return False
        return _S()

    def barrier(self):
        last = {}
        used = set()
        for i in range(self.bar_start, len(self.ops)):
            o = self.ops[i]
            used.update(o.deps)
        for i in range(self.bar_start, len(self.ops)):
            o = self.ops[i]
            if o.dma:
                if i not in used:
                    last[("dma", i)] = i
            else:
                last[o.eng] = i
        deps = set(last.values()) | set(self.bar_deps)
        self.bar_deps = deps
        self.bar_start = len(self.ops)

    def wait_all(self, eng, ids):
        o = Op(eng, None, set(ids), False)
        self.ops.append(o)
        return len(self.ops) - 1

    def emit(self):
        nc = self.nc
        ops = self.ops
        per = {e: [] for e in self.ENGS}
        for i, o in enumerate(ops):
            o.ordinal = len(per[o.eng])
            per[o.eng].append(i)
        dsems = {}
        dcount = {e: 0 for e in self.ENGS}
        for e in self.ENGS:
            if any(ops[i].dma for i in per[e]):
                dsems[e] = [self.es.enter_context(nc.semaphore(f"d_{e}_{k}")) for k in range(NDSEM)]
        esem = {e: self.es.enter_context(nc.semaphore(f"s_{e}")) for e in self.ENGS}
        known = {e: {s: 0 for s in self.ENGS} for e in self.ENGS}
        known_dma = {e: set() for e in self.ENGS}
        for i, o in enumerate(ops):
            eb = o.eng
            kn = known[eb]
            if o.dma:
                n = dcount[eb]
                dcount[eb] += 1
                o.dsem = dsems[eb][n % NDSEM]
                o.dval = 16 * (n // NDSEM + 1)
                if n >= NDSEM:
                    o.prewait = (o.dsem, 16 * (n // NDSEM))
            need = {}
            for d in o.deps:
                a = ops[d]
                if a.dma:
                    if d not in known_dma[eb]:
                        known_dma[eb].add(d)
                        o.waits.append(("dma", d))
                else:
                    ea = a.eng
                    if ea == eb and (ea == "pe" or not SAME_ENGINE_SYNC):
                        continue
                    if a.ordinal + 1 > kn[ea]:
                        need[ea] = max(need.get(ea, 0), a.ordinal + 1)
            for ea, v in need.items():
                kn[ea] = v
                src = per[ea][v - 1]
                ops[src].sig = True
                o.waits.append(("eng", src))
        for e in self.ENGS:
            c = 0
            for i in per[e]:
                if ops[i].sig:
                    c += 1
                    ops[i].sigidx = c
        self.nsig = {e: sum(1 for i in per[e] if ops[i].sig) for e in self.ENGS}
        self.nops = {e: len(per[e]) for e in self.ENGS}

        def run(eng_name):
            def body(e):
                for i in per[eng_name]:
                    o = ops[i]
                    if o.prewait is not None:
                        e.wait_ge(o.prewait[0], o.prewait[1])
                    for kind, d in o.waits:
                        a = ops[d]
                        if kind == "dma":
                            e.wait_ge(a.dsem, a.dval)
                        else:
                            e.wait_ge(esem[a.eng], a.sigidx)
                    if o.fn is None:
                        continue
                    ins = o.fn(e)
                    if o.dma:
                        ins.then_inc(o.dsem, 16)
                    elif o.sig:
                        ins.then_inc(esem[eng_name], 1)
            return body

        with nc.Block() as block:
            block.tensor(run("pe"))
            block.scalar(run("act"))
            block.vector(run("dve"))
            block.gpsimd(run("pool"))
            block.sync(run("sp"))

    def close(self):
        self.es.close()
D = 2048
KC = 16
EPS = 1e-6


class TA:
    def __init__(self, P, nk, blocks, dtype, name):
        self.blocks = blocks
        self.nt = blocks[-1][0] + blocks[-1][1]
        self.nk = nk
        h = P.sbuf([128, nk, self.nt], dtype, name)
        self.ap = h[:]
        self.trk = [[Trk(f"{name}{k}_{b}") for b in range(len(blocks))] for k in range(nk)]

    def all(self):
        return [t for row in self.trk for t in row]

    def col(self, b):
        return [self.trk[k][b] for k in range(self.nk)]


def rr(lst, st):
    i = st[0] % len(lst)
    st[0] += 1
    return lst[i]


def emit_adaln(P, cT_d, wada_d, bada_d, mod):
    with P.scope():
        cT = P.tile([128, 16, 2], F32, "cT")
        sc = P.tile([128, 16, 2], F32, "scT")
        bt = P.tile([128, 96], F32, "badaT")
        P.dma("sp", cT.ap, cT_d, writes=[cT])
        P.dma("sp", bt.ap, bada_d, writes=[bt])
        P.act(sc.ap, cT.ap, AF.Silu, reads=[cT], writes=[sc])
        wb = [P.tile([128, 16, 512], F32, f"wada{i}") for i in range(2)]
        pm = [P.ptile([128, 512], F32, f"pm{i}") for i in range(2)]
        wv = wada_d.rearrange("(k p) n -> p k n", p=128)
        for cb in range(24):
            w = wb[cb % 2]
            P.dma("sp", w.ap, wv[:, :, cb * 512:(cb + 1) * 512], writes=[w])
            ps = pm[cb % 2]
            for j in range(4):
                for k in range(16):
                    P.mm(ps.ap[:, j * 2:(j + 1) * 2], w.ap[:, k, j * 128:(j + 1) * 128], sc.ap[:, k, :], k == 0, k == 15,
                         reads=[w, sc], writes=[ps])
            P.dve(lambda e, ps=ps, cb=cb: e.tensor_tensor(mod.ap[:, cb * 4:(cb + 1) * 4, :], ps.ap[:, 0:8].rearrange("p (j s) -> p j s", s=2),
                                                           bt.ap[:, cb * 4:(cb + 1) * 4].unsqueeze(2).to_broadcast([128, 4, 2]), ALU.add),
                  reads=[ps, bt], writes=[mod])


def emit_gsc(P, gsc, mod, gn_d, scale_j):
    gn = P.tile([128, 16], F32, "gn")
    P.dma("sp", gn.ap, gn_d, writes=[gn])
    P.dve(lambda e: e.tensor_scalar(gsc.ap, mod.ap[:, scale_j * 16:(scale_j + 1) * 16, :], 1.0, None, ALU.add), reads=[mod], writes=[gsc])
    P.dve(lambda e: e.tensor_tensor(gsc.ap, gsc.ap, gn.ap.unsqueeze(2).to_broadcast([128, 16, 2]), ALU.mult), reads=[gsc, gn], writes=[gsc])


def emit_norm_mod(P, xT, hT, sel, gsc, mod, shift_j, ones, pss, router=None):
    sq = [P.tile([128, 512], BF16, f"sq{i}") for i in range(2)]
    rs = [P.tile([128, 512], F32, f"rs{i}") for i in range(2)]
    tmp = [P.tile([128, 512], F32, f"nt{i}") for i in range(3)]
    epst = P.tile([128, 1], F32, "epst")
    P.dve(lambda e: e.memset(epst.ap, EPS), writes=[epst])
    n = [0]
    m = [0]
    for bi, (t0, nt) in enumerate(xT.blocks):
        s = sel[bi]
        ps = pss[bi % len(pss)]
        for k in range(16):
            q = rr(sq, n)
            P.act(q.ap[:, :nt], xT.ap[:, k, t0:t0 + nt], AF.Square, reads=[xT.trk[k][bi]], writes=[q])
            P.mm(ps.ap[:, :nt], ones.ap, q.ap[:, :nt], k == 0, k == 15, reads=[ones, q], writes=[ps])
        r = rs[bi % 2]
        P.act(r.ap[:, :nt], ps.ap[:, :nt], AF.Sqrt, reads=[ps, epst], writes=[r], bias=epst.ap, scale=1.0 / D)
        P.dve(lambda e, r=r, nt=nt: e.reciprocal(r.ap[:, :nt], r.ap[:, :nt]), reads=[r], writes=[r])
        for k in range(16):
            tt = rr(tmp, m)
            P.dve(lambda e, tt=tt, k=k, r=r, t0=t0, nt=nt: e.tensor_tensor(tt.ap[:, :nt], xT.ap[:, k, t0:t0 + nt], r.ap[:, :nt], ALU.mult),
                  reads=[xT.trk[k][bi], r], writes=[tt])
            if gsc is None:
                P.dve(lambda e, tt=tt, k=k, t0=t0, nt=nt: e.tensor_scalar(hT.ap[:, k, t0:t0 + nt], tt.ap[:, :nt], mod.ap[:, k:k + 1], None, ALU.mult),
                      reads=[tt, mod], writes=[hT.trk[k][bi]])
            elif router is None:
                P.dve(lambda e, tt=tt, k=k, t0=t0, nt=nt, s=s: e.tensor_scalar(hT.ap[:, k, t0:t0 + nt], tt.ap[:, :nt], gsc.ap[:, k, s:s + 1],
                                                                          mod.ap[:, shift_j * 16 + k, s:s + 1], ALU.mult, ALU.add),
                      reads=[tt, gsc, mod], writes=[hT.trk[k][bi]])
            else:
                wr, psr, logT = router
                P.dve(lambda e, tt=tt, k=k, t0=t0, nt=nt, s=s: e.tensor_scalar(tt.ap[:, :nt], tt.ap[:, :nt], gsc.ap[:, k, s:s + 1],
                                                                          mod.ap[:, shift_j * 16 + k, s:s + 1], ALU.mult, ALU.add),
                      reads=[tt, gsc, mod], writes=[tt])
                P.act(hT.ap[:, k, t0:t0 + nt], tt.ap[:, :nt], AF.Copy, reads=[tt], writes=[hT.trk[k][bi]])
                P.mm(psr.ap[:8, :nt], wr.ap[:, k, :], tt.ap[:, :nt], k == 0, k == 15, reads=[wr, tt], writes=[psr])
        if router is not None:
            wr, psr, logT = router
            P.act(logT.ap[:, t0:t0 + nt], psr.ap[:8, :nt], AF.Copy, reads=[psr], writes=[logT])


class WStream:
    def __init__(self, P, nk, mcols, nbuf, name, q="pool"):
        self.P = P
        self.bufs = [P.tile([128, nk, mcols], BF16, f"{name}{i}") for i in range(nbuf)]
        self.n = [0]
        self.nk = nk
        self.q = q

    def load(self, wd, c0, mc):
        t = rr(self.bufs, self.n)
        self.P.dma(self.q, t.ap[:, :, :mc], wd.rearrange("(k p) n -> p k n", p=128)[:, :, c0:c0 + mc], writes=[t])
        return t


def gemm_fm(P, ws, wd, M, rhs, pss, pst, evac, mcols=None):
    mcols = mcols or ws.bufs[0].ap.shape[2]
    nk = rhs.nk
    for c0 in range(0, M, mcols):
        mc = min(mcols, M - c0)
        wt = ws.load(wd, c0, mc)
        for mi in range(mc // 128):
            for bi, (t0, nt) in enumerate(rhs.blocks):
                ps = rr(pss, pst)
                for k in range(nk):
                    P.mm(ps.ap[:, :nt], wt.ap[:, k, mi * 128:(mi + 1) * 128], rhs.ap[:, k, t0:t0 + nt], k == 0, k == nk - 1,
                         reads=[wt, rhs.trk[k][bi]], writes=[ps])
                evac((c0 // 128) + mi, bi, t0, nt, ps)
DFF = 5632
DFFE = 7168
NEXP = 8


def emit_ffn(P, xT, h2T, sel, mod, w1d, w3d, w2d, FF, ws1, ws3, ws2, gTs, psAB, psO, sa_t, tg_t, st, gbc=None):
    for g in range(FF // 256):
        w1t = ws1.load(w1d, g * 256, 256)
        w3t = ws3.load(w3d, g * 256, 256)
        w2t = rr(ws2.bufs, ws2.n)
        P.dma(ws2.q, w2t.ap, w2d[g * 256:(g + 1) * 256, :].rearrange("(j p) n -> p j n", p=128), writes=[w2t])
        gT = rr(gTs, st["g"])
        for bi, (t0, nt) in enumerate(h2T.blocks):
            s = sel[bi]
            for j in range(2):
                pa = rr(psAB, st["ab"])
                for k in range(16):
                    P.mm(pa.ap[:, :nt], w1t.ap[:, k, j * 128:(j + 1) * 128], h2T.ap[:, k, t0:t0 + nt], k == 0, k == 15,
                         reads=[w1t, h2T.trk[k][bi]], writes=[pa])
                pb = rr(psAB, st["ab"])
                for k in range(16):
                    P.mm(pb.ap[:, :nt], w3t.ap[:, k, j * 128:(j + 1) * 128], h2T.ap[:, k, t0:t0 + nt], k == 0, k == 15,
                         reads=[w3t, h2T.trk[k][bi]], writes=[pb])
                sa = rr(sa_t, st["sa"])
                P.act(sa.ap[:, :nt], pa.ap[:, :nt], AF.Silu, reads=[pa], writes=[sa])
                if gbc is None:
                    P.dve(lambda e, sa=sa, pb=pb, gT=gT, j=j, t0=t0, nt=nt: e.tensor_tensor(gT.ap[:, j, t0:t0 + nt], sa.ap[:, :nt], pb.ap[:, :nt], ALU.mult),
                          reads=[sa, pb], writes=[gT.trk[j][bi]])
                else:
                    tg = rr(tg_t, st["tg"])
                    P.dve(lambda e, tg=tg, pb=pb, t0=t0, nt=nt: e.tensor_tensor(tg.ap[:, :nt], pb.ap[:, :nt], gbc.ap[:, t0:t0 + nt], ALU.mult),
                          reads=[pb, gbc], writes=[tg])
                    P.dve(lambda e, sa=sa, tg=tg, gT=gT, j=j, t0=t0, nt=nt: e.tensor_tensor(gT.ap[:, j, t0:t0 + nt], sa.ap[:, :nt], tg.ap[:, :nt], ALU.mult),
                          reads=[sa, tg], writes=[gT.trk[j][bi]])
            for m in range(16):
                po = rr(psO, st["o"])
                for j in range(2):
                    P.mm(po.ap[:, :nt], w2t.ap[:, j, m * 128:(m + 1) * 128], gT.ap[:, j, t0:t0 + nt], j == 0, j == 1,
                         reads=[w2t, gT.trk[j][bi]], writes=[po])
                P.dve(lambda e, po=po, m=m, t0=t0, nt=nt, s=s: e.scalar_tensor_tensor(xT.ap[:, m, t0:t0 + nt], po.ap[:, :nt], mod.ap[:, 80 + m, s:s + 1],
                                                                                 xT.ap[:, m, t0:t0 + nt], ALU.mult, ALU.add),
                      reads=[po, mod, xT.trk[m][bi]], writes=[xT.trk[m][bi]])


def emit_routing(P, logT, gateT, ident, nt_total, PSr):
    with P.scope():
        pt = PSr
        lg = [P.tile([128, 8], F32, f"rt_lg{i}") for i in range(2)]
        mk1 = [P.tile([128, 8], F32, f"rt_m1{i}") for i in range(2)]
        l2 = [P.tile([128, 8], F32, f"rt_l2{i}") for i in range(2)]
        mk2 = [P.tile([128, 8], F32, f"rt_m2{i}") for i in range(2)]
        sc = [P.tile([128, 8], F32, f"rt_sc{i}") for i in range(2)]
        ga = [P.tile([128, 8], F32, f"rt_ga{i}") for i in range(2)]
        for ti in range(nt_total // 128):
            i = ti % 2
            p = pt[i]
            tsl = slice(ti * 128, (ti + 1) * 128)
            P.transpose(p.ap[:, 0:8], logT.ap[:, tsl], ident.ap[:8, :8], reads=[logT, ident], writes=[p])
            P.dve(lambda e, i=i, p=p: e.tensor_copy(lg[i].ap, p.ap[:, 0:8]), reads=[p], writes=[lg[i]])
            P.dve(lambda e, i=i: e.reduce_max(sc[i].ap[:, 0:1], lg[i].ap, AX.X), reads=[lg[i]], writes=[sc[i]])
            P.dve(lambda e, i=i: e.tensor_scalar(mk1[i].ap, lg[i].ap, sc[i].ap[:, 0:1], None, ALU.is_equal), reads=[lg[i], sc[i]], writes=[mk1[i]])
            P.dve(lambda e, i=i: e.scalar_tensor_tensor(l2[i].ap, mk1[i].ap, -1e30, lg[i].ap, ALU.mult, ALU.add), reads=[mk1[i], lg[i]], writes=[l2[i]])
            P.dve(lambda e, i=i: e.reduce_max(sc[i].ap[:, 1:2], l2[i].ap, AX.X), reads=[l2[i]], writes=[sc[i]])
            P.dve(lambda e, i=i: e.tensor_scalar(mk2[i].ap, l2[i].ap, sc[i].ap[:, 1:2], None, ALU.is_equal), reads=[l2[i], sc[i]], writes=[mk2[i]])
            P.dve(lambda e, i=i: e.tensor_tensor(sc[i].ap[:, 2:3], sc[i].ap[:, 0:1], sc[i].ap[:, 1:2], ALU.subtract), reads=[sc[i]], writes=[sc[i]])
            P.act(sc[i].ap[:, 3:4], sc[i].ap[:, 2:3], AF.Sigmoid, reads=[sc[i]], writes=[sc[i]])
            P.act(sc[i].ap[:, 4:5], sc[i].ap[:, 2:3], AF.Sigmoid, reads=[sc[i]], writes=[sc[i]], scale=-1.0)
            P.dve(lambda e, i=i: e.tensor_scalar(ga[i].ap, mk1[i].ap, sc[i].ap[:, 3:4], None, ALU.mult), reads=[mk1[i], sc[i]], writes=[ga[i]])
            P.dve(lambda e, i=i: e.scalar_tensor_tensor(ga[i].ap, mk2[i].ap, sc[i].ap[:, 4:5], ga[i].ap, ALU.mult, ALU.add),
                  reads=[mk2[i], sc[i], ga[i]], writes=[ga[i]])
            P.mm(p.ap[:8, 128:256], ga[i].ap, ident.ap, True, True, reads=[ga[i], ident], writes=[p])
            P.act(gateT.ap[:, tsl], p.ap[:8, 128:256], AF.Copy, reads=[p], writes=[gateT])


def build_kc(layer, NT):
    moe = layer == 1
    nc = bass.Bass("TRN2", target_bir_lowering=False)
    dt = lambda name, shape, dtype=F32, kind="ExternalInput": nc.dram_tensor(name, list(shape), dtype, kind=kind).ap()
    xT_d = dt("xT", [D, NT]); hT_d = dt("hT", [D, NT], BF16); attT_d = dt("attT", [D, NT], BF16); uT_d = dt("uT", [2 * D, NT], BF16)
    mod_d = dt("mod", [128, 96, 2]); gn_d = dt("gn", [128, 16]); ident_d = dt("ident", [128, 128])
    wg_d = dt("wg", [D, 2 * D]); woa_d = dt("woa", [D, D]); wob_d = dt("wob", [2 * D, D]); wout_d = dt("wout", [D, D])
    if moe:
        wr_d = dt("wr", [128, 16, 8])
        h2_d = dt("h2T", [D, NT], BF16, "ExternalOutput"); gate_d = dt("gateT", [8, NT], F32, "ExternalOutput")
    else:
        w1_d = dt("w1", [D, DFF]); w3_d = dt("w3", [D, DFF]); w2_d = dt("w2", [DFF, D])
    out_d = dt("xoutT", [D, NT], F32, "ExternalOutput")
    P = Prog(nc)
    blocks = [(0, 512), (512, 512)] + ([(1024, 64)] if NT > 1024 else [])
    sel = [0, 0, 1]
    view = lambda d: d.rearrange("(k p) t -> p k t", p=128)

    def load_ta(ta, d):
        v = view(d)
        for bi, (t0, nt) in enumerate(ta.blocks):
            P.dma("sp", ta.ap[:, :, t0:t0 + nt], v[:, :, t0:t0 + nt], writes=ta.col(bi))

    mod = P.tile([128, 96, 2], F32, "mod")
    P.dma("sp", mod.ap, mod_d, writes=[mod])
    ones = P.tile([128, 128], BF16, "ones")
    P.dve(lambda e: e.memset(ones.ap, 1.0), writes=[ones])
    ident = P.tile([128, 128], F32, "ident")
    P.dma("sp", ident.ap, ident_d, writes=[ident])
    mrg = TA(P, 16, blocks, BF16, "mrg")
    PS = [P.ptile([128, 512], F32, f"PS{i}") for i in range(8)]
    if True:
        pss = PS[0:4]
        pst = [0]
        with P.scope():
            hT = TA(P, 16, blocks, BF16, "hT")
            load_ta(hT, hT_d)
            ws16 = WStream(P, 16, 256, 2, "ws16")
            with P.scope():
                uT = TA(P, 32, blocks, BF16, "uT")
                load_ta(uT, uT_d)
                ws32 = WStream(P, 32, 256, 2, "ws32")
                def ev_sgb(m, bi, t0, nt, ps):
                    P.act(mrg.ap[:, m, t0:t0 + nt], ps.ap[:, :nt], AF.Sigmoid, reads=[ps], writes=[mrg.trk[m][bi]])
                gemm_fm(P, ws16, wg_d[:, D:2 * D], D, hT, pss, pst, ev_sgb)
                def ev_ob(m, bi, t0, nt, ps):
                    P.dve(lambda e: e.tensor_tensor(mrg.ap[:, m, t0:t0 + nt], ps.ap[:, :nt], mrg.ap[:, m, t0:t0 + nt], ALU.mult),
                          reads=[ps, mrg.trk[m][bi]], writes=[mrg.trk[m][bi]])
                gemm_fm(P, ws32, wob_d, D, uT, pss, pst, ev_ob)
            with P.scope():
                attT = TA(P, 16, blocks, BF16, "attT")
                load_ta(attT, attT_d)
                sga = TA(P, 16, blocks, BF16, "sga")
                tmpa = [P.tile([128, 512], F32, f"tmpa{i}") for i in range(2)]
                tn = [0]
                def ev_sga(m, bi, t0, nt, ps):
                    P.act(sga.ap[:, m, t0:t0 + nt], ps.ap[:, :nt], AF.Sigmoid, reads=[ps], writes=[sga.trk[m][bi]])
                gemm_fm(P, ws16, wg_d[:, 0:D], D, hT, pss, pst, ev_sga)
                def ev_oa(m, bi, t0, nt, ps):
                    tt = rr(tmpa, tn)
                    P.dve(lambda e: e.tensor_tensor(tt.ap[:, :nt], ps.ap[:, :nt], sga.ap[:, m, t0:t0 + nt], ALU.mult),
                          reads=[ps, sga.trk[m][bi]], writes=[tt])
                    P.dve(lambda e: e.tensor_tensor(mrg.ap[:, m, t0:t0 + nt], tt.ap[:, :nt], mrg.ap[:, m, t0:t0 + nt], ALU.add),
                          reads=[tt, mrg.trk[m][bi]], writes=[mrg.trk[m][bi]])
                gemm_fm(P, ws16, woa_d, D, attT, pss, pst, ev_oa)
        xT = TA(P, 16, blocks, F32, "xT")
        load_ta(xT, xT_d)
        ws16b = WStream(P, 16, 256, 2, "ws16b")
        def ev_out(m, bi, t0, nt, ps):
            s = sel[bi]
            P.dve(lambda e: e.scalar_tensor_tensor(xT.ap[:, m, t0:t0 + nt], ps.ap[:, :nt], mod.ap[:, 32 + m, s:s + 1], xT.ap[:, m, t0:t0 + nt],
                                                   ALU.mult, ALU.add), reads=[ps, mod, xT.trk[m][bi]], writes=[xT.trk[m][bi]])
        gemm_fm(P, ws16b, wout_d, D, mrg, pss, pst, ev_out)
    h2T = mrg
    gsc = P.tile([128, 16, 2], F32, "gsc")
    router = None
    if moe:
        logT = P.tile([8, NT], F32, "logT")
        gateT = P.tile([8, NT], F32, "gateT")
    with P.scope():
        emit_gsc(P, gsc, mod, gn_d, 4)
        npss = PS[4:6]
        if moe:
            wr = P.tile([128, 16, 8], F32, "wr")
            P.dma("sp", wr.ap, wr_d, writes=[wr])
            psr = PS[6]
            router = (wr, psr, logT)
        emit_norm_mod(P, xT, h2T, sel, gsc, mod, 3, ones, npss, router)
    if moe:
        emit_routing(P, logT, gateT, ident, NT, PS[0:2])
    outs = []
    ov = view(out_d)
    if moe:
        hv2 = view(h2_d)
        for bi, (t0, nt) in enumerate(blocks):
            outs.append(P.dma("sp", hv2[:, :, t0:t0 + nt], h2T.ap[:, :, t0:t0 + nt], reads=h2T.col(bi)))
        outs.append(P.dma("sp", gate_d, gateT.ap, reads=[gateT]))
    else:
        with P.scope():
            ws1 = WStream(P, 16, 256, 2, "w1s")
            ws3 = WStream(P, 16, 256, 2, "w3s")
            ws2 = WStream(P, 2, 2048, 2, "w2s")
            gTs = [TA(P, 2, blocks, BF16, f"gT{i}") for i in range(2)]
            sa_t = [P.tile([128, 512], F32, f"sa{i}") for i in range(2)]
            tg_t = [P.tile([128, 512], F32, f"tg{i}") for i in range(2)]
            st = {k: [0] for k in ("g", "ab", "sa", "tg", "o")}
            emit_ffn(P, xT, h2T, sel, mod, w1_d, w3_d, w2_d, DFF, ws1, ws3, ws2, gTs, PS[0:4], PS[4:7], sa_t, tg_t, st)
    for bi, (t0, nt) in enumerate(blocks):
        outs.append(P.dma("sp", ov[:, :, t0:t0 + nt], xT.ap[:, :, t0:t0 + nt], reads=xT.col(bi)))
    P.wait_all("sp", outs)
    P.emit()
    return nc, P


def build_ke():
    nc = bass.Bass("TRN2", target_bir_lowering=False)
    dt = lambda name, shape, dtype=F32, kind="ExternalInput": nc.dram_tensor(name, list(shape), dtype, kind=kind).ap()
    h2_d = dt("h2T", [D, 8192], BF16); gbc_d = dt("gbc", [128, 8192]); mod_d = dt("mod", [128, 96, 2])
    w1_d = dt("w1", [D, DFFE]); w3_d = dt("w3", [D, DFFE]); w2_d = dt("w2", [DFFE, D])
    part_d = dt("part", [D, 8192], F32, "ExternalOutput")
    P = Prog(nc)
    PS = [P.ptile([128, 512], F32, f"PS{i}") for i in range(8)]
    blocks = [(0, 512), (512, 512)]
    mod = P.tile([128, 96, 2], F32, "mod")
    P.dma("sp", mod.ap, mod_d, writes=[mod])
    ws1 = WStream(P, 16, 256, 2, "w1s"); ws3 = WStream(P, 16, 256, 2, "w3s"); ws2 = WStream(P, 2, 2048, 2, "w2s")
    gTs = [TA(P, 2, blocks, BF16, f"gT{i}") for i in range(2)]
    sa_t = [P.tile([128, 512], F32, f"sa{i}") for i in range(2)]
    tg_t = [P.tile([128, 512], F32, f"tg{i}") for i in range(2)]
    st = {k: [0] for k in ("g", "ab", "sa", "tg", "o")}
    xTs = [TA(P, 16, blocks, F32, f"acc{i}") for i in range(1)]
    h2s = [TA(P, 16, blocks, BF16, f"h2_{i}") for i in range(1)]
    gbs = [P.tile([128, 1024], F32, f"gb{i}") for i in range(2)]
    hv = h2_d.rearrange("(k p) t -> p k t", p=128); pv = part_d.rearrange("(k p) t -> p k t", p=128)
    outs = []
    for c in range(8):
        xT = xTs[0]; h2T = h2s[0]; gbc = gbs[c % 2]
        c0 = c * 1024
        for bi, (t0, nt) in enumerate(blocks):
            P.dma("sp", h2T.ap[:, :, t0:t0 + nt], hv[:, :, c0 + t0:c0 + t0 + nt], writes=h2T.col(bi))
            for k in range(16):
                P.dve(lambda e, xT=xT, k=k, t0=t0, nt=nt: e.memset(xT.ap[:, k, t0:t0 + nt], 0.0), writes=[xT.trk[k][bi]])
        P.dma("sp", gbc.ap, gbc_d[:, c0:c0 + 1024], writes=[gbc])
        s = 0 if c < 4 else 1
        emit_ffn(P, xT, h2T, [s, s], mod, w1_d, w3_d, w2_d, DFFE, ws1, ws3, ws2, gTs, PS[0:4], PS[4:7], sa_t, tg_t, st, gbc=gbc)
        for bi, (t0, nt) in enumerate(blocks):
            outs.append(P.dma("sp", pv[:, :, c0 + t0:c0 + t0 + nt], xT.ap[:, :, t0:t0 + nt], reads=xT.col(bi)))
    P.wait_all("sp", outs)
    P.emit()
    return nc, P


def build_kf():
    nc = bass.Bass("TRN2", target_bir_lowering=False)
    dt = lambda name, shape, dtype=F32, kind="ExternalInput": nc.dram_tensor(name, list(shape), dtype, kind=kind).ap()
    xT_d = dt("xT", [D, 1024]); parts_d = dt("parts", [8, D, 1024]); fn_d = dt("fn", [128, 16])
    out_d = dt("outT", [D, 1024], F32, "ExternalOutput")
    P = Prog(nc)
    PS = [P.ptile([128, 512], F32, f"PS{i}") for i in range(2)]
    blocks = [(0, 512), (512, 512)]
    ones = P.tile([128, 128], BF16, "ones")
    P.dve(lambda e: e.memset(ones.ap, 1.0), writes=[ones])
    fn = P.tile([128, 16], F32, "fn")
    P.dma("sp", fn.ap, fn_d, writes=[fn])
    xT = TA(P, 16, blocks, F32, "xT"); oT = TA(P, 16, blocks, F32, "oT")
    xv = xT_d.rearrange("(k p) t -> p k t", p=128); ov = out_d.rearrange("(k p) t -> p k t", p=128)
    for bi, (t0, nt) in enumerate(blocks):
        P.dma("sp", xT.ap[:, :, t0:t0 + nt], xv[:, :, t0:t0 + nt], writes=xT.col(bi))
    pb = [P.tile([128, 8, 512], F32, f"pb{i}") for i in range(2)]
    n = 0
    for ex in range(8):
        pvw = parts_d[ex].rearrange("(k p) t -> p k t", p=128)
        for bi, (t0, nt) in enumerate(blocks):
            for hf in range(2):
                t = pb[n % 2]; n += 1
                P.dma("sp", t.ap, pvw[:, hf * 8:(hf + 1) * 8, t0:t0 + nt], writes=[t])
                for k8 in range(8):
                    k = hf * 8 + k8
                    P.dve(lambda e, t=t, k=k, k8=k8, t0=t0, nt=nt: e.tensor_tensor(xT.ap[:, k, t0:t0 + nt], xT.ap[:, k, t0:t0 + nt], t.ap[:, k8, :], ALU.add),
                          reads=[t, xT.trk[k][bi]], writes=[xT.trk[k][bi]])
    emit_norm_mod(P, xT, oT, [0, 0], None, fn, 0, ones, PS)
    outs = []
    for bi, (t0, nt) in enumerate(blocks):
        outs.append(P.dma("sp", ov[:, :, t0:t0 + nt], oT.ap[:, :, t0:t0 + nt], reads=oT.col(bi)))
    P.wait_all("sp", outs)
    P.emit()
    return nc, P
NTOK = 8704
WS_COLS = 1288


def build_ssd():
    nc = bass.Bass("TRN2", target_bir_lowering=False)
    dt_ = lambda name, shape, dtype=F32, kind="ExternalInput": nc.dram_tensor(name, list(shape), dtype, kind=kind).ap()
    hT_d = dt_("hT", [D, NTOK], BF16)
    w_d = dt_("wssd", [D, WS_COLS])
    cw_d = dt_("convw", [128, 6, 5]); cb_d = dt_("convb", [128, 6])
    dtb_d = dt_("dtb", [128, 8]); alog_d = dt_("alog", [128, 8]); dsk_d = dt_("dsk", [128, 2, 8])
    gain_d = dt_("gain", [128, 512])
    yprev_d = dt_("yprev", [NTOK, 512])
    ident_d = dt_("ident", [128, 128]); mU_d = dt_("mU", [128, 128]); mL_d = dt_("mL", [128, 128]); mF_d = dt_("mF", [128, 128])
    y_d = dt_("y", [NTOK, 512], F32, "ExternalOutput")
    u_d = dt_("u", [NTOK, 512], BF16, "ExternalOutput")
    P = Prog(nc)
    PS = [P.ptile([128, 512], F32, f"PS{i}") for i in range(8)]
    cst = lambda shape, d, name, dtype=F32: (lambda t: (P.dma("sp", t.ap, d, writes=[t]), t)[1])(P.tile(shape, dtype, name))
    ident = cst([128, 128], ident_d, "ident"); mU = cst([128, 128], mU_d, "mU"); mL = cst([128, 128], mL_d, "mL"); mF = cst([128, 128], mF_d, "mF")
    cw = cst([128, 6, 5], cw_d, "cw"); cb = cst([128, 6], cb_d, "cb"); dtb = cst([128, 8], dtb_d, "dtb")
    alog = cst([128, 8], alog_d, "alog"); dsk = cst([128, 2, 8], dsk_d, "dsk"); gain = cst([128, 512], gain_d, "gain")
    onesf = P.tile([128, 128], F32, "onesf")
    P.dve(lambda e: e.memset(onesf.ap, 1.0), writes=[onesf])
    A = P.tile([128, 8], F32, "A")
    P.act(A.ap, alog.ap, AF.Exp, reads=[alog], writes=[A])
    P.dve(lambda e: e.tensor_scalar(A.ap, A.ap, -1.0, None, ALU.mult), reads=[A], writes=[A])
    dsum = P.tile([128, 8], F32, "dsum")
    P.dve(lambda e: e.tensor_tensor(dsum.ap, dsk.ap[:, 0, :], dsk.ap[:, 1, :], ALU.add), reads=[dsk], writes=[dsum])
    epst = P.tile([128, 1], F32, "epst")
    P.dve(lambda e: e.memset(epst.ap, EPS), writes=[epst])
    w = P.tile([128, 16, WS_COLS], BF16, "wssd")
    P.dma("pool", w.ap, w_d.rearrange("(k p) n -> p k n", p=128), writes=[w])
    hb = [P.tile([128, 16, 260], BF16, f"hb{i}") for i in range(2)]
    xsT = [P.tile([128, 4, 256], F32, f"xsT{i}") for i in range(2)]
    BTf = [P.tile([128, 256], F32, f"BTf{i}") for i in range(2)]
    BTb = [P.tile([128, 256], BF16, f"BTb{i}") for i in range(2)]
    CTb = [P.tile([128, 256], BF16, f"CTb{i}") for i in range(2)]
    acc = [P.tile([128, 256], F32, f"acc{i}") for i in range(2)]
    S = P.tile([128, 512], F32, "S"); Sb = P.tile([128, 512], BF16, "Sb")
    mk = lambda shape, dtype, name, n=2: [P.tile(shape, dtype, f"{name}{i}") for i in range(n)]
    zs = mk([128, 512], F32, "zs"); xs_sb = mk([128, 512], F32, "xs_sb"); xc = mk([128, 512], BF16, "xc"); xw = mk([128, 512], BF16, "xw")
    Btok = mk([128, 128], BF16, "Btok"); dta = mk([128, 16], F32, "dta"); ex = mk([128, 24], F32, "ex")
    cbm = mk([128, 128], F32, "cbm"); R = mk([128, 4, 128], F32, "R"); dec = mk([128, 4, 128], F32, "dec"); Mt = mk([128, 8, 128], BF16, "Mt")
    yo = mk([128, 512], F32, "yo"); ysb = mk([128, 512], F32, "ysb"); ypv = mk([128, 512], F32, "ypv"); ug = mk([128, 512], F32, "ug")
    usq = mk([128, 512], F32, "usq"); ss = mk([128, 2], F32, "ss"); uo = mk([128, 512], BF16, "uo"); tdt = mk([128, 8], F32, "tdt")
    hview = hT_d.rearrange("(k p) t -> p k t", p=128)
    outs = []
    ci = 0
    for blk in range(NTOK // 256):
        t0 = blk * 256
        bseq = blk % 17
        left0 = bseq in (0, 1)
        right0 = bseq in (0, 16)
        h = hb[blk % 2]
        lo = t0 - (0 if left0 else 2); hi = t0 + 256 + (0 if right0 else 2)
        if left0:
            P.dve(lambda e, h=h: e.memset(h.ap[:, :, 0:2], 0.0), writes=[h])
        if right0:
            P.dve(lambda e, h=h: e.memset(h.ap[:, :, 258:260], 0.0), writes=[h])
        P.dma("pool", h.ap[:, :, (2 if left0 else 0):(258 if right0 else 260)], hview[:, :, lo:hi], writes=[h])
        if bseq == 0:
            P.dve(lambda e: e.memset(S.ap, 0.0), writes=[S])
            P.dve(lambda e: e.memset(Sb.ap, 0.0), writes=[Sb])
        b2 = blk % 2
        for ch in range(6):
            ps = PS[ch % 2]
            c0 = 512 + ch * 128
            for k in range(16):
                P.mm(ps.ap[:, 0:260], w.ap[:, k, c0:c0 + 128], h.ap[:, k, :], k == 0, k == 15, reads=[w, h], writes=[ps])
            a = acc[ch % 2]
            P.dve(lambda e, a=a, ps=ps, ch=ch: e.tensor_scalar(a.ap, ps.ap[:, 0:256], cw.ap[:, ch, 0:1], cb.ap[:, ch:ch + 1], ALU.mult, ALU.add),
                  reads=[ps, cw, cb], writes=[a])
            for j in range(1, 5):
                P.dve(lambda e, a=a, ps=ps, ch=ch, j=j: e.scalar_tensor_tensor(a.ap, ps.ap[:, j:j + 256], cw.ap[:, ch, j:j + 1], a.ap, ALU.mult, ALU.add),
                      reads=[ps, cw, a], writes=[a])
            if ch < 4:
                P.act(xsT[b2].ap[:, ch, :], a.ap, AF.Silu, reads=[a], writes=[xsT[b2]])
            elif ch == 4:
                P.act(BTf[b2].ap, a.ap, AF.Silu, reads=[a], writes=[BTf[b2]])
                P.dve(lambda e, b2=b2: e.tensor_copy(BTb[b2].ap, BTf[b2].ap), reads=[BTf[b2]], writes=[BTb[b2]])
            else:
                P.act(CTb[b2].ap, a.ap, AF.Silu, reads=[a], writes=[CTb[b2]])
        for c in range(2):
            i = ci % 2
            ci += 1
            cs = slice(c * 128, (c + 1) * 128)
            hs = slice(2 + c * 128, 2 + (c + 1) * 128)
            tok0 = t0 + c * 128
            P.dma("pool", ypv[i].ap, yprev_d[tok0:tok0 + 128, :], writes=[ypv[i]])
            pz = PS[2]
            for k in range(16):
                P.mm(pz.ap, h.ap[:, k, hs], w.ap[:, k, 0:512], k == 0, k == 15, reads=[w, h], writes=[pz])
            P.act(zs[i].ap, pz.ap, AF.Silu, reads=[pz], writes=[zs[i]])
            pd = PS[3]
            for k in range(16):
                P.mm(pd.ap[:, 0:8], h.ap[:, k, hs], w.ap[:, k, 1280:1288], k == 0, k == 15, reads=[w, h], writes=[pd])
            P.dve(lambda e, i=i, pd=pd: e.tensor_tensor(tdt[i].ap, pd.ap[:, 0:8], dtb.ap, ALU.add), reads=[pd, dtb], writes=[tdt[i]])
            P.act(tdt[i].ap, tdt[i].ap, AF.Exp, reads=[tdt[i]], writes=[tdt[i]])
            P.act(dta[i].ap[:, 0:8], tdt[i].ap, AF.Ln, reads=[tdt[i]], writes=[dta[i]], bias=1.0)
            P.dve(lambda e, i=i: e.tensor_tensor(dta[i].ap[:, 8:16], dta[i].ap[:, 0:8], A.ap, ALU.mult), reads=[dta[i], A], writes=[dta[i]])
            pc = PS[3]
            P.mm(pc.ap[:, 16:24], mU.ap, dta[i].ap[:, 8:16], True, True, reads=[mU, dta[i]], writes=[pc])
            P.mm(pc.ap[:, 24:32], mL.ap, dta[i].ap[:, 8:16], True, True, reads=[mL, dta[i]], writes=[pc])
            P.mm(pc.ap[:, 32:40], onesf.ap, dta[i].ap[:, 8:16], True, True, reads=[onesf, dta[i]], writes=[pc])
            P.act(ex[i].ap, pc.ap[:, 16:40], AF.Exp, reads=[pc], writes=[ex[i]])
            px = PS[4]
            for ch in range(4):
                P.transpose(px.ap[:, ch * 128:(ch + 1) * 128], xsT[b2].ap[:, ch, cs], ident.ap, reads=[xsT[b2], ident], writes=[px])
            P.act(xs_sb[i].ap, px.ap, AF.Copy, reads=[px], writes=[xs_sb[i]])
            bc = lambda t8: t8.unsqueeze(2).to_broadcast([128, 8, 64])
            v3 = lambda ap: ap.rearrange("p (e q) -> p e q", q=64)
            P.dve(lambda e, i=i: e.tensor_tensor(v3(xc[i].ap), v3(xs_sb[i].ap), bc(dta[i].ap[:, 0:8]), ALU.mult), reads=[xs_sb[i], dta[i]], writes=[xc[i]])
            P.dve(lambda e, i=i: e.tensor_tensor(v3(xw[i].ap), v3(xc[i].ap), bc(ex[i].ap[:, 0:8]), ALU.mult), reads=[xc[i], ex[i]], writes=[xw[i]])
            pb = PS[5]
            P.transpose(pb.ap[:, 0:128], BTf[b2].ap[:, cs], ident.ap, reads=[BTf[b2], ident], writes=[pb])
            P.act(Btok[i].ap, pb.ap[:, 0:128], AF.Copy, reads=[pb], writes=[Btok[i]])
            P.mm(pb.ap[:, 128:256], BTb[b2].ap[:, cs], CTb[b2].ap[:, cs], True, True, reads=[BTb[b2], CTb[b2]], writes=[pb])
            P.dve(lambda e, i=i, pb=pb: e.tensor_tensor(cbm[i].ap, pb.ap[:, 128:256], mF.ap, ALU.mult), reads=[pb, mF], writes=[cbm[i]])
            for hh in range(2):
                r = R[hh]; d = dec[hh]
                P.dve(lambda e, r=r, i=i, hh=hh: e.tensor_tensor(r.ap, mL.ap.unsqueeze(1).to_broadcast([128, 4, 128]),
                                                                 dta[i].ap[:, 8 + hh * 4:12 + hh * 4].unsqueeze(2).to_broadcast([128, 4, 128]), ALU.mult),
                      reads=[mL, dta[i]], writes=[r])
                pg = PS[6 + hh]
                P.mm(pg.ap, mU.ap, r.ap.rearrange("p e l -> p (e l)"), True, True, reads=[mU, r], writes=[pg])
                P.act(d.ap.rearrange("p e l -> p (e l)"), pg.ap, AF.Exp, reads=[pg], writes=[d])
                P.dve(lambda e, d=d, i=i, hh=hh: e.tensor_tensor(Mt[i].ap[:, hh * 4:(hh + 1) * 4, :], d.ap, cbm[i].ap.unsqueeze(1).to_broadcast([128, 4, 128]), ALU.mult),
                      reads=[d, cbm[i]], writes=[Mt[i]])
            py = PS[0]
            for e8 in range(8):
                P.mm(py.ap[:, e8 * 64:(e8 + 1) * 64], Mt[i].ap[:, e8, :], xc[i].ap[:, e8 * 64:(e8 + 1) * 64], True, True, reads=[Mt[i], xc[i]], writes=[py])
            po = PS[1]
            P.mm(po.ap, CTb[b2].ap[:, cs], Sb.ap, True, True, reads=[CTb[b2], Sb], writes=[po])
            P.dve(lambda e, i=i, po=po: e.tensor_tensor(v3(yo[i].ap), v3(po.ap), bc(ex[i].ap[:, 8:16]), ALU.mult), reads=[po, ex[i]], writes=[yo[i]])
            P.dve(lambda e, i=i, py=py: e.tensor_tensor(ysb[i].ap, py.ap, yo[i].ap, ALU.add), reads=[py, yo[i]], writes=[ysb[i]])
            outs.append(P.dma("sp", y_d[tok0:tok0 + 128, :], ysb[i].ap, reads=[ysb[i]]))
            pst_ = PS[2]
            P.mm(pst_.ap, Btok[i].ap, xw[i].ap, True, True, reads=[Btok[i], xw[i]], writes=[pst_])
            P.dve(lambda e, i=i: e.tensor_tensor(v3(S.ap), v3(S.ap), bc(ex[i].ap[:, 16:24]), ALU.mult), reads=[S, ex[i]], writes=[S])
            P.dve(lambda e, pst_=pst_: e.tensor_tensor(S.ap, S.ap, pst_.ap, ALU.add), reads=[S, pst_], writes=[S])
            P.act(Sb.ap, S.ap, AF.Copy, reads=[S], writes=[Sb])
            P.dve(lambda e, i=i: e.tensor_tensor(v3(ug[i].ap), v3(xs_sb[i].ap), bc(dsum.ap), ALU.mult), reads=[xs_sb[i], dsum], writes=[ug[i]])
            P.dve(lambda e, i=i: e.tensor_tensor(ug[i].ap, ug[i].ap, ysb[i].ap, ALU.add), reads=[ug[i], ysb[i]], writes=[ug[i]])
            P.dve(lambda e, i=i: e.tensor_tensor(ug[i].ap, ug[i].ap, ypv[i].ap, ALU.add), reads=[ug[i], ypv[i]], writes=[ug[i]])
            P.dve(lambda e, i=i: e.tensor_tensor(ug[i].ap, ug[i].ap, zs[i].ap, ALU.mult), reads=[ug[i], zs[i]], writes=[ug[i]])
            P.dve(lambda e, i=i: e.memset(ss[i].ap, 0.0), writes=[ss[i]])
            P.act(usq[i].ap, ug[i].ap, AF.Square, reads=[ug[i]], writes=[usq[i], ss[i]], accum_out=ss[i].ap[:, 0:1])
            P.act(ss[i].ap[:, 1:2], ss[i].ap[:, 0:1], AF.Sqrt, reads=[ss[i], epst], writes=[ss[i]], bias=epst.ap, scale=1.0 / 512)
            P.dve(lambda e, i=i: e.reciprocal(ss[i].ap[:, 1:2], ss[i].ap[:, 1:2]), reads=[ss[i]], writes=[ss[i]])
            P.dve(lambda e, i=i: e.scalar_tensor_tensor(uo[i].ap, ug[i].ap, ss[i].ap[:, 1:2], gain.ap, ALU.mult, ALU.mult), reads=[ug[i], ss[i], gain], writes=[uo[i]])
            outs.append(P.dma("sp", u_d[tok0:tok0 + 128, :], uo[i].ap, reads=[uo[i]]))
    P.wait_all("sp", outs)
    P.emit()
    return nc, P
NKEY = 4352
NQ = 1088
SM_SCALE = 192.0 ** -0.5


def build_att(with_ctx=True):
    nc = bass.Bass("TRN2", target_bir_lowering=False)
    dt_ = lambda name, shape, dtype=F32, kind="ExternalInput": nc.dram_tensor(name, list(shape), dtype, kind=kind).ap()
    hTb_d = dt_("hTb", [D, NKEY], BF16); hTq_d = dt_("hTq", [D, NQ], BF16)
    wkv_d = dt_("wkv", [D, 640]); wq_d = dt_("wq", [D, 512])
    wuq_d = dt_("wuq", [512, 4096]); wukv_d = dt_("wukv", [512, 4096])
    qg_d = dt_("qg", [128, 4]); kvg_d = dt_("kvg", [128, 4])
    cosk_d = dt_("cosk", [64, NKEY]); sink_d = dt_("sink", [64, NKEY]); cosq_d = dt_("cosq", [64, NQ]); sinq_d = dt_("sinq", [64, NQ])
    att_d = dt_("attT", [D, NQ], BF16, "ExternalOutput")
    P = Prog(nc)
    PS = [P.ptile([128, 512], F32, f"PS{i}") for i in range(8)]
    ones = P.tile([128, 128], BF16, "ones")
    P.dve(lambda e: e.memset(ones.ap, 1.0), writes=[ones])
    epst = P.tile([128, 1], F32, "epst")
    P.dve(lambda e: e.memset(epst.ap, EPS), writes=[epst])
    kblocks = [(0, 256)] + [(256 + i * 512, 512) for i in range(8)]
    qblocks = [(0, 512), (512, 512)] + ([(1024, 64)] if with_ctx else [])
    ckvnT = TA(P, 4, kblocks, BF16, "ckvnT")
    krot = TA(P, 1, kblocks, BF16, "krot")
    cqnT = TA(P, 4, qblocks, BF16, "cqnT")
    cosq = P.tile([64, NQ], F32, "cosq"); sinq = P.tile([64, NQ], F32, "sinq")
    P.dma("sp", cosq.ap, cosq_d, writes=[cosq]); P.dma("sp", sinq.ap, sinq_d, writes=[sinq])

    def norm4(cf, nt, gain, out_ta, bi, t0, sq, rs, psn):
        for k in range(4):
            P.act(sq.ap[:, :nt], cf.ap[:, k, :nt], AF.Square, reads=[cf], writes=[sq])
            P.mm(psn.ap[:, :nt], ones.ap, sq.ap[:, :nt], k == 0, k == 3, reads=[ones, sq], writes=[psn])
        P.act(rs.ap[:, :nt], psn.ap[:, :nt], AF.Sqrt, reads=[psn, epst], writes=[rs], bias=epst.ap, scale=1.0 / 512)
        P.dve(lambda e: e.reciprocal(rs.ap[:, :nt], rs.ap[:, :nt]), reads=[rs], writes=[rs])
        for k in range(4):
            P.dve(lambda e, k=k: e.scalar_tensor_tensor(out_ta.ap[:, k, t0:t0 + nt], cf.ap[:, k, :nt], gain.ap[:, k:k + 1], rs.ap[:, :nt], ALU.mult, ALU.mult),
                  reads=[cf, gain, rs], writes=[out_ta.trk[k][bi]])

    with P.scope():
        wkv = P.tile([128, 16, 640], BF16, "wkv"); wq = P.tile([128, 16, 512], BF16, "wq")
        P.dma("pool", wkv.ap, wkv_d.rearrange("(k p) n -> p k n", p=128), writes=[wkv])
        P.dma("pool", wq.ap, wq_d.rearrange("(k p) n -> p k n", p=128), writes=[wq])
        qg = P.tile([128, 4], F32, "qg"); kvg = P.tile([128, 4], F32, "kvg")
        P.dma("sp", qg.ap, qg_d, writes=[qg]); P.dma("sp", kvg.ap, kvg_d, writes=[kvg])
        hb = [P.tile([128, 16, 512], BF16, f"hb{i}") for i in range(2)]
        cf = [P.tile([128, 4, 512], F32, f"cf{i}") for i in range(2)]
        sq = P.tile([128, 512], BF16, "sq"); rs = P.tile([128, 512], F32, "rs")
        ck = [P.tile([64, 512], F32, f"ck{i}") for i in range(2)]; sk = [P.tile([64, 512], F32, f"sk{i}") for i in range(2)]
        ra = P.tile([64, 512], F32, "ra"); rb = P.tile([64, 512], F32, "rb")
        hvb = hTb_d.rearrange("(k p) t -> p k t", p=128)
        hvq = hTq_d.rearrange("(k p) t -> p k t", p=128)
        n = 0
        for bi, (t0, nt) in enumerate(kblocks):
            h = hb[n % 2]; c = cf[n % 2]; n += 1
            P.dma("sp", h.ap[:, :, :nt], hvb[:, :, t0:t0 + nt], writes=[h])
            P.dma("sp", ck[bi % 2].ap[:, :nt], cosk_d[:, t0:t0 + nt], writes=[ck[bi % 2]])
            P.dma("sp", sk[bi % 2].ap[:, :nt], sink_d[:, t0:t0 + nt], writes=[sk[bi % 2]])
            for m in range(4):
                ps = PS[m % 2]
                for k in range(16):
                    P.mm(ps.ap[:, :nt], wkv.ap[:, k, m * 128:(m + 1) * 128], h.ap[:, k, :nt], k == 0, k == 15, reads=[wkv, h], writes=[ps])
                P.act(c.ap[:, m, :nt], ps.ap[:, :nt], AF.Copy, reads=[ps], writes=[c])
            pa1, pb1 = PS[2], PS[3]
            for k in range(16):
                P.mm(pa1.ap[:64, :nt], wkv.ap[:, k, 512:576], h.ap[:, k, :nt], k == 0, k == 15, reads=[wkv, h], writes=[pa1])
            for k in range(16):
                P.mm(pb1.ap[:64, :nt], wkv.ap[:, k, 576:640], h.ap[:, k, :nt], k == 0, k == 15, reads=[wkv, h], writes=[pb1])
            P.dve(lambda e, nt=nt, bi=bi: e.tensor_tensor(ra.ap[:, :nt], pa1.ap[:64, :nt], ck[bi % 2].ap[:, :nt], ALU.mult), reads=[pa1, ck[bi % 2]], writes=[ra])
            P.dve(lambda e, nt=nt, bi=bi: e.tensor_tensor(rb.ap[:, :nt], pb1.ap[:64, :nt], sk[bi % 2].ap[:, :nt], ALU.mult), reads=[pb1, sk[bi % 2]], writes=[rb])
            P.dve(lambda e, nt=nt, t0=t0: e.tensor_tensor(krot.ap[:64, 0, t0:t0 + nt], ra.ap[:, :nt], rb.ap[:, :nt], ALU.add), reads=[ra, rb], writes=[krot.trk[0][bi]])
            norm4(c, nt, kvg, ckvnT, bi, t0, sq, rs, PS[4])
        for bi, (t0, nt) in enumerate(qblocks):
            h = hb[n % 2]; c = cf[n % 2]; n += 1
            P.dma("sp", h.ap[:, :, :nt], hvq[:, :, t0:t0 + nt], writes=[h])
            for m in range(4):
                ps = PS[m % 2]
                for k in range(16):
                    P.mm(ps.ap[:, :nt], wq.ap[:, k, m * 128:(m + 1) * 128], h.ap[:, k, :nt], k == 0, k == 15, reads=[wq, h], writes=[ps])
                P.act(c.ap[:, m, :nt], ps.ap[:, :nt], AF.Copy, reads=[ps], writes=[c])
            norm4(c, nt, qg, cqnT, bi, t0, sq, rs, PS[4])
    wuq = P.tile([128, 4, 4096], BF16, "wuq"); wukv = P.tile([128, 4, 4096], BF16, "wukv")
    P.dma("pool", wuq.ap, wuq_d.rearrange("(k p) n -> p k n", p=128), writes=[wuq])
    P.dma("pool", wukv.ap, wukv_d.rearrange("(k p) n -> p k n", p=128), writes=[wukv])
    KnT = [TA(P, 1, kblocks, BF16, f"KnT{i}") for i in range(2)]
    Vt = [P.tile([128, 34, 128], BF16, f"Vt{i}") for i in range(2)]
    QnT = [TA(P, 1, qblocks, BF16, f"QnT{i}") for i in range(2)]
    qrot = [TA(P, 1, qblocks, BF16, f"qrot{i}") for i in range(2)]
    ra2 = P.tile([64, 512], F32, "ra2"); rb2 = P.tile([64, 512], F32, "rb2")
    pT = [P.tile([128, 512], BF16, f"pT{i}") for i in range(3)]
    rec = [P.tile([128, 512], F32, f"rec{i}") for i in range(2)]
    ao = [P.tile([128, NQ], BF16, f"ao{i}") for i in range(2)]
    outs = []
    av = att_d.rearrange("(h p) t -> p h t", p=128)
    pn = [0]
    for hd in range(16):
        i = hd % 2
        c0 = hd * 256
        for bi, (t0, nt) in enumerate(kblocks):
            ps = PS[bi % 2]
            for k in range(4):
                P.mm(ps.ap[:, :nt], wukv.ap[:, k, c0:c0 + 128], ckvnT.ap[:, k, t0:t0 + nt], k == 0, k == 3, reads=[wukv, ckvnT.trk[k][bi]], writes=[ps])
            P.act(KnT[i].ap[:, 0, t0:t0 + nt], ps.ap[:, :nt], AF.Copy, reads=[ps], writes=[KnT[i].trk[0][bi]])
        for g4 in range(9):
            ps = PS[2 + g4 % 2]
            nkt = min(4, 34 - g4 * 4)
            for j in range(nkt):
                kt = g4 * 4 + j
                for k in range(4):
                    P.mm(ps.ap[:, j * 128:(j + 1) * 128], ckvnT.ap[:, k, kt * 128:(kt + 1) * 128], wukv.ap[:, k, c0 + 128:c0 + 256], k == 0, k == 3,
                         reads=[wukv] + ckvnT.col((kt * 128 + 256) // 512 if kt >= 2 else 0)[k:k + 1], writes=[ps])
            P.dve(lambda e, ps=ps, g4=g4, nkt=nkt, i=i: e.tensor_copy(Vt[i].ap[:, g4 * 4:g4 * 4 + nkt, :], ps.ap[:, :nkt * 128].rearrange("p (j d) -> p j d", d=128)),
                  reads=[ps], writes=[Vt[i]])
        for bi, (t0, nt) in enumerate(qblocks):
            ps = PS[4]
            for k in range(4):
                P.mm(ps.ap[:, :nt], wuq.ap[:, k, c0:c0 + 128], cqnT.ap[:, k, t0:t0 + nt], k == 0, k == 3, reads=[wuq, cqnT.trk[k][bi]], writes=[ps])
            P.act(QnT[i].ap[:, 0, t0:t0 + nt], ps.ap[:, :nt], AF.Copy, reads=[ps], writes=[QnT[i].trk[0][bi]])
            pa, pb = PS[5], PS[6]
            for k in range(4):
                P.mm(pa.ap[:64, :nt], wuq.ap[:, k, c0 + 128:c0 + 192], cqnT.ap[:, k, t0:t0 + nt], k == 0, k == 3, reads=[wuq, cqnT.trk[k][bi]], writes=[pa])
            for k in range(4):
                P.mm(pb.ap[:64, :nt], wuq.ap[:, k, c0 + 192:c0 + 256], cqnT.ap[:, k, t0:t0 + nt], k == 0, k == 3, reads=[wuq, cqnT.trk[k][bi]], writes=[pb])
            P.dve(lambda e, nt=nt, t0=t0: e.tensor_tensor(ra2.ap[:, :nt], pa.ap[:64, :nt], cosq.ap[:, t0:t0 + nt], ALU.mult), reads=[pa, cosq], writes=[ra2])
            P.dve(lambda e, nt=nt, t0=t0: e.tensor_tensor(rb2.ap[:, :nt], pb.ap[:64, :nt], sinq.ap[:, t0:t0 + nt], ALU.mult), reads=[pb, sinq], writes=[rb2])
            P.dve(lambda e, nt=nt, t0=t0, i=i: e.tensor_tensor(qrot[i].ap[:64, 0, t0:t0 + nt], ra2.ap[:, :nt], rb2.ap[:, :nt], ALU.add), reads=[ra2, rb2], writes=[qrot[i].trk[0][bi]])
        for bi, (t0, nt) in enumerate(qblocks):
            kts = range(34) if bi < 2 else range(2)
            pO, pD = PS[bi % 2], PS[2 + bi % 2]
            kts = list(kts)
            def emit_S(kt):
                kb = (kt * 128 + 256) // 512 if kt >= 2 else 0
                pS = PS[4 + pn[0] % 3]
                pt = pT[pn[0] % 3]
                pn[0] += 1
                ksl = slice(kt * 128, (kt + 1) * 128)
                P.mm(pS.ap[:, :nt], KnT[i].ap[:, 0, ksl], QnT[i].ap[:, 0, t0:t0 + nt], True, False, reads=[KnT[i].trk[0][kb], QnT[i].trk[0][bi]], writes=[pS])
                P.mm(pS.ap[:, :nt], krot.ap[:64, 0, ksl], qrot[i].ap[:64, 0, t0:t0 + nt], False, True, reads=[krot.trk[0][kb], qrot[i].trk[0][bi]], writes=[pS])
                P.act(pt.ap[:, :nt], pS.ap[:, :nt], AF.Exp, reads=[pS], writes=[pt], scale=SM_SCALE)
                return pt
            pend = emit_S(kts[0])
            for n_, kt in enumerate(kts):
                pt = pend
                if n_ + 1 < len(kts):
                    pend = emit_S(kts[n_ + 1])
                P.mm(pO.ap[:, :nt], Vt[i].ap[:, kt, :], pt.ap[:, :nt], kt == kts[0], kt == kts[-1], reads=[Vt[i], pt], writes=[pO])
                P.mm(pD.ap[:, :nt], ones.ap, pt.ap[:, :nt], kt == kts[0], kt == kts[-1], reads=[ones, pt], writes=[pD])
            r = rec[bi % 2]
            P.dve(lambda e, r=r, pD=pD, nt=nt: e.reciprocal(r.ap[:, :nt], pD.ap[:, :nt]), reads=[pD], writes=[r])
            P.dve(lambda e, r=r, pO=pO, nt=nt, t0=t0, i=i: e.tensor_tensor(ao[i].ap[:, t0:t0 + nt], pO.ap[:, :nt], r.ap[:, :nt], ALU.mult), reads=[pO, r], writes=[ao[i]])
        nq = qblocks[-1][0] + qblocks[-1][1]
        outs.append(P.dma("sp", av[:, hd, :nq], ao[i].ap[:, :nq], reads=[ao[i]]))
    P.wait_all("sp", outs)
    P.emit()
    return nc, P
def rope_tables(q):
    n = 4096
    rows = n // 64
    row = np.repeat(np.arange(rows, dtype=np.float32), 64)
    col = np.tile(np.arange(64, dtype=np.float32), rows)
    inv = (np.float32(10000.0) ** (-np.arange(0, 32, 2, dtype=np.float32) / np.float32(32))).astype(np.float32)
    ang = np.stack([row[:, None] * inv, col[:, None] * inv], axis=1)
    cos = np.cos(ang).astype(np.float32); sin = np.sin(ang).astype(np.float32)
    C = np.zeros((64, n), np.float32); S = np.zeros((64, n), np.float32)
    for ax in range(2):
        for half in range(2):
            r = slice(ax * 32 + half * 16, ax * 32 + half * 16 + 16)
            C[r] = cos[:, ax, :].T
            S[r] = (-sin[:, ax, :].T) if half == 0 else sin[:, ax, :].T
    onesc = np.ones((64, 256), np.float32); zc = np.zeros((64, 256), np.float32)
    cosk = np.concatenate([onesc, C], 1); sink = np.concatenate([zc, S], 1)
    cosq = np.concatenate([C[:, q * 1024:(q + 1) * 1024], onesc[:, :64]], 1); sinq = np.concatenate([S[:, q * 1024:(q + 1) * 1024], zc[:, :64]], 1)
    return {"cosk": cosk, "sink": sink, "cosq": np.ascontiguousarray(cosq), "sinq": np.ascontiguousarray(sinq)}


ROPE_PERM = np.concatenate([np.arange(16, 32), np.arange(0, 16), np.arange(48, 64), np.arange(32, 48)])


def att_weights(w_in, w_uq, w_ukv, q_norm, kv_norm):
    wkv = np.concatenate([w_in[:, 512:1088], w_in[:, 1024:1088][:, ROPE_PERM]], 1)
    u = w_uq.reshape(512, 16, 192)
    wuq = np.concatenate([u, u[:, :, 128:][:, :, ROPE_PERM]], 2).reshape(512, 4096)
    return {"wkv": np.ascontiguousarray(wkv), "wq": np.ascontiguousarray(w_in[:, 0:512]), "wuq": np.ascontiguousarray(wuq), "wukv": w_ukv,
            "qg": np.ascontiguousarray(q_norm.reshape(4, 128).T), "kvg": np.ascontiguousarray(kv_norm.reshape(4, 128).T)}
def build_ka(NT):
    nc = bass.Bass("TRN2", target_bir_lowering=False)
    xT_d = nc.dram_tensor("xT", [D, NT], F32, kind="ExternalInput").ap()
    cT_d = nc.dram_tensor("cT", [128, 16, 2], F32, kind="ExternalInput").ap()
    wada_d = nc.dram_tensor("wada", [D, 6 * D], F32, kind="ExternalInput").ap()
    bada_d = nc.dram_tensor("bada", [128, 96], F32, kind="ExternalInput").ap()
    gn_d = nc.dram_tensor("gn", [128, 16], F32, kind="ExternalInput").ap()
    mod_d = nc.dram_tensor("mod", [128, 96, 2], F32, kind="ExternalOutput").ap()
    hT_d = nc.dram_tensor("hT", [D, NT], BF16, kind="ExternalOutput").ap()
    P = Prog(nc)
    mod = P.tile([128, 96, 2], F32, "mod")
    emit_adaln(P, cT_d, wada_d, bada_d, mod)
    dm = P.dma("sp", mod_d, mod.ap, reads=[mod])
    gsc = P.tile([128, 16, 2], F32, "gsc")
    emit_gsc(P, gsc, mod, gn_d, 1)
    ones = P.tile([128, 128], BF16, "ones")
    P.dve(lambda e: e.memset(ones.ap, 1.0), writes=[ones])
    blocks = [(0, 512), (512, 512)] + ([(1024, 64)] if NT > 1024 else [])
    xT = TA(P, 16, blocks, F32, "xT"); hT = TA(P, 16, blocks, BF16, "hT")
    xv = xT_d.rearrange("(k p) t -> p k t", p=128); hv = hT_d.rearrange("(k p) t -> p k t", p=128)
    for bi, (t0, nt) in enumerate(blocks):
        P.dma("sp", xT.ap[:, :, t0:t0 + nt], xv[:, :, t0:t0 + nt], writes=xT.col(bi))
    pss = [P.ptile([128, 512], F32, f"n_ps{i}") for i in range(2)]
    emit_norm_mod(P, xT, hT, [0, 0, 1], gsc, mod, 0, ones, pss)
    outs = [dm]
    for bi, (t0, nt) in enumerate(blocks):
        outs.append(P.dma("sp", hv[:, :, t0:t0 + nt], hT.ap[:, :, t0:t0 + nt], reads=hT.col(bi)))
    P.wait_all("sp", outs)
    P.emit()
    return nc


def _run(nc, in_maps):
    res = run_bass_kernel_spmd(nc, in_maps, core_ids=list(range(8)))
    return res.results


def _ssd_consts(l, g, d, w_in, conv_w, conv_b, dt_bias, a_log, d_skip, ssm_norm):
    cols = np.concatenate([np.arange(1088 + g * 512, 1088 + (g + 1) * 512), np.arange(5184 + g * 512, 5184 + (g + 1) * 512),
                           np.arange(9280 + g * 128, 9280 + (g + 1) * 128), np.arange(10304 + g * 128, 10304 + (g + 1) * 128),
                           np.arange(11328 + d * 64 + g * 8, 11328 + d * 64 + g * 8 + 8)])
    chans = np.concatenate([np.arange(g * 512, (g + 1) * 512), 4096 + np.arange(g * 128, (g + 1) * 128), 5120 + np.arange(g * 128, (g + 1) * 128)])
    cw = conv_w[:, chans]
    if d == 1:
        cw = cw[::-1]
    kk = np.arange(128)
    hs = slice(8 * g, 8 * g + 8)
    return {"wssd": np.ascontiguousarray(w_in[:, cols]), "convw": np.ascontiguousarray(cw.reshape(5, 6, 128).transpose(2, 1, 0)),
            "convb": np.ascontiguousarray(conv_b[chans].reshape(6, 128).T),
            "dtb": np.broadcast_to(dt_bias[d, hs], (128, 8)).copy(), "alog": np.broadcast_to(a_log[d, hs], (128, 8)).copy(),
            "dsk": np.broadcast_to(d_skip[:, hs], (128, 2, 8)).copy(), "gain": np.broadcast_to(ssm_norm[g * 512:(g + 1) * 512], (128, 512)).copy(),
            "ident": np.eye(128, dtype=np.float32), "mU": (kk[:, None] > kk[None, :]).astype(np.float32),
            "mL": (kk[:, None] <= kk[None, :]).astype(np.float32), "mF": (kk[None, :] >= kk[:, None]).astype(np.float32)}


def _rev(y):
    out = np.empty_like(y)
    for b in range(2):
        o = b * 4352
        out[o:o + 256] = y[o:o + 256][::-1]
        out[o + 256:o + 4352] = y[o + 256:o + 4352][::-1]
    return out


def kernel(x, c, ctx, c_ctx, norm_mix, norm_ffn, w_ada, b_ada, w_in, q_norm, w_uq, kv_norm, w_ukv, conv_w, conv_b, a_log, dt_bias,
           d_skip, ssm_norm, w_oa, w_ob, w_out, w1_dense, w3_dense, w2_dense, w_router, w1_moe, w3_moe, w2_moe, final_norm):
    A = lambda a: np.ascontiguousarray(np.asarray(a))
    x = np.asarray(x, np.float32); ctx = np.asarray(ctx, np.float32)
    ident = np.eye(128, dtype=np.float32)
    progs = {}

    def prog(name, fn, *a):
        if name not in progs:
            r = fn(*a)
            progs[name] = r[0] if isinstance(r, tuple) else r
        return progs[name]

    out = None
    for l in range(2):
        last = l == 1
        ims = []
        for core in range(8):
            b, q = core // 4, core % 4
            xo = np.concatenate([x[b, q * 1024:(q + 1) * 1024], ctx[b, q * 64:(q + 1) * 64]], 0)
            cv = np.stack([np.asarray(c)[b], np.asarray(c_ctx)], 0)
            ims.append({"xT": A(xo.T), "cT": A(cv.reshape(2, 16, 128).transpose(2, 1, 0)), "wada": A(w_ada[l]),
                        "bada": A(np.asarray(b_ada[l]).reshape(96, 128).T), "gn": A(np.asarray(norm_mix[l]).reshape(16, 128).T)})
        ra = _run(prog("ka", build_ka, 1088), ims)
        mods = [r["mod"] for r in ra]
        hTs = [np.asarray(r["hT"]) for r in ra]
        xTs = [im["xT"] for im in ims]
        h_lat = [np.concatenate([hTs[b * 4 + q][:, :1024] for q in range(4)], 1) for b in range(2)]
        h_ctx = [np.concatenate([hTs[b * 4 + q][:, 1024:] for q in range(4)], 1) for b in range(2)]
        aw = att_weights(np.asarray(w_in[l]), np.asarray(w_uq[l]), np.asarray(w_ukv[l]), np.asarray(q_norm[l]), np.asarray(kv_norm[l]))
        ims = []
        for core in range(8):
            b, q = core // 4, core % 4
            m = dict(aw); m.update(rope_tables(q))
            m["hTb"] = A(np.concatenate([h_ctx[b], h_lat[b]], 1)); m["hTq"] = hTs[core]
            ims.append(m)
        rt = _run(prog("att", build_att, True), ims)
        attTs = [np.asarray(r["attT"]) for r in rt]
        hT0 = A(np.concatenate([h_ctx[0], h_lat[0], h_ctx[1], h_lat[1]], 1))
        hT1 = A(np.concatenate([h_ctx[0][:, ::-1], h_lat[0][:, ::-1], h_ctx[1][:, ::-1], h_lat[1][:, ::-1]], 1))
        sargs = (np.asarray(w_in[l]), np.asarray(conv_w[l]), np.asarray(conv_b[l]), np.asarray(dt_bias[l]), np.asarray(a_log[l]),
                 np.asarray(d_skip[l]), np.asarray(ssm_norm[l]))
        zero_y = np.zeros((8704, 512), np.float32)
        r0 = _run(prog("ssd", build_ssd), [dict(_ssd_consts(l, g, 0, *sargs), hT=hT0, yprev=zero_y) for g in range(8)])
        r1 = _run(prog("ssd", build_ssd), [dict(_ssd_consts(l, g, 1, *sargs), hT=hT1, yprev=A(_rev(r0[g]["y"]))) for g in range(8)])
        U = np.concatenate([_rev(np.asarray(r1[g]["u"])) for g in range(8)], 1)
        NT = 1024 if last else 1088
        ims = []
        for core in range(8):
            b, q = core // 4, core % 4
            o = b * 4352
            uo = np.concatenate([U[o + 256 + q * 1024:o + 256 + (q + 1) * 1024], U[o + q * 64:o + (q + 1) * 64]], 0)[:NT]
            m = {"xT": A(xTs[core][:, :NT]), "hT": A(hTs[core][:, :NT]), "attT": A(attTs[core][:, :NT]), "uT": A(uo.T), "mod": mods[core],
                 "gn": A(np.asarray(norm_ffn[l]).reshape(16, 128).T), "ident": ident, "wg": A(np.asarray(w_in[l])[:, 11456:]),
                 "woa": A(w_oa[l]), "wob": A(w_ob[l]), "wout": A(w_out[l])}
            if not last:
                m.update({"w1": A(w1_dense[0]), "w3": A(w3_dense[0]), "w2": A(w2_dense[0])})
            else:
                m["wr"] = A(np.asarray(w_router[0]).reshape(16, 128, 8).transpose(1, 0, 2))
            ims.append(m)
        rc = _run(prog("kc%d" % l, build_kc, l, NT), ims)
        if not last:
            xn = np.empty_like(x); cn = np.empty_like(ctx)
            for core in range(8):
                b, q = core // 4, core % 4
                o = rc[core]["xoutT"]
                xn[b, q * 1024:(q + 1) * 1024] = o[:, :1024].T
                cn[b, q * 64:(q + 1) * 64] = o[:, 1024:].T
            x, ctx = xn, cn
        else:
            h2_all = A(np.concatenate([np.asarray(rc[core]["h2T"]) for core in range(8)], 1))
            gate_all = np.concatenate([rc[core]["gateT"] for core in range(8)], 1)
            modb = A(np.stack([mods[0][:, :, 0], mods[4][:, :, 0]], -1))
            ims = [{"h2T": h2_all, "gbc": np.broadcast_to(gate_all[e], (128, 8192)).copy(), "mod": modb,
                    "w1": A(w1_moe[0][e]), "w3": A(w3_moe[0][e]), "w2": A(w2_moe[0][e])} for e in range(8)]
            re_ = _run(prog("ke", build_ke), ims)
            fnl = A(np.asarray(final_norm).reshape(16, 128).T)
            ims = [{"xT": rc[core]["xoutT"], "parts": A(np.stack([re_[e]["part"][:, core * 1024:(core + 1) * 1024] for e in range(8)], 0)), "fn": fnl}
                   for core in range(8)]
            rf = _run(prog("kf", build_kf), ims)
            out = np.empty((2, 4096, 2048), np.float32)
            for core in range(8):
                b, q = core // 4, core % 4
                out[b, q * 1024:(q + 1) * 1024] = rf[core]["outT"].T
    return out
```
